# Optimizing a Trainium2 kernel written in Bass

```python
import math
import jax
import jax.numpy as jnp
from jax import lax
import numpy as np

D_MODEL = 1024
BATCH = 8
SEQ = 4096
DEPTH = 2

HEAD_DIM = 64
N_EVEN = (DEPTH + 1) // 2
N_ODD = DEPTH // 2
NUM_BUCKETS = 32
MAX_DISTANCE = 128
N_BIAS_HEADS = 16
ALPHA = (2 * DEPTH) ** 0.25
BETA = (8 * DEPTH) ** -0.25
A_HEADS = 8
A_Q_RANK = 256
A_KV_RANK = 128
IDX_HEADS = 16
IDX_DIM = 64
DSA_TOPK = 256
B_HEADS = 8
B_GROUPS = 2
B_HPG = B_HEADS // B_GROUPS
CMP_LEN = 32
CMP_STRIDE = 16
SLC_BLOCK = 64
SLC_TOPN = 16
WINDOW = 512
C_HEADS = 16
MOBA_BLOCK = 256
MOBA_TOPK = 3
D_FF = 2816
N_EXPERTS = 8
TOP_K = 2
D_FF_EXPERT = 3584
EXPERT_ROWS = 256
Q_BLOCK = 64
MOBA_Q_BLOCK = 16
EVEN_SPLITS = (A_Q_RANK, A_KV_RANK, IDX_DIM, IDX_HEADS, B_HEADS * HEAD_DIM) + (B_GROUPS * HEAD_DIM,) * 6 + (B_HEADS * 3,)
EVEN_IN = sum(EVEN_SPLITS)
ODD_IN = 3 * C_HEADS * HEAD_DIM
MIX_WIDTH_EVEN = (A_HEADS + B_HEADS) * HEAD_DIM
MIX_WIDTH_ODD = C_HEADS * HEAD_DIM

kernel_name = 'hybrid_dsa_nsa_moba_moe_deepnorm'


def split_cols(y, sizes):
    cuts = [int(c) for c in np.cumsum(sizes)[:-1]]
    return jnp.split(y, cuts, axis=-1)


def layer_norm(x, g, b, eps=1e-5):
    xf = x.astype(jnp.float32)
    mu = jnp.mean(xf, axis=-1, keepdims=True)
    var = jnp.mean(jnp.square(xf - mu), axis=-1, keepdims=True)
    return ((xf - mu) * lax.rsqrt(var + eps) * g + b).astype(x.dtype)


def rms_norm(x, g, eps=1e-6):
    xf = x.astype(jnp.float32)
    return (xf * lax.rsqrt(jnp.mean(jnp.square(xf), axis=-1, keepdims=True) + eps) * g).astype(x.dtype)


def swiglu(h, w1, w3, w2):
    return (jax.nn.silu(h @ w1) * (h @ w3)) @ w2


def masked_softmax(logits, mask):
    z = jnp.where(mask, logits.astype(jnp.float32), -jnp.inf)
    m = jnp.max(z, axis=-1, keepdims=True)
    m = jnp.where(jnp.isfinite(m), m, 0.0)
    e = jnp.exp(z - m)
    s = jnp.sum(e, axis=-1, keepdims=True)
    return e / jnp.where(s > 0, s, 1.0)


def t5_bucket(dist):
    n = jnp.maximum(dist, 0)
    max_exact = NUM_BUCKETS // 2
    large = max_exact + (jnp.log(jnp.maximum(n, 1).astype(jnp.float32) / max_exact)
                         / math.log(MAX_DISTANCE / max_exact) * (NUM_BUCKETS - max_exact)).astype(jnp.int32)
    large = jnp.minimum(large, NUM_BUCKETS - 1)
    return jnp.where(n < max_exact, n, large)


def dsa_attention(c_q, c_kv, k_idx, w_idx, q_norm, kv_norm, w_uq, w_uk, w_uv, w_qidx, bias_table):
    bsz, seq = c_q.shape[:2]
    scale = HEAD_DIM ** -0.5
    c_q = rms_norm(c_q, q_norm)
    c_kv = rms_norm(c_kv, kv_norm)
    q = jnp.einsum('bsr,rhd->bshd', c_q, w_uq)
    q_lat = jnp.einsum('bshd,chd->bshc', q, w_uk)
    q_idx = jnp.einsum('bsr,rhd->bshd', c_q, w_qidx)
    w_idx = w_idx * IDX_HEADS ** -0.5
    n_keep = min(DSA_TOPK, seq // 4)
    b_idx = jnp.arange(bsz)[:, None, None]
    s_pos = jnp.arange(seq)

    def chunk(c):
        start = c * Q_BLOCK
        t = start + jnp.arange(Q_BLOCK)
        ql = lax.dynamic_slice_in_dim(q_lat, start, Q_BLOCK, axis=1)
        qi = lax.dynamic_slice_in_dim(q_idx, start, Q_BLOCK, axis=1)
        wi = lax.dynamic_slice_in_dim(w_idx, start, Q_BLOCK, axis=1)
        dots = jax.nn.relu(jnp.einsum('bqhd,bsd->bqhs', qi, k_idx))
        score = jnp.einsum('bqhs,bqh->bqs', dots, wi).astype(jnp.float32)
        score = jnp.where(s_pos[None, None, :] <= t[None, :, None], score, -jnp.inf)
        _, idx = lax.top_k(score, n_keep)
        c_sel = c_kv[b_idx, idx]
        dist = t[None, :, None] - idx
        logits = jnp.einsum('bqhr,bqkr->bhqk', ql, c_sel).astype(jnp.float32) * scale
        logits = logits + jnp.moveaxis(bias_table[t5_bucket(dist)], -1, 1)
        p = masked_softmax(logits, (dist >= 0)[:, None])
        o_lat = jnp.einsum('bhqk,bqkr->bqhr', p.astype(c_sel.dtype), c_sel)
        o = jnp.einsum('bqhr,rhd->bqhd', o_lat, w_uv)
        return o.astype(c_q.dtype).reshape(bsz, Q_BLOCK, A_HEADS * HEAD_DIM)

    out = lax.map(chunk, jnp.arange(seq // Q_BLOCK))
    return out.transpose(1, 0, 2, 3).reshape(bsz, seq, A_HEADS * HEAD_DIM)


def nsa_attention(q, k_cmp, v_cmp, k_slc, v_slc, k_win, v_win, gate_logits,
                  pos_k, pos_v, ck1, ck2, cv1, cv2, bias_table):
    bsz, seq = q.shape[:2]
    scale = HEAD_DIM ** -0.5
    q = q.reshape(bsz, seq, B_GROUPS, B_HPG, HEAD_DIM)
    kv_shape = (bsz, seq, B_GROUPS, HEAD_DIM)
    k_cmp, v_cmp, k_slc, v_slc, k_win, v_win = [a.reshape(kv_shape) for a in (k_cmp, v_cmp, k_slc, v_slc, k_win, v_win)]
    n_cmp = (seq - CMP_LEN) // CMP_STRIDE + 1
    cmp_start = np.arange(n_cmp) * CMP_STRIDE
    cmp_tok = cmp_start[:, None] + np.arange(CMP_LEN)[None, :]

    def compress(a, pos, w1, w2):
        blocks = a[:, cmp_tok] + pos[None, None, :, None, :]
        hid = jax.nn.gelu(jnp.einsum('bnlgd,lde->bnge', blocks, w1))
        return jnp.einsum('bnge,ef->bngf', hid, w2)

    kc = compress(k_cmp, pos_k, ck1, ck2)
    vc = compress(v_cmp, pos_v, cv1, cv2)
    cmp_end = jnp.asarray(cmp_start + CMP_LEN - 1, jnp.int32)
    n_slc = seq // SLC_BLOCK
    n_sel = min(SLC_TOPN, n_slc)
    slc_start = np.arange(n_slc) * SLC_BLOCK
    overlap = jnp.asarray(((cmp_start[:, None] + CMP_LEN - 1 >= slc_start[None, :])
                           & (cmp_start[:, None] <= slc_start[None, :] + SLC_BLOCK - 1)).astype(np.float32))
    ks_blocks = k_slc.reshape(bsz, n_slc, SLC_BLOCK, B_GROUPS, HEAD_DIM).transpose(0, 3, 1, 2, 4)
    vs_blocks = v_slc.reshape(bsz, n_slc, SLC_BLOCK, B_GROUPS, HEAD_DIM).transpose(0, 3, 1, 2, 4)
    kw_pad = jnp.pad(k_win, ((0, 0), (WINDOW, 0), (0, 0), (0, 0)))
    vw_pad = jnp.pad(v_win, ((0, 0), (WINDOW, 0), (0, 0), (0, 0)))
    gates = jax.nn.sigmoid(gate_logits.astype(jnp.float32)).reshape(bsz, seq, B_GROUPS, B_HPG, 3)
    table = bias_table.reshape(NUM_BUCKETS, B_GROUPS, B_HPG).transpose(1, 0, 2)
    b_idx = jnp.arange(bsz)[:, None, None, None]
    g_idx = jnp.arange(B_GROUPS)[None, :, None, None]
    blk = jnp.arange(n_slc)

    def chunk(c):
        start = c * Q_BLOCK
        t = start + jnp.arange(Q_BLOCK)
        qc = lax.dynamic_slice_in_dim(q, start, Q_BLOCK, axis=1)
        dist_c = t[:, None] - cmp_end[None, :]
        lg = jnp.einsum('bqgnd,bigd->bgnqi', qc, kc).astype(jnp.float32) * scale
        lg = lg + table[:, t5_bucket(dist_c)].transpose(0, 3, 1, 2)
        p_c = masked_softmax(lg, dist_c >= 0)
        o_c = jnp.einsum('bgnqi,bigd->bqgnd', p_c.astype(vc.dtype), vc)
        cur = t // SLC_BLOCK
        sc = jnp.einsum('bgnqi,ij->bgqj', p_c, overlap)
        admissible = blk[None, :] <= cur[:, None]
        forced = (blk[None, :] == 0) | (blk[None, :] == cur[:, None]) | (blk[None, :] == cur[:, None] - 1)
        sc = jnp.where(admissible, jnp.where(forced, jnp.inf, sc), -jnp.inf)
        _, sel = lax.top_k(sc, n_sel)
        n_key = n_sel * SLC_BLOCK
        k_s = ks_blocks[b_idx, g_idx, sel].reshape(bsz, B_GROUPS, Q_BLOCK, n_key, HEAD_DIM)
        v_s = vs_blocks[b_idx, g_idx, sel].reshape(bsz, B_GROUPS, Q_BLOCK, n_key, HEAD_DIM)
        pos_s = (sel[..., None] * SLC_BLOCK + jnp.arange(SLC_BLOCK)).reshape(bsz, B_GROUPS, Q_BLOCK, n_key)
        dist_s = t[None, None, :, None] - pos_s
        lg = jnp.einsum('bqgnd,bgqkd->bgnqk', qc, k_s).astype(jnp.float32) * scale
        lg = lg + jnp.moveaxis(table[g_idx, t5_bucket(dist_s)], -1, 2)
        p_s = masked_softmax(lg, (dist_s >= 0)[:, :, None])
        o_s = jnp.einsum('bgnqk,bgqkd->bqgnd', p_s.astype(v_s.dtype), v_s)
        pos_w = start - WINDOW + jnp.arange(Q_BLOCK + WINDOW)
        dist_w = t[:, None] - pos_w[None, :]
        valid_w = (dist_w >= 0) & (dist_w < WINDOW) & (pos_w[None, :] >= 0)
        k_w = lax.dynamic_slice_in_dim(kw_pad, start, Q_BLOCK + WINDOW, axis=1)
        v_w = lax.dynamic_slice_in_dim(vw_pad, start, Q_BLOCK + WINDOW, axis=1)
        lg = jnp.einsum('bqgnd,bkgd->bgnqk', qc, k_w).astype(jnp.float32) * scale
        lg = lg + table[:, t5_bucket(dist_w)].transpose(0, 3, 1, 2)
        p_w = masked_softmax(lg, valid_w)
        o_w = jnp.einsum('bgnqk,bkgd->bqgnd', p_w.astype(v_w.dtype), v_w)
        g = lax.dynamic_slice_in_dim(gates, start, Q_BLOCK, axis=1)
        o = g[..., 0:1] * o_c + g[..., 1:2] * o_s + g[..., 2:3] * o_w
        return o.astype(q.dtype).reshape(bsz, Q_BLOCK, B_HEADS * HEAD_DIM)

    out = lax.map(chunk, jnp.arange(seq // Q_BLOCK))
    return out.transpose(1, 0, 2, 3).reshape(bsz, seq, B_HEADS * HEAD_DIM)


def moba_attention(q, k, v, bias_table):
    bsz, seq = q.shape[:2]
    scale = HEAD_DIM ** -0.5
    n_blk = -(-seq // MOBA_BLOCK)
    pad = n_blk * MOBA_BLOCK - seq
    k_pad = jnp.pad(k, ((0, 0), (0, pad), (0, 0), (0, 0)))
    v_pad = jnp.pad(v, ((0, 0), (0, pad), (0, 0), (0, 0)))
    k_blocks = k_pad.reshape(bsz, n_blk, MOBA_BLOCK, C_HEADS, HEAD_DIM)
    k_mean = jnp.mean(k_blocks, axis=2)
    kb = k_blocks.transpose(0, 3, 1, 2, 4)
    vb = v_pad.reshape(bsz, n_blk, MOBA_BLOCK, C_HEADS, HEAD_DIM).transpose(0, 3, 1, 2, 4)
    n_sel = min(MOBA_TOPK, n_blk - 1)
    table = bias_table.T
    b_idx = jnp.arange(bsz)[:, None, None, None]
    h_idx = jnp.arange(C_HEADS)[None, :, None, None]
    blk = jnp.arange(n_blk)

    def chunk(c):
        start = c * MOBA_Q_BLOCK
        t = start + jnp.arange(MOBA_Q_BLOCK)
        own = start // MOBA_BLOCK
        qc = lax.dynamic_slice_in_dim(q, start, MOBA_Q_BLOCK, axis=1)
        k_o = lax.dynamic_slice_in_dim(k_pad, own * MOBA_BLOCK, MOBA_BLOCK, axis=1)
        v_o = lax.dynamic_slice_in_dim(v_pad, own * MOBA_BLOCK, MOBA_BLOCK, axis=1)
        dist_o = t[:, None] - (own * MOBA_BLOCK + jnp.arange(MOBA_BLOCK))[None, :]
        lg_o = jnp.einsum('bqhd,bkhd->bhqk', qc, k_o).astype(jnp.float32) * scale
        lg_o = lg_o + table[:, t5_bucket(dist_o)]
        mask_o = jnp.broadcast_to(dist_o >= 0, lg_o.shape)
        if n_sel == 0:
            p = masked_softmax(lg_o, mask_o)
            o = jnp.einsum('bhqk,bkhd->bqhd', p.astype(v_o.dtype), v_o)
        else:
            gate = jnp.einsum('bqhd,bjhd->bhqj', qc, k_mean).astype(jnp.float32)
            gate = jnp.where(blk < own, gate, -jnp.inf)
            _, sel = lax.top_k(gate, n_sel)
            n_key = n_sel * MOBA_BLOCK
            k_s = kb[b_idx, h_idx, sel].reshape(bsz, C_HEADS, MOBA_Q_BLOCK, n_key, HEAD_DIM)
            v_s = vb[b_idx, h_idx, sel].reshape(bsz, C_HEADS, MOBA_Q_BLOCK, n_key, HEAD_DIM)
            pos_s = (sel[..., None] * MOBA_BLOCK + jnp.arange(MOBA_BLOCK)).reshape(bsz, C_HEADS, MOBA_Q_BLOCK, n_key)
            lg_s = jnp.einsum('bqhd,bhqkd->bhqk', qc, k_s).astype(jnp.float32) * scale
            lg_s = lg_s + table[h_idx, t5_bucket(t[None, None, :, None] - pos_s)]
            mask_s = jnp.broadcast_to((sel < own)[..., None], sel.shape + (MOBA_BLOCK,)).reshape(lg_s.shape)
            p = masked_softmax(jnp.concatenate([lg_s, lg_o], axis=-1), jnp.concatenate([mask_s, mask_o], axis=-1))
            o = (jnp.einsum('bhqk,bhqkd->bqhd', p[..., :n_key].astype(v_s.dtype), v_s)
                 + jnp.einsum('bhqk,bkhd->bqhd', p[..., n_key:].astype(v_o.dtype), v_o))
        return o.astype(q.dtype).reshape(bsz, MOBA_Q_BLOCK, C_HEADS * HEAD_DIM)

    out = lax.map(chunk, jnp.arange(seq // MOBA_Q_BLOCK))
    return out.transpose(1, 0, 2, 3).reshape(bsz, seq, C_HEADS * HEAD_DIM)


def moe_swiglu(h, router, w1, w3, w2):
    bsz, seq, dm = h.shape
    n_tok = bsz * seq
    xf = h.reshape(n_tok, dm)
    logits = (xf @ router).astype(jnp.float32)
    top_val, top_e = lax.top_k(logits, TOP_K)
    gate = jax.nn.softmax(top_val, axis=-1)
    e_flat = top_e.reshape(-1)
    tok_flat = jnp.repeat(jnp.arange(n_tok), TOP_K)
    g_flat = gate.reshape(-1)
    order = jnp.argsort(e_flat)
    e_s, tok_s, g_s = e_flat[order], tok_flat[order], g_flat[order]
    counts = jnp.bincount(e_flat, length=N_EXPERTS)
    padded = (counts + EXPERT_ROWS - 1) // EXPERT_ROWS * EXPERT_ROWS
    start = jnp.cumsum(counts) - counts
    pend = jnp.cumsum(padded)
    pstart = pend - padded
    n_assign = n_tok * TOP_K
    dest = pstart[e_s] + jnp.arange(n_assign) - start[e_s]
    n_rows = -(-n_assign // EXPERT_ROWS) * EXPERT_ROWS + N_EXPERTS * EXPERT_ROWS
    n_groups = n_rows // EXPERT_ROWS
    row_tok = jnp.full((n_rows,), n_tok, jnp.int32).at[dest].set(tok_s)
    x_pad = jnp.concatenate([xf, jnp.zeros((1, dm), xf.dtype)], axis=0)
    x_rows = x_pad[row_tok].reshape(n_groups, EXPERT_ROWS, dm)
    grp_e = jnp.minimum(jnp.searchsorted(pend, jnp.arange(n_groups) * EXPERT_ROWS, side='right'), N_EXPERTS - 1)

    def expert_group(args):
        xg, e = args
        return swiglu(xg, w1[e], w3[e], w2[e])

    y_rows = lax.map(expert_group, (x_rows, grp_e)).reshape(n_rows, dm)
    y = jax.ops.segment_sum(y_rows[dest] * g_s[:, None], tok_s, num_segments=n_tok)
    return y.astype(h.dtype).reshape(bsz, seq, dm)


def setup_inputs(seed: int = 0) -> dict:
    key = jax.random.key(seed)
    keys = iter(jax.random.split(key, 40))

    def nrm(shape, scale):
        return jax.random.normal(next(keys), shape, jnp.float32) * scale

    def gain(shape):
        return 1.0 + nrm(shape, 0.02)

    d = D_MODEL
    return {
        'x': nrm((BATCH, SEQ, d), 1.0),
        'rel_bias': nrm((NUM_BUCKETS, N_BIAS_HEADS), 0.5),
        'e_w_in': nrm((N_EVEN, d, EVEN_IN), d ** -0.5),
        'e_q_norm': gain((N_EVEN, A_Q_RANK)),
        'e_kv_norm': gain((N_EVEN, A_KV_RANK)),
        'e_w_uq': nrm((N_EVEN, A_Q_RANK, A_HEADS, HEAD_DIM), A_Q_RANK ** -0.5),
        'e_w_uk': nrm((N_EVEN, A_KV_RANK, A_HEADS, HEAD_DIM), A_KV_RANK ** -0.5),
        'e_w_uv': nrm((N_EVEN, A_KV_RANK, A_HEADS, HEAD_DIM), A_KV_RANK ** -0.5),
        'e_w_qidx': nrm((N_EVEN, A_Q_RANK, IDX_HEADS, IDX_DIM), A_Q_RANK ** -0.5),
        'e_pos_k': nrm((N_EVEN, CMP_LEN, HEAD_DIM), 0.1),
        'e_pos_v': nrm((N_EVEN, CMP_LEN, HEAD_DIM), 0.1),
        'e_ck1': nrm((N_EVEN, CMP_LEN, HEAD_DIM, HEAD_DIM), (CMP_LEN * HEAD_DIM) ** -0.5),
        'e_ck2': nrm((N_EVEN, HEAD_DIM, HEAD_DIM), HEAD_DIM ** -0.5),
        'e_cv1': nrm((N_EVEN, CMP_LEN, HEAD_DIM, HEAD_DIM), (CMP_LEN * HEAD_DIM) ** -0.5),
        'e_cv2': nrm((N_EVEN, HEAD_DIM, HEAD_DIM), HEAD_DIM ** -0.5),
        'e_w_out': nrm((N_EVEN, MIX_WIDTH_EVEN, d), MIX_WIDTH_EVEN ** -0.5 * BETA),
        'e_ln1_g': gain((N_EVEN, d)),
        'e_ln1_b': nrm((N_EVEN, d), 0.02),
        'e_ffn_w1': nrm((N_EVEN, d, D_FF), d ** -0.5),
        'e_ffn_w3': nrm((N_EVEN, d, D_FF), d ** -0.5),
        'e_ffn_w2': nrm((N_EVEN, D_FF, d), D_FF ** -0.5 * BETA),
        'e_ln2_g': gain((N_EVEN, d)),
        'e_ln2_b': nrm((N_EVEN, d), 0.02),
        'o_w_in': nrm((N_ODD, d, ODD_IN), d ** -0.5),
        'o_w_out': nrm((N_ODD, MIX_WIDTH_ODD, d), MIX_WIDTH_ODD ** -0.5 * BETA),
        'o_ln1_g': gain((N_ODD, d)),
        'o_ln1_b': nrm((N_ODD, d), 0.02),
        'o_router': nrm((N_ODD, d, N_EXPERTS), d ** -0.5),
        'o_moe_w1': nrm((N_ODD, N_EXPERTS, d, D_FF_EXPERT), d ** -0.5),
        'o_moe_w3': nrm((N_ODD, N_EXPERTS, d, D_FF_EXPERT), d ** -0.5),
        'o_moe_w2': nrm((N_ODD, N_EXPERTS, D_FF_EXPERT, d), D_FF_EXPERT ** -0.5 * BETA),
        'o_ln2_g': gain((N_ODD, d)),
        'o_ln2_b': nrm((N_ODD, d), 0.02),
    }


def reference(x, rel_bias,
              e_w_in, e_q_norm, e_kv_norm, e_w_uq, e_w_uk, e_w_uv, e_w_qidx,
              e_pos_k, e_pos_v, e_ck1, e_ck2, e_cv1, e_cv2, e_w_out,
              e_ln1_g, e_ln1_b, e_ffn_w1, e_ffn_w3, e_ffn_w2, e_ln2_g, e_ln2_b,
              o_w_in, o_w_out, o_ln1_g, o_ln1_b, o_router, o_moe_w1, o_moe_w3, o_moe_w2,
              o_ln2_g, o_ln2_b):
    bsz, seq = x.shape[:2]
    for layer in range(DEPTH):
        i = layer // 2
        if layer % 2 == 0:
            parts = split_cols(x @ e_w_in[i], EVEN_SPLITS)
            c_q, c_kv, k_idx, w_idx, q_b = parts[0], parts[1], parts[2], parts[3], parts[4]
            k_c, v_c, k_sl, v_sl, k_w, v_w = parts[5], parts[6], parts[7], parts[8], parts[9], parts[10]
            gate_logits = parts[11]
            o_a = dsa_attention(c_q, c_kv, k_idx, w_idx, e_q_norm[i], e_kv_norm[i],
                                e_w_uq[i], e_w_uk[i], e_w_uv[i], e_w_qidx[i], rel_bias[:, :A_HEADS])
            o_b = nsa_attention(q_b, k_c, v_c, k_sl, v_sl, k_w, v_w, gate_logits,
                                e_pos_k[i], e_pos_v[i], e_ck1[i], e_ck2[i], e_cv1[i], e_cv2[i],
                                rel_bias[:, A_HEADS:A_HEADS + B_HEADS])
            mix = jnp.concatenate([o_a, o_b], axis=-1) @ e_w_out[i]
            h = layer_norm(ALPHA * x + mix, e_ln1_g[i], e_ln1_b[i])
            ffn = swiglu(h, e_ffn_w1[i], e_ffn_w3[i], e_ffn_w2[i])
            x = layer_norm(ALPHA * h + ffn, e_ln2_g[i], e_ln2_b[i])
        else:
            q, k, v = [a.reshape(bsz, seq, C_HEADS, HEAD_DIM) for a in jnp.split(x @ o_w_in[i], 3, axis=-1)]
            mix = moba_attention(q, k, v, rel_bias[:, :C_HEADS]) @ o_w_out[i]
            h = layer_norm(ALPHA * x + mix, o_ln1_g[i], o_ln1_b[i])
            ffn = moe_swiglu(h, o_router[i], o_moe_w1[i], o_moe_w3[i], o_moe_w2[i])
            x = layer_norm(ALPHA * h + ffn, o_ln2_g[i], o_ln2_b[i])
    return x
```

```python
import math
from contextlib import ExitStack

import numpy as np
import ml_dtypes
import concourse.bass as bass
import concourse.mybir as mybir
from concourse.bass_utils import run_bass_kernel_spmd

F32 = mybir.dt.float32
F32R = mybir.dt.float32r
BF16 = mybir.dt.bfloat16
AF = mybir.ActivationFunctionType
ALU = mybir.AluOpType
AX = mybir.AxisListType

S = 4096
D = 1024
NT = S // 128
ALPHA = 4 ** 0.25
EVEN_IN = 1768
D_FF = 2816
D_FFE = 3584
NEXP = 8
NEG = -1.0e30


class Op:
    __slots__ = ("eng", "fn", "deps", "sig", "ticket", "isdma", "slot", "sval", "idx", "prev")

    def __init__(self, eng, fn, isdma):
        self.eng = eng
        self.fn = fn
        self.deps = []
        self.sig = False
        self.ticket = None
        self.isdma = isdma
        self.slot = None
        self.sval = None


class Sched:
    KSLOT = 6
    CENG = ("pe", "act", "dve", "pool")

    def __init__(self, nc, es):
        self.nc = nc
        self.sem = {e: es.enter_context(nc.semaphore("s_" + e)) for e in self.CENG}
        self.cnt = {e: 0 for e in self.CENG}
        self.dsem = {q: [es.enter_context(nc.semaphore("d_%s%d" % (q, i))) for i in range(self.KSLOT)]
                     for q in ("sp", "pool")}
        self.dval = {q: [0] * self.KSLOT for q in ("sp", "pool")}
        self.dnext = {q: 0 for q in ("sp", "pool")}
        self.waited = {e: {} for e in ("pe", "act", "dve", "pool", "sp")}
        self.first_phase = True
        self.begin()

    def begin(self):
        self.ops = []
        self.lastw = {}
        self.readers = {}

    def add(self, eng, fn, reads=(), writes=(), isdma=False):
        op = Op(eng, fn, isdma)
        op.idx = len(self.ops)
        deps = set()
        for k in reads:
            w = self.lastw.get(k)
            if w is not None:
                deps.add(w)
        for k in writes:
            w = self.lastw.get(k)
            if w is not None:
                deps.add(w)
            for r in self.readers.get(k, ()):
                deps.add(r)
        deps.discard(op.idx)
        op.deps = sorted(deps)
        for k in reads:
            self.readers.setdefault(k, []).append(op.idx)
        for k in writes:
            self.lastw[k] = op.idx
            self.readers[k] = []
        self.ops.append(op)
        return op

    def mm(self, out, lhsT, rhs, start=True, stop=True, reads=(), writes=()):
        return self.add("pe", lambda e: e.matmul(out, lhsT, rhs, start=start, stop=stop), reads, writes)

    def tr(self, out, in_, ident, reads=(), writes=()):
        return self.add("pe", lambda e: e.transpose(out, in_, ident), reads, writes)

    def act(self, out, in_, func, reads=(), writes=(), **kw):
        return self.add("act", lambda e: e.activation(out=out, in_=in_, func=func, **kw), reads, writes)

    def dve(self, fn, reads=(), writes=()):
        return self.add("dve", fn, reads, writes)

    def pool(self, fn, reads=(), writes=()):
        return self.add("pool", fn, reads, writes)

    def V(self, name, *args, reads=(), writes=(), **kw):
        return self.add("dve", lambda e: getattr(e, name)(*args, **kw), reads, writes)

    def G(self, name, *args, reads=(), writes=(), **kw):
        return self.add("pool", lambda e: getattr(e, name)(*args, **kw), reads, writes)

    def dma(self, q, out, in_, reads=(), writes=(), **kw):
        return self.add(q, lambda e: e.dma_start(out=out, in_=in_, **kw), reads, writes, isdma=True)

    def emit(self, final=False):
        nc = self.nc
        ops = self.ops
        for op in ops:
            for d in op.deps:
                dop = ops[d]
                if dop.isdma:
                    continue
                if dop.eng == "pe" and op.eng == "pe" and not op.isdma:
                    continue
                dop.sig = True
        lastc = {}
        for op in ops:
            if not op.isdma:
                lastc[op.eng] = op
        for op in lastc.values():
            op.sig = True
        for op in ops:
            if op.isdma:
                q = op.eng
                s = self.dnext[q]
                self.dnext[q] = (s + 1) % self.KSLOT
                op.slot = s
                op.prev = self.dval[q][s]
                self.dval[q][s] += 16
                op.sval = self.dval[q][s]
            elif op.sig:
                self.cnt[op.eng] += 1
                op.ticket = self.cnt[op.eng]
        start_waits = []
        if not self.first_phase:
            for e in self.CENG:
                if self.prev_cnt[e] > 0:
                    start_waits.append((self.sem[e], self.prev_cnt[e]))
            for q in ("sp", "pool"):
                for s in range(self.KSLOT):
                    if self.prev_dval[q][s] > 0:
                        start_waits.append((self.dsem[q][s], self.prev_dval[q][s]))
        streams = {e: [] for e in ("sp", "act", "dve", "pool", "pe")}
        for op in ops:
            streams[op.eng].append(op)

        def run_stream(ename, eng):
            wd = self.waited[ename]

            def wait(sem, val):
                key = id(sem)
                if wd.get(key, 0) >= val:
                    return
                eng.wait_ge(sem, val)
                wd[key] = val

            for sem, val in start_waits:
                wait(sem, val)
            for op in streams[ename]:
                need = {}
                for d in op.deps:
                    dop = ops[d]
                    if dop.isdma:
                        sem, val = self.dsem[dop.eng][dop.slot], dop.sval
                    else:
                        if dop.eng == "pe" and ename == "pe" and not op.isdma:
                            continue
                        sem, val = self.sem[dop.eng], dop.ticket
                    k = id(sem)
                    if k not in need or need[k][1] < val:
                        need[k] = (sem, val)
                if op.isdma and op.prev > 0:
                    sem = self.dsem[ename][op.slot]
                    k = id(sem)
                    if k not in need or need[k][1] < op.prev:
                        need[k] = (sem, op.prev)
                for sem, val in need.values():
                    wait(sem, val)
                ins = op.fn(eng)
                if op.isdma:
                    ins.then_inc(self.dsem[ename][op.slot], 16)
                elif op.sig:
                    ins.then_inc(self.sem[ename], 1)
            if final:
                for q in ("sp", "pool"):
                    for s in range(self.KSLOT):
                        if self.dval[q][s] > 0:
                            wait(self.dsem[q][s], self.dval[q][s])
                for e in self.CENG:
                    if self.cnt[e] > 0:
                        wait(self.sem[e], self.cnt[e])

        with nc.Block() as block:
            @block.sync
            def _(e):
                run_stream("sp", e)

            @block.scalar
            def _(e):
                run_stream("act", e)

            @block.vector
            def _(e):
                run_stream("dve", e)

            @block.gpsimd
            def _(e):
                run_stream("pool", e)

            @block.tensor
            def _(e):
                run_stream("pe", e)

        self.prev_cnt = dict(self.cnt)
        self.prev_dval = {q: list(v) for q, v in self.dval.items()}
        self.first_phase = False
        self.begin()


class Rot:
    def __init__(self, items):
        self.items = items
        self.i = 0

    def next(self):
        it = self.items[self.i]
        self.i = (self.i + 1) % len(self.items)
        return it


def bc_rows(ap1d, nparts, n, off=0):
    return bass.AP(ap1d.tensor, ap1d.offset + off, [[0, nparts], [1, n]])


class Ctx:
    pass


def r32(ap):
    return ap.bitcast(F32)


def rr(ap):
    return ap.bitcast(F32R)


def pipeline(steps, depth=2):
    pend = []
    for fr, bk in steps:
        cx = fr()
        pend.append((bk, cx))
        if len(pend) > depth:
            b, c = pend.pop(0)
            b(c)
    for b, c in pend:
        b(c)


def phase_A(C):
    nc, Sc, T = C.nc, C.S, C.T
    ps = C.ps
    with ExitStack() as es:
        def sb(name, shape, dt=F32):
            return es.enter_context(nc.sbuf_tensor(name, shape, dt))

        Win = sb("A_Win", [128, 8, EVEN_IN], F32R)
        wqi = sb("A_wqi", [128, 2, 1024], F32R)
        Wql = sb("A_Wql", [128, 2, 8, 128], F32R)
        uq = sb("A_uq", [128, 2, 512])
        uk = sb("A_uk", [128, 512])
        tmpT = sb("A_tmpT", [64, 384])
        ident = sb("A_ident", [128, 128])
        onesr = sb("A_onesr", [128, 128], F32R)
        selw = sb("A_selw", [16, 8, 128])
        fold = sb("A_fold", [128, 64])
        qn_col = sb("A_qncol", [128, 2])
        kvn_col = sb("A_kvncol", [128, 1])
        kvn_bc = sb("A_kvnbc", [128, 128])
        xin = [sb("A_xin%d" % i, [128, 4, 1024]) for i in range(2)]
        xT = sb("A_xT", [128, 8, 512], F32R)
        cq = sb("A_cq", [128, 2, 512])
        sq = sb("A_sq", [128, 3, 512], F32R)
        rstd = sb("A_rstd", [128, 2, 512])
        cqn = sb("A_cqn", [128, 2, 512], F32R)
        ckv = sb("A_ckv", [128, 512])
        wT = sb("A_wT", [16, 512])
        absw = sb("A_absw", [16, 512])
        wbc = sb("A_wbc", [128, 2, 512])
        prod = sb("A_prod", [128, 512])
        sstat = sb("A_sstat", [128, 8])
        stg = Rot([(sb("A_stg%d" % i, [128, 512]), ("stg", i)) for i in range(6)])
        psr = Rot([(ps[i], ("ps", i)) for i in range(6)])
        evi = [0]

        def evac(out, in_, reads, writes, scale=None):
            evi[0] += 1
            if evi[0] % 2 == 0:
                if scale is None:
                    Sc.act(out, in_, AF.Copy, reads, writes)
                else:
                    Sc.act(out, in_, AF.Copy, reads, writes, scale=float(scale))
            else:
                if scale is None:
                    Sc.dve(lambda e: e.tensor_copy(out, in_), reads, writes)
                else:
                    Sc.dve(lambda e: e.tensor_scalar(out, in_, float(scale), None, ALU.mult), reads, writes)

        Sc.dma("sp", ident[:], T["c_ident"], writes=["ident"])
        Sc.dma("pool", onesr[:], T["c_ones"], writes=["onesr"])
        Sc.dma("sp", selw[:], T["c_selw"], writes=["selw"])
        Sc.dma("sp", fold[:], T["c_fold"], writes=["fold"])
        for k in range(8):
            Sc.dma("pool", Win[:, k, :], T["e_w_in"][k * 128:(k + 1) * 128, :], writes=[("Win", k)])
        for rc in range(2):
            Sc.dma("pool", wqi[:, rc, :], T["e_w_qidx"][rc * 128:(rc + 1) * 128, :], writes=["wqi"])
            Sc.dma("sp", uq[:, rc, :], T["e_w_uq"][rc * 128:(rc + 1) * 128, :], writes=["uq"])
            Sc.dma("sp", qn_col[:, rc:rc + 1], T["e_q_norm"][rc * 128:(rc + 1) * 128].rearrange("(p o) -> p o", o=1),
                   writes=["qncol"])
        Sc.dma("sp", uk[:], T["e_w_uk"], writes=["uk"])
        Sc.dma("sp", kvn_col[:], T["e_kv_norm"].rearrange("(p o) -> p o", o=1), writes=["kvncol"])
        Sc.dma("sp", kvn_bc[:], bc_rows(T["e_kv_norm"], 128, 128), writes=["kvnbc"])
        for h in range(8):
            p, pk = psr.next()
            for rc in range(2):
                Sc.tr(p[0:64, rc * 128:(rc + 1) * 128], uq[:, rc, h * 64:(h + 1) * 64], ident[:],
                      reads=["uq", "ident"], writes=[pk])
            Sc.tr(p[0:64, 256:384], uk[:, h * 64:(h + 1) * 64], ident[:], reads=["uk", "ident"], writes=[pk])
            Sc.dve(lambda e, p=p: e.tensor_copy(tmpT[:], p[0:64, 0:384]), reads=[pk], writes=["tmpT"])
            p2, pk2 = psr.next()
            for rc in range(2):
                Sc.mm(p2[:, rc * 128:(rc + 1) * 128], tmpT[:, rc * 128:(rc + 1) * 128], tmpT[:, 256:384],
                      reads=["tmpT"], writes=[pk2])
            Sc.act(Wql[:, :, h, :], p2[:, 0:256].rearrange("p (a c) -> p a c", a=2), AF.Copy, reads=[pk2],
                   writes=["Wql"])

        col_chunks = [
            ("kc", 976, 128), ("vc", 1104, 128), ("ks", 1232, 128), ("kw", 1488, 128),
        ]
        for blk in range(S // 512):
            t0 = blk * 512
            xi = xin[blk % 2]
            xk = ("xin", blk % 2)
            Sc.dma("sp", xi[:], T["x"][t0:t0 + 512, :].rearrange("(a p) d -> p a d", p=128), writes=[xk])
            for k in range(8):
                p, pk = psr.next()
                for a in range(4):
                    Sc.tr(p[:, a * 128:(a + 1) * 128], xi[:, a, k * 128:(k + 1) * 128], ident[:],
                          reads=[xk, "ident"], writes=[pk])
                evac(xT[:, k, :], p[:], [pk], [("xT", k)])
            xTk = [("xT", k) for k in range(8)]

            def proj(c0, w, p, pk):
                for k in range(8):
                    Sc.mm(p[0:w, :], Win[:, k, c0:c0 + w], xT[:, k, :], start=(k == 0), stop=(k == 7),
                          reads=[("Win", k), ("xT", k)], writes=[pk])

            for rc in range(2):
                p, pk = psr.next()
                proj(rc * 128, 128, p, pk)
                Sc.act(cq[:, rc, :], p[:], AF.Copy, reads=[pk], writes=[("cq", rc)])
                Sc.dve(lambda e, rc=rc: e.tensor_tensor(sq[:, rc, :], cq[:, rc, :], cq[:, rc, :], ALU.mult),
                       reads=[("cq", rc)], writes=[("sq", rc)])
            p, pk = psr.next()
            for rc in range(2):
                Sc.mm(p[:], onesr[:], sq[:, rc, :], start=(rc == 0), stop=(rc == 1),
                      reads=["onesr", ("sq", rc)], writes=[pk])
            Sc.act(rstd[:, 0, :], p[:], AF.Sqrt, reads=[pk], writes=["rstd0"], scale=1.0 / 256, bias=C.eps6[:])
            Sc.dve(lambda e: e.reciprocal(rstd[:, 0, :], rstd[:, 0, :]), reads=["rstd0"], writes=["rstd0"])
            for rc in range(2):
                Sc.dve(lambda e, rc=rc: e.scalar_tensor_tensor(cqn[:, rc, :], cq[:, rc, :], qn_col[:, rc:rc + 1],
                                                               rstd[:, 0, :], ALU.mult, ALU.mult),
                       reads=[("cq", rc), "qncol", "rstd0"], writes=[("cqn", rc)])
            p, pk = psr.next()
            proj(256, 128, p, pk)
            Sc.act(ckv[:], p[:], AF.Copy, reads=[pk], writes=["ckv"])
            Sc.dve(lambda e: e.tensor_tensor(sq[:, 2, :], ckv[:], ckv[:], ALU.mult), reads=["ckv"], writes=[("sq", 2)])
            p, pk = psr.next()
            Sc.mm(p[:], onesr[:], sq[:, 2, :], reads=["onesr", ("sq", 2)], writes=[pk])
            Sc.act(rstd[:, 1, :], p[:], AF.Sqrt, reads=[pk], writes=["rstd1"], scale=1.0 / 128, bias=C.eps6[:])
            Sc.dve(lambda e: e.reciprocal(rstd[:, 1, :], rstd[:, 1, :]), reads=["rstd1"], writes=["rstd1"])
            st, sk = stg.next()
            Sc.dve(lambda e, st=st: e.scalar_tensor_tensor(st[:], ckv[:], kvn_col[:, 0:1], rstd[:, 1, :], ALU.mult,
                                                           ALU.mult),
                   reads=["ckv", "kvncol", "rstd1"], writes=[sk])
            Sc.dma("sp", T["s_ckvT"][:, t0:t0 + 512], st[:], reads=[sk], writes=["s_ckvT"])
            p, pk = psr.next()
            proj(384, 64, p, pk)
            st, sk = stg.next()
            evac(st[0:64, :], p[0:64, :], [pk], [sk])
            Sc.dma("sp", T["s_kidxT"][:, t0:t0 + 512], st[0:64, :], reads=[sk], writes=["s_kidxT"])
            p, pk = psr.next()
            proj(448, 16, p, pk)
            Sc.act(wT[:], p[0:16, :], AF.Copy, reads=[pk], writes=["wT"], scale=0.25)
            Sc.act(absw[:], p[0:16, :], AF.Abs, reads=[pk], writes=["absw"], scale=0.25)
            for h in range(8):
                p, pk = psr.next()
                for rc in range(2):
                    Sc.mm(p[:], Wql[:, rc, h, :], cqn[:, rc, :], start=(rc == 0), stop=(rc == 1),
                          reads=["Wql", ("cqn", rc)], writes=[pk])
                st, sk = stg.next()
                evac(st[:], p[:], [pk], [sk], scale=0.125)
                Sc.dma("sp", T["s_qlatT"][h * 128:(h + 1) * 128, t0:t0 + 512], st[:], reads=[sk], writes=["s_qlatT"])
            pqs, pqsk = ps[6], ("ps", 6)
            for j in range(8):
                p, pk = psr.next()
                for rc in range(2):
                    Sc.mm(p[:], wqi[:, rc, j * 128:(j + 1) * 128], cqn[:, rc, :], start=(rc == 0), stop=(rc == 1),
                          reads=["wqi", ("cqn", rc)], writes=[pk])
                pb, pbk = psr.next()
                Sc.mm(pb[:, 0:512], selw[:, j, :], absw[:], reads=["selw", "absw"], writes=[pbk])
                Sc.act(wbc[:, 0, :], pb[:, 0:512], AF.Copy, reads=[pbk], writes=["wbc0"])
                pb2, pbk2 = psr.next()
                Sc.mm(pb2[:, 0:512], selw[:, j, :], wT[:], reads=["selw", "wT"], writes=[pbk2])
                Sc.act(wbc[:, 1, :], pb2[:, 0:512], AF.Copy, reads=[pbk2], writes=["wbc1"])
                st, sk = stg.next()
                Sc.dve(lambda e, st=st, p=p: e.tensor_tensor(st[:], p[:], wbc[:, 0, :], ALU.mult),
                       reads=[pk, "wbc0"], writes=[sk])
                Sc.dma("sp", T["s_qaT"][j * 128:(j + 1) * 128, t0:t0 + 512], st[:], reads=[sk], writes=["s_qaT"])
                Sc.dve(lambda e, p=p: e.tensor_tensor(prod[:], p[:], wbc[:, 1, :], ALU.mult),
                       reads=[pk, "wbc1"], writes=["prod"])
                Sc.mm(pqs[0:64, :], fold[:], prod[:], start=(j == 0), stop=(j == 7), reads=["fold", "prod"],
                      writes=[pqsk])
            st, sk = stg.next()
            evac(st[0:64, :], pqs[0:64, :], [pqsk], [sk])
            Sc.dma("sp", T["s_qsT"][:, t0:t0 + 512], st[0:64, :], reads=[sk], writes=["s_qsT"])
            for j in range(4):
                p, pk = psr.next()
                proj(464 + j * 128, 128, p, pk)
                st, sk = stg.next()
                evac(st[:], p[:], [pk], [sk], scale=0.125)
                Sc.dma("sp", T["s_qbT"][j * 128:(j + 1) * 128, t0:t0 + 512], st[:], reads=[sk], writes=["s_qbT"])
            for name, c0, w in col_chunks:
                p, pk = psr.next()
                proj(c0, w, p, pk)
                st, sk = stg.next()
                evac(st[:], p[:], [pk], [sk])
                Sc.dma("sp", T["s_" + name + "T"][:, t0:t0 + 512], st[:], reads=[sk], writes=["s_" + name])
            p, pk = psr.next()
            proj(1744, 24, p, pk)
            st, sk = stg.next()
            Sc.act(st[0:24, :], p[0:24, :], AF.Sigmoid, reads=[pk], writes=[sk])
            Sc.dma("sp", T["s_gatesT"][:, t0:t0 + 512], st[0:24, :], reads=[sk], writes=["s_gatesT"])
            for a in range(4):
                r0 = t0 + a * 128
                p, pk = psr.next()
                for k in range(8):
                    Sc.mm(p[:, 0:128], xT[:, k, a * 128:(a + 1) * 128], Win[:, k, 256:384], start=(k == 0),
                          stop=(k == 7), reads=[("Win", k), ("xT", k)], writes=[pk])
                st, sk = stg.next()
                col = sstat[:, 2 * a:2 * a + 1]
                Sc.act(st[:, 128:256], p[:, 0:128], AF.Square, reads=[pk], writes=[sk, ("ss", a)], accum_out=col)
                Sc.act(col, col, AF.Sqrt, reads=[("ss", a)], writes=[("ss", a)], scale=1.0 / 128, bias=C.eps6[:])
                Sc.dve(lambda e, col=col: e.reciprocal(col, col), reads=[("ss", a)], writes=[("ss", a)])
                Sc.dve(lambda e, st=st, p=p, col=col: e.scalar_tensor_tensor(st[:, 0:128], p[:, 0:128], col,
                                                                             kvn_bc[:], ALU.mult, ALU.mult),
                       reads=[pk, ("ss", a), "kvnbc", sk], writes=[sk])
                Sc.dma("sp", T["s_ckv"][r0:r0 + 128, :], st[:, 0:128], reads=[sk], writes=["s_ckv"])
                p, pk = psr.next()
                for k in range(8):
                    Sc.mm(p[:, 0:16], xT[:, k, a * 128:(a + 1) * 128], Win[:, k, 448:464], start=(k == 0),
                          stop=(k == 7), reads=[("Win", k), ("xT", k)], writes=[pk])
                st, sk = stg.next()
                Sc.act(st[:, 0:16], p[:, 0:16], AF.Sign, reads=[pk], writes=[sk])
                Sc.dma("sp", T["s_sgn"][r0:r0 + 128, :], st[:, 0:16], reads=[sk], writes=["s_sgn"])
                p, pk = psr.next()
                for k in range(8):
                    Sc.mm(p[:, 0:384], xT[:, k, a * 128:(a + 1) * 128], Win[:, k, 1360:1744], start=(k == 0),
                          stop=(k == 7), reads=[("Win", k), ("xT", k)], writes=[pk])
                st, sk = stg.next()
                evac(st[:, 0:384], p[:, 0:384], [pk], [sk])
                Sc.dma("sp", T["s_vs"][r0:r0 + 128, :], st[:, 0:128], reads=[sk], writes=["s_vs"])
                Sc.dma("sp", T["s_vw"][r0:r0 + 128, :], st[:, 256:384], reads=[sk], writes=["s_vw"])
        Sc.emit()


IN_SPECS = {
    "x": ([S, D], F32),
    "e_w_in": ([D, EVEN_IN], F32R),
    "e_q_norm": ([256], F32),
    "e_kv_norm": ([128], F32),
    "e_w_uq": ([256, 512], F32),
    "e_w_uk": ([128, 512], F32),
    "e_w_uv": ([128, 512], F32R),
    "e_w_qidx": ([256, 1024], F32R),
    "c_ident": ([128, 128], F32),
    "c_ones": ([128, 128], F32R),
    "c_selw": ([16, 8, 128], F32),
    "c_fold": ([128, 64], F32),
    "c_negtri": ([128, 128], F32),
    "c_cdiag": ([128, 128], F32),
    "g_bdiag": ([128, 16, 128], F32),
    "g_boff": ([128, 16, 128], F32),
    "g_b31": ([128, 16], F32),
    "e_ck1": ([32, 64, 64], F32), "e_cv1": ([32, 64, 64], F32), "e_ck2": ([64, 64], F32), "e_cv2": ([64, 64], F32),
    "e_pos_kT": ([64, 32], F32), "e_pos_vT": ([64, 32], F32),
    "g_pb": ([32, 8, 128], F32), "c_pm": ([32, 128], F32), "c_xs": ([64, 32, 128], BF16),
    "c_ovl": ([128, 2, 64], F32), "c_shift": ([128, 64], F32R), "c_w4": ([128, 128], F32), "c_gmask": ([24, 6, 4], F32),
    "c_fc": ([128, 32, 64], F32), "c_ac": ([128, 32, 64], F32),
    "c_xm": ([128, 16, 128], F32R), "c_zeros": ([128, S], F32R), "c_gm": ([128, 32, 16], F32), "c_adm": ([128, 32, 16], F32),
    "e_w_out": ([D, D], F32R), "e_ln1_g": ([D], F32), "e_ln1_b": ([D], F32),
    "e_ffn_w1": ([D, D_FF], F32R), "e_ffn_w3": ([D, D_FF], F32R), "e_ffn_w2": ([D_FF, D], F32R),
    "e_ln2_g": ([D], F32), "e_ln2_b": ([D], F32),
    "o_w_in": ([D, 3072], F32R), "o_w_out": ([D, D], F32R), "o_ln1_g": ([D], F32), "o_ln1_b": ([D], F32),
    "o_routerT": ([8 * D], F32),
    "o_moe_w1": ([NEXP, D, D_FFE], F32R), "o_moe_w3": ([NEXP, D, D_FFE], F32R), "o_moe_w2": ([NEXP, D_FFE, D], F32R),
    "o_ln2_g": ([D], F32), "o_ln2_b": ([D], F32),
}

SCRATCH = {
    "s_ckvT": [128, S], "s_ckv": [S, 128], "s_kidxT": [64, S], "s_qlatT": [1024, S], "s_qaT": [1024, S],
    "s_qsT": [64, S], "s_qbT": [512, S], "s_kcT": [128, S], "s_vcT": [128, S], "s_ksT": [128, S],
    "s_kwT": [128, S], "s_vs": [S, 128], "s_vw": [S, 128], "s_gatesT": [24, S],
    "s_oT": [1024, S], "s_sgn": [S, 16],
    "s_h0": [S, D], "s_hT0": [D, S], "s_x1": [S, D], "s_qT1": [D, S], "s_kT1": [D, S], "s_v1": [S, D],
    "s_oT1": [D, S], "s_h1": [S, D], "s_hT1": [D, S], "s_gate": [S, 8],
    "s_su1": [D, S], "s_G": [528, 8, 128], "s_kcc": [64, 2, 256], "s_vcc": [256, 2, 64],
}


def host_gathers(rel_bias):
    g = {}
    ss = np.arange(128)[:, None]
    tt = np.arange(128)[None, :]
    bd = t5_bucket_np(tt - ss)
    bo = t5_bucket_np(128 + tt - ss)
    g["g_bdiag"] = np.ascontiguousarray(rel_bias[bd].transpose(0, 2, 1))
    g["g_boff"] = np.ascontiguousarray(rel_bias[bo].transpose(0, 2, 1))
    g["g_b31"] = np.ascontiguousarray(np.broadcast_to(rel_bias[31][None, :], (128, 16)))
    m = np.arange(32)[:, None]
    dist = np.arange(128)[None, :] - 16 * m + 225
    g["g_pb"] = np.ascontiguousarray(rel_bias[:, 8:16][t5_bucket_np(dist)].transpose(0, 2, 1))
    return g


def host_consts():
    c = {}
    ss = np.arange(128)[:, None]
    tt = np.arange(128)[None, :]
    c["c_negtri"] = np.where(tt <= ss, 0.0, NEG).astype(np.float32)
    c["c_cdiag"] = (tt >= ss).astype(np.float32)
    xm = np.zeros((128, 16, 128), np.float32)
    for b in range(16):
        xm[b, b, :] = 1.0
    c["c_xm"] = xm
    own = (np.arange(32) // 2)[:, None]
    blk = np.arange(16)[None, :]
    c["c_gm"] = np.ascontiguousarray(np.broadcast_to(np.where(blk < own, 0.0, NEG)[None], (128, 32, 16))).astype(np.float32)
    c["c_adm"] = np.ascontiguousarray(np.broadcast_to((blk < own).astype(np.float32)[None], (128, 32, 16)))
    m = np.arange(32)[:, None]
    c["c_pm"] = ((np.arange(128)[None, :] - 16 * m + 225) >= 0).astype(np.float32)
    xs = np.zeros((64, 32, 128), np.float32)
    for j in range(32):
        xs[2 * j, j, 0:64] = 1.0
        xs[2 * j + 1, j, 64:128] = 1.0
    c["c_xs"] = xs
    cs = np.arange(256) * 16
    ss_ = np.arange(64) * 64
    ov = ((cs[:, None] + 31 >= ss_[None, :]) & (cs[:, None] <= ss_[None, :] + 63)).astype(np.float32)
    ov[255] = 0.0
    c["c_ovl"] = np.ascontiguousarray(ov.reshape(2, 128, 64).transpose(1, 0, 2))
    sh_ = np.zeros((128, 64), np.float32)
    sh_[64:128, :] = np.eye(64, dtype=np.float32)
    c["c_shift"] = sh_
    c["c_w4"] = (np.arange(128)[:, None] > np.arange(128)[None, :]).astype(np.float32)
    gmk = np.zeros((24, 6, 4), np.float32)
    for g_ in range(2):
        for br in range(3):
            for h_ in range(4):
                gmk[(4 * g_ + h_) * 3 + br, g_ * 3 + br, h_] = 1.0
    c["c_gmask"] = gmk
    tq = np.arange(S).reshape(32, 128).T
    cur = tq // 64
    jb = np.arange(64)[None, None, :]
    forced = (jb == 0) | (jb == cur[:, :, None]) | (jb == cur[:, :, None] - 1)
    c["c_fc"] = np.where(forced, 1.0e30, NEG).astype(np.float32)
    c["c_ac"] = np.where(jb > cur[:, :, None], NEG, 1.0e30).astype(np.float32)
    c["c_zeros"] = np.zeros((128, S), np.float32)
    c["c_ident"] = np.eye(128, dtype=np.float32)
    c["c_ones"] = np.ones((128, 128), np.float32)
    selw = np.zeros((16, 8, 128), np.float32)
    for j in range(8):
        selw[2 * j, j, 0:64] = 1.0
        selw[2 * j + 1, j, 64:128] = 1.0
    c["c_selw"] = selw
    c["c_fold"] = np.concatenate([np.eye(64, dtype=np.float32)] * 2, axis=0)
    return c


DEBUG_T = {"d_ps": [128, 1024], "d_pt": [128, 512], "d_pt2": [128, 512], "d_MTh": [128, 8, 128], "d_Ed": [128, 8, 128], "d_acc": [128, S], "d_Mm": [128, S], "d_stt": [128, 8], "d_olat": [128, 8, 512], "d_MT": [128, NT, 128]}


def build(phases=("A",), debug_outs=(), dbg_tile=None, debug_ins=()):
    nc = bass.Bass("TRN2", target_bir_lowering=False)
    C = Ctx()
    C.nc = nc
    C.dbg_tile = dbg_tile
    T = {}
    if dbg_tile is not None:
        for name, shape in DEBUG_T.items():
            T[name] = nc.dram_tensor(name, shape, F32, kind="ExternalOutput").ap()
    used = needed_inputs(phases)
    for name, (shape, dt) in IN_SPECS.items():
        if name in used:
            T[name] = nc.dram_tensor(name, shape, dt, kind="ExternalInput").ap()
    for name, shape in SCRATCH.items():
        kind = "ExternalOutput" if name in debug_outs else ("ExternalInput" if name in debug_ins else "Internal")
        T[name] = nc.dram_tensor(name, shape, F32, kind=kind).ap()
    T["out"] = nc.dram_tensor("out", [S, D], F32, kind="ExternalOutput").ap()
    C.T = T
    with ExitStack() as es:
        C.S = Sched(nc, es)
        C.ps = [es.enter_context(nc.psum_tensor("ps%d" % i, [128, 512], F32)) for i in range(8)]
        C.eps6 = es.enter_context(nc.sbuf_tensor("eps6", [128, 1], F32))
        C.eps5 = es.enter_context(nc.sbuf_tensor("eps5", [128, 1], F32))
        C.S.dve(lambda e: e.memset(C.eps6[:], 1e-6), writes=["eps6"])
        C.S.dve(lambda e: e.memset(C.eps5[:], 1e-5), writes=["eps5"])
        C.S.emit()
        if "A" in phases:
            phase_A(C)
        if "B" in phases:
            phase_B(C)
        if "C" in phases:
            phase_C(C)
        if "D1" in phases:
            phase_outproj(C, "D1_", T["s_oT"], T["e_w_out"], T["x"], T["e_ln1_g"], T["e_ln1_b"], T["s_h0"], T["s_hT0"])
        if "D2" in phases:
            phase_ffn(C, "D2_", T["s_hT0"], T["s_h0"], [T["e_ffn_w1"]], [T["e_ffn_w3"]], [T["e_ffn_w2"]], D_FF, None,
                      T["e_ln2_g"], T["e_ln2_b"], T["s_x1"])
        if "E" in phases:
            phase_E(C)
        if "F" in phases:
            phase_F(C)
        if "G" in phases:
            phase_outproj(C, "G_", T["s_oT1"], T["o_w_out"], T["s_x1"], T["o_ln1_g"], T["o_ln1_b"], T["s_h1"],
                          T["s_hT1"], router=T["o_routerT"], gate_dst=T["s_gate"], su_src=T["s_su1"])
        if "H" in phases:
            phase_ffn(C, "H_", T["s_hT1"], T["s_h1"], [T["o_moe_w1"][e_] for e_ in range(NEXP)],
                      [T["o_moe_w3"][e_] for e_ in range(NEXP)], [T["o_moe_w2"][e_] for e_ in range(NEXP)], D_FFE,
                      T["s_gate"], T["o_ln2_g"], T["o_ln2_b"], T["out"])
        C.S.dve(lambda e: e.memset(C.eps6[:], 1e-6), writes=["eps6"])
        C.S.emit(final=True)
    return nc


PHASE_INPUTS = {
    "A": ["x", "e_w_in", "e_q_norm", "e_kv_norm", "e_w_uq", "e_w_uk", "e_w_qidx", "c_ident", "c_ones", "c_selw",
          "c_fold"],
    "B": ["c_ident", "c_ones", "c_negtri", "c_cdiag", "g_bdiag", "g_boff", "g_b31", "e_w_uv", "c_zeros"],
    "C": ["e_ck1", "e_cv1", "e_ck2", "e_cv2", "e_pos_kT", "e_pos_vT", "g_pb", "c_pm", "c_xs", "c_ovl", "c_w4",
          "c_gmask", "c_fc", "c_ac", "c_shift", "c_zeros", "c_ident", "c_ones", "c_cdiag", "g_bdiag", "g_boff", "g_b31"],
    "D1": ["x", "e_w_out", "e_ln1_g", "e_ln1_b", "c_ident"],
    "D2": ["e_ffn_w1", "e_ffn_w3", "e_ffn_w2", "e_ln2_g", "e_ln2_b"],
    "E": ["o_w_in", "c_ident"],
    "F": ["c_ident", "c_ones", "c_cdiag", "g_bdiag", "g_boff", "g_b31", "c_xm", "c_gm", "c_adm", "c_zeros"],
    "G": ["o_w_out", "o_ln1_g", "o_ln1_b", "o_routerT", "c_ident"],
    "H": ["o_moe_w1", "o_moe_w3", "o_moe_w2", "o_ln2_g", "o_ln2_b"],
}


def needed_inputs(phases):
    u = set()
    for p in phases:
        u.update(PHASE_INPUTS[p])
    return u


NIT = 16


def t5_bucket_np(dist):
    n = np.maximum(dist, 0)
    large = 16 + (np.log(np.maximum(n, 1).astype(np.float32) / np.float32(16)) / np.float32(math.log(8.0))
                  * np.float32(16)).astype(np.int32)
    large = np.minimum(large, 31)
    return np.where(n < 16, n, large)


def make_E(C, es, h0, nh, tagp):
    nc, Sc, T = C.nc, C.S, C.T
    Ed = es.enter_context(nc.sbuf_tensor(tagp + "Ed", [128, nh, 128], F32))
    Eo = es.enter_context(nc.sbuf_tensor(tagp + "Eo", [128, nh, 128], F32))
    b31 = es.enter_context(nc.sbuf_tensor(tagp + "b31", [128, nh], F32))
    cd = es.enter_context(nc.sbuf_tensor(tagp + "cd", [128, 128], F32))
    Sc.dma("sp", Ed[:], T["g_bdiag"][:, h0:h0 + nh, :], writes=[tagp + "Ed"])
    Sc.dma("sp", Eo[:], T["g_boff"][:, h0:h0 + nh, :], writes=[tagp + "Eo"])
    Sc.dma("sp", b31[:], T["g_b31"][:, h0:h0 + nh], writes=[tagp + "b31"])
    Sc.dma("sp", cd[:], T["c_cdiag"], writes=[tagp + "cd"])
    for E, k in ((Ed, tagp + "Ed"), (Eo, tagp + "Eo")):
        Sc.dve(lambda e, E=E: e.tensor_tensor(E[:], E[:], b31[:].unsqueeze(2).to_broadcast([128, nh, 128]),
                                              ALU.subtract), reads=[k, tagp + "b31"], writes=[k])
        Sc.act(E[:], E[:], AF.Exp, reads=[k], writes=[k])
    Sc.dve(lambda e: e.tensor_tensor(Ed[:], Ed[:], cd[:].unsqueeze(1).to_broadcast([128, nh, 128]), ALU.mult),
           reads=[tagp + "Ed", tagp + "cd"], writes=[tagp + "Ed"])
    return Ed, Eo


def phase_B(C):
    nc, Sc, T = C.nc, C.S, C.T
    ps = C.ps
    with ExitStack() as es:
        def sb(name, shape, dt=F32):
            return es.enter_context(nc.sbuf_tensor(name, shape, dt))

        kidx2 = sb("B_kidx2", [128, S], F32R)
        ckvT = sb("B_ckvT", [128, S], F32R)
        ckv = sb("B_ckv", [128, NT, 128], F32R)
        onesr = sb("B_onesr", [128, 128], F32R)
        ident = sb("B_ident", [128, 128])
        negtri = sb("B_negtri", [128, 128])
        wuv = sb("B_wuv", [128, 512], F32R)
        qa = sb("B_qa", [128, 8, 2, 512], F32R)
        ql = sb("B_ql", [128, 4, 8, 128], F32R)
        acc = sb("B_acc", [128, S])
        Mm = sb("B_Mm", [128, S])
        MT = sb("B_MT", [128, NT, 128], mybir.dt.bfloat16)
        MTh = sb("B_MTh", [128, 8, 128])
        tmps = Rot([(sb("B_tmp%d" % i, [128, 512], F32R), ("tmp", i)) for i in range(4)])
        pts = Rot([(sb("B_pt%d" % i, [128, 512]), ("pt", i)) for i in range(3)])
        pt2s = Rot([(sb("B_pt2%d" % i, [128, 512], F32R), ("pt2", i)) for i in range(3)])
        olat = sb("B_olat", [128, 8, 512], F32R)
        rec = sb("B_rec", [128, 2, 512])
        stt = sb("B_stt", [128, 8])
        ost = Rot([(sb("B_ost%d" % i, [64, 512]), ("ost", i)) for i in range(2)])
        Ed, Eo = make_E(C, es, 0, 8, "B_")
        sg = sb("B_sg", [128, 4, 16])
        dg = sb("B_dg", [128, 16, 128], F32R)

        Sc.dma("pool", kidx2[0:64, :], T["s_kidxT"].bitcast(F32R), writes=["kidx2"])
        Sc.dma("pool", kidx2[64:128, :], T["s_kidxT"].bitcast(F32R), writes=["kidx2"])
        Sc.dma("pool", ckvT[:], T["s_ckvT"].bitcast(F32R), writes=["ckvT"])
        Sc.dma("pool", ckv[:], T["s_ckv"].bitcast(F32R).rearrange("(j p) c -> p j c", p=128), writes=["ckv"])
        Sc.dma("pool", onesr[:], T["c_ones"], writes=["onesr"])
        Sc.dma("pool", qa[64:128, :, 0, :], T["c_zeros"][64:128, :].rearrange("p (j t) -> p j t", j=8), writes=["qa"])
        Sc.dma("pool", qa[0:64, :, 1, :], T["c_zeros"][0:64, :].rearrange("p (j t) -> p j t", j=8), writes=["qa"])
        Sc.dma("sp", ident[:], T["c_ident"], writes=["ident"])
        Sc.dma("sp", negtri[:], T["c_negtri"], writes=["negtri"])
        Sc.dma("pool", wuv[:], T["e_w_uv"], writes=["wuv"])
        mx, mn, w0, lo, mid, cnt, tg = [stt[:, i:i + 1] for i in range(7)]

        def load_idx_block(blk):
            t0 = blk * 512
            qav = T["s_qaT"].bitcast(F32R)[:, t0:t0 + 512].rearrange("(j r d) t -> r d j t", r=2, d=64)
            Sc.dma("pool", qa[0:64, :, 0, :], qav[0], writes=["qa"])
            Sc.dma("pool", qa[64:128, :, 1, :], qav[1], writes=["qa"])
            Sc.dma("sp", sg[:], T["s_sgn"][t0:t0 + 512, :].rearrange("(a p) h -> p a h", p=128), writes=["sg"])

        def load_att_block(blk):
            t0 = blk * 512
            for a_ in range(4):
                Sc.dma("pool", ql[:, a_, :, :],
                       T["s_qlatT"].bitcast(F32R)[:, t0 + a_ * 128:t0 + (a_ + 1) * 128].rearrange(
                           "(h c) t -> c h t", c=128), writes=["ql"])

        def indexer(i):
            a = i % 4
            nk = i + 1
            n = nk * 128
            tsl = slice(a * 128, (a + 1) * 128)
            for h in range(16):
                Sc.V("tensor_scalar", dg[:, h, :], ident[:], sg[:, a, h:h + 1], None, ALU.mult,
                     reads=["ident", "sg"], writes=[("dg", h)])
            for kb in range((nk + 3) // 4):
                w = min(512, n - kb * 512)
                ks = slice(kb * 512, kb * 512 + w)
                sacc, sk = ps[3], ("ps", 3)
                steps = []
                for h in range(16):
                    def fr(h=h, w=w, ks=ks):
                        p, pk = ps[h % 3], ("ps", h % 3)
                        Sc.mm(p[:, :w], qa[:, h // 2, h % 2, tsl], kidx2[:, ks], reads=["qa", "kidx2"], writes=[pk])
                        tm, tk = tmps.next()
                        if h % 2 == 0:
                            Sc.act(tm[:, :w], p[:, :w], AF.Relu, reads=[pk], writes=[tk])
                        else:
                            Sc.V("tensor_scalar", tm[:, :w], p[:, :w], 0.0, None, ALU.max, reads=[pk], writes=[tk])
                        return tm, tk

                    def bk(cx, h=h, w=w, sacc=sacc, sk=sk):
                        tm, tk = cx
                        Sc.mm(sacc[:, :w], dg[:, h, :], tm[:, :w], start=(h == 0), stop=(h == 15),
                              reads=[("dg", h), tk], writes=[sk])
                    steps.append((fr, bk))
                pipeline(steps)
                Sc.act(acc[:, ks], sacc[:, :w], AF.Copy, reads=[sk], writes=["acc"])
            if i >= 2:
                Sc.V("tensor_reduce", mx, acc[:, :n], AX.X, ALU.max, reads=["acc"], writes=["mx"])
                Sc.V("tensor_reduce", mn, acc[:, :n], AX.X, ALU.min, reads=["acc"], writes=["mn"])
                Sc.V("tensor_tensor", w0, mx, mn, ALU.subtract, reads=["mx", "mn"], writes=["w0"])
                Sc.V("tensor_copy", lo, mn, reads=["mn"], writes=["lo"])
            else:
                Sc.V("memset", lo, -1.0e29, writes=["lo"])
            Sc.V("tensor_tensor", acc[:, i * 128:(i + 1) * 128], acc[:, i * 128:(i + 1) * 128], negtri[:], ALU.add,
                 reads=["acc", "negtri"], writes=["acc"])

        def bisect_ops(i):
            n = (i + 1) * 128
            ops = []
            if i < 2:
                return ops
            for it in range(NIT):
                ck = 2.0 ** -(it + 1)
                ops.append(lambda ck=ck: Sc.V("scalar_tensor_tensor", mid, w0, ck, lo, ALU.mult, ALU.add,
                                              reads=["w0", "lo"], writes=["mid"]))
                ops.append(lambda n=n: Sc.V("tensor_scalar", Mm[:, :n], acc[:, :n], mid, None, ALU.is_ge, ALU.add,
                                            accum_out=cnt, reads=["acc", "mid"], writes=["Mm", "cnt"]))
                ops.append(lambda ck=ck: Sc.V("tensor_scalar", tg, cnt, 255.5, ck, ALU.is_ge, ALU.mult,
                                              reads=["cnt"], writes=["tg"]))
                ops.append(lambda: Sc.V("scalar_tensor_tensor", lo, tg, w0, lo, ALU.mult, ALU.add,
                                        reads=["tg", "w0", "lo"], writes=["lo"]))
            return ops

        def mask_op(i):
            n = (i + 1) * 128
            Sc.V("tensor_scalar", Mm[:, :n], acc[:, :n], lo, None, ALU.is_ge, reads=["acc", "lo"], writes=["Mm"])

        def transposes(i):
            nk = i + 1
            for j0 in range(0, nk, 4):
                g = min(4, nk - j0)
                p, pk = ps[4 + (j0 // 4) % 2], ("ps", 4 + (j0 // 4) % 2)
                for jj in range(g):
                    j = j0 + jj
                    Sc.tr(p[:, jj * 128:(jj + 1) * 128], Mm[:, j * 128:(j + 1) * 128], ident[:],
                          reads=["Mm", "ident"], writes=[pk])
                Sc.act(MT[:, j0:j0 + g, :], p[:, :g * 128].rearrange("p (g t) -> p g t", g=g), AF.Copy,
                       reads=[pk], writes=[("MT", j0 // 4)])

        def attention(i, extra):
            a = i % 4
            nk = i + 1
            tsl = slice(a * 128, (a + 1) * 128)
            nsteps = 2 * nk
            per = -(-len(extra) // nsteps) if extra else 0
            steps = []
            for j in range(nk):
                near = (j >= i - 1)
                for half in range(2):
                    def fr(j=j, half=half, near=near):
                        if near and half == 0:
                            E = Ed if j == i else Eo
                            ek = "B_Ed" if j == i else "B_Eo"
                            Sc.V("tensor_tensor", MTh[:], E[:], MT[:, j:j + 1, :].to_broadcast([128, 8, 128]),
                                 ALU.mult, reads=[ek, ("MT", j // 4)], writes=["MTh"])
                        sb_ = (2 * j + half) % 3
                        st_, stk = ps[sb_], ("ps", sb_)
                        Sc.mm(st_[:], ckvT[:, j * 128:(j + 1) * 128], ql[:, a, 4 * half:4 * half + 4, :],
                              reads=["ckvT", "ql"], writes=[stk])
                        pt, ptk = pts.next()
                        Sc.act(pt[:], st_[:], AF.Exp, reads=[stk], writes=[ptk])
                        pt2, pt2k = pt2s.next()
                        if near:
                            Sc.V("tensor_tensor", pt2[:].rearrange("p (h t) -> p h t", h=4),
                                 pt[:].rearrange("p (h t) -> p h t", h=4), MTh[:, 4 * half:4 * half + 4, :],
                                 ALU.mult, reads=[ptk, "MTh"], writes=[pt2k])
                        else:
                            Sc.V("tensor_tensor", pt2[:].rearrange("p (h t) -> p h t", h=4),
                                 pt[:].rearrange("p (h t) -> p h t", h=4),
                                 MT[:, j:j + 1, :].to_broadcast([128, 4, 128]), ALU.mult,
                                 reads=[ptk, ("MT", j // 4)], writes=[pt2k])
                        for _ in range(per):
                            if extra:
                                extra.pop(0)()
                        return pt2, pt2k

                    def bk(cx, j=j, half=half):
                        pt2, pt2k = cx
                        Sc.mm(ps[4 + half][:], ckv[:, j, :], pt2[:], start=(j == 0), stop=(j == nk - 1),
                              reads=["ckv", pt2k], writes=[("ps", 4 + half)])
                        Sc.mm(ps[6 + half][:], onesr[:], pt2[:], start=(j == 0), stop=(j == nk - 1),
                              reads=["onesr", pt2k], writes=[("ps", 6 + half)])
                    steps.append((fr, bk))
            pipeline(steps)
            while extra:
                extra.pop(0)()
            for half in range(2):
                Sc.act(rec[:, half, :], ps[6 + half][:], AF.Ln, reads=[("ps", 6 + half)], writes=[("rec", half)])
                Sc.act(rec[:, half, :], rec[:, half, :], AF.Exp, reads=[("rec", half)], writes=[("rec", half)],
                       scale=-1.0)
                Sc.V("tensor_tensor", olat[:, 4 * half:4 * half + 4, tsl],
                     ps[4 + half][:].rearrange("p (h t) -> p h t", h=4),
                     rec[:, half, :].rearrange("p (h t) -> p h t", h=4), ALU.mult,
                     reads=[("ps", 4 + half), ("rec", half)], writes=["olat"])

        def block_end(blk):
            t0 = blk * 512
            for h in range(8):
                p, pk = ps[h % 3], ("ps", h % 3)
                Sc.mm(p[0:64, :], wuv[:, h * 64:(h + 1) * 64], olat[:, h, :], reads=["wuv", "olat"], writes=[pk])
                o_, ok = ost.next()
                Sc.act(o_[:], p[0:64, :], AF.Copy, reads=[pk], writes=[ok])
                Sc.dma("sp", T["s_oT"][h * 64:(h + 1) * 64, t0:t0 + 512], o_[:], reads=[ok], writes=["s_oT"])

        load_idx_block(0)
        indexer(0)
        mask_op(0)
        indexer(1)
        transposes(0)
        for i in range(NT):
            if i % 4 == 0:
                load_att_block(i // 4)
            nxt = i + 1
            ops = bisect_ops(nxt) if nxt < NT else []
            attention(i, ops)
            if nxt < NT:
                mask_op(nxt)
                if nxt + 1 < NT:
                    if (nxt + 1) % 4 == 0:
                        load_idx_block((nxt + 1) // 4)
                    indexer(nxt + 1)
                transposes(nxt)
            if i % 4 == 3:
                block_end(i // 4)
        Sc.emit()


def make_feeds(inputs, batches, phases=None):
    consts = host_consts()
    consts.update(host_gathers(np.asarray(inputs["rel_bias"], np.float32)))
    shared = dict(consts)
    sh = {
        "e_w_in": inputs["e_w_in"][0], "e_q_norm": inputs["e_q_norm"][0], "e_kv_norm": inputs["e_kv_norm"][0],
        "e_w_uq": inputs["e_w_uq"][0].reshape(256, 512), "e_w_uk": inputs["e_w_uk"][0].reshape(128, 512),
        "e_w_uv": inputs["e_w_uv"][0].reshape(128, 512), "e_w_qidx": inputs["e_w_qidx"][0].reshape(256, 1024),
        "o_routerT": inputs["o_router"][0].T.reshape(-1),
        "e_ck1": inputs["e_ck1"][0], "e_cv1": inputs["e_cv1"][0], "e_ck2": inputs["e_ck2"][0],
        "e_cv2": inputs["e_cv2"][0], "e_pos_kT": inputs["e_pos_k"][0].T, "e_pos_vT": inputs["e_pos_v"][0].T,
    }
    for k in ("e_w_out", "e_ln1_g", "e_ln1_b", "e_ffn_w1", "e_ffn_w3", "e_ffn_w2", "e_ln2_g", "e_ln2_b", "o_w_in",
              "o_w_out", "o_ln1_g", "o_ln1_b", "o_moe_w1", "o_moe_w3", "o_moe_w2", "o_ln2_g", "o_ln2_b"):
        sh[k] = inputs[k][0]
    shared.update(sh)
    used = needed_inputs(phases) if phases is not None else set(IN_SPECS)
    shared = {k: np.ascontiguousarray(v, dtype=(ml_dtypes.bfloat16 if IN_SPECS[k][1] == BF16 else np.float32))
              for k, v in shared.items() if k in IN_SPECS and k in used}
    feeds = []
    for b in batches:
        f = dict(shared)
        if "x" in used:
            f["x"] = np.ascontiguousarray(inputs["x"][b], dtype=np.float32)
        feeds.append(f)
    return feeds


def layer_norm_tile(C, z, zk, out, outk, g_bc, b_bc, st, stk, junk, junkk):
    Sc = C.S
    mean, var = st[:, 0:1], st[:, 1:2]
    Sc.V("tensor_reduce", mean, z, AX.X, ALU.add, reads=[zk], writes=[stk])
    Sc.V("tensor_scalar", mean, mean, 1.0 / D, None, ALU.mult, reads=[stk], writes=[stk])
    Sc.V("tensor_scalar", z, z, mean, None, ALU.subtract, reads=[zk, stk], writes=[zk])
    Sc.act(junk, z, AF.Square, reads=[zk], writes=[junkk, stk], accum_out=var)
    Sc.act(var, var, AF.Sqrt, reads=[stk], writes=[stk], scale=1.0 / D, bias=C.eps5[:])
    Sc.V("reciprocal", var, var, reads=[stk], writes=[stk])
    Sc.V("scalar_tensor_tensor", out, z, var, g_bc, ALU.mult, ALU.mult, reads=[zk, stk, "ln_g"], writes=[outk])
    Sc.V("tensor_tensor", out, out, b_bc, ALU.add, reads=[outk, "ln_b"], writes=[outk])


def phase_outproj(C, tag, oT_src, wout, x_src, ln_g, ln_b, h_dst, hT_dst, router=None, gate_dst=None,
                  su_src=None):
    nc, Sc, T = C.nc, C.S, C.T
    ps = C.ps
    with ExitStack() as es:
        def sb(name, shape, dt=F32):
            return es.enter_context(nc.sbuf_tensor(tag + name, shape, dt))

        W = sb("W", [128, 8, 1024], F32R)
        ident = sb("ident", [128, 128])
        g_bc = sb("g_bc", [128, 1024])
        b_bc = sb("b_bc", [128, 1024])
        oT = [sb("oT%d" % i, [128, 8, 512], F32R) for i in range(2)]
        xt = [sb("xt%d" % i, [128, 1024]) for i in range(2)]
        z = [sb("z%d" % i, [128, 1024]) for i in range(2)]
        hh = [sb("h%d" % i, [128, 1024]) for i in range(2)]
        hT = [sb("hT%d" % i, [128, 8, 128]) for i in range(2)]
        junk = sb("junk", [128, 1024])
        st = sb("st", [128, 16])
        if su_src is not None:
            o32 = sb("o32", [128, 8, 512])
            su = sb("su", [128, 8, 512])
        if router is not None:
            rT = sb("rT", [128, 8, 8])
            lg = sb("lg", [128, 8])
            gt = sb("gt", [128, 8])
            tmp8 = sb("tmp8", [128, 8])
        for k in range(8):
            Sc.dma("pool", W[:, k, :], wout[k * 128:(k + 1) * 128, :], writes=[("W", k)])
        Sc.dma("sp", ident[:], T["c_ident"], writes=["ident"])
        Sc.dma("sp", g_bc[:], bc_rows(ln_g, 128, 1024), writes=["ln_g"])
        Sc.dma("sp", b_bc[:], bc_rows(ln_b, 128, 1024), writes=["ln_b"])
        if router is not None:
            rview = router.rearrange("(e k p) -> k p e", e=8, k=8)
            for k in range(8):
                Sc.dma("sp", rT[:, k, :], rview[k], writes=["rT"], allow_slow_non_contiguous=True)
        for blk in range(S // 512):
            t0 = blk * 512
            o_ = oT[blk % 2]
            ok = ("oT", blk % 2)
            if su_src is None:
                Sc.dma("pool", o_[:], oT_src.bitcast(F32R)[:, t0:t0 + 512].rearrange("(k p) t -> p k t", p=128),
                       writes=[ok])
            else:
                Sc.dma("sp", o32[:], oT_src[:, t0:t0 + 512].rearrange("(k p) t -> p k t", p=128), writes=["o32"])
                Sc.dma("sp", su[:], su_src[:, t0:t0 + 512].rearrange("(k p) t -> p k t", p=128), writes=["su"])
                Sc.act(su[:], su[:], AF.Ln, reads=["su"], writes=["su"])
                Sc.act(su[:], su[:], AF.Exp, reads=["su"], writes=["su"], scale=-1.0)
                Sc.V("tensor_tensor", o_[:], o32[:], su[:], ALU.mult, reads=["o32", "su"], writes=[ok])
            for a in range(4):
                ti = blk * 4 + a
                r0 = ti * 128
                b2 = ti % 2
                xk, zk, hk, hTk = ("xt", b2), ("z", b2), ("h", b2), ("hT", b2)
                Sc.dma("sp", xt[b2][:], x_src[r0:r0 + 128, :], writes=[xk])
                for half in range(2):
                    p, pk = ps[(2 * ti + half) % 4], ("ps", (2 * ti + half) % 4)
                    for k in range(8):
                        Sc.mm(p[:], o_[:, k, a * 128:(a + 1) * 128], W[:, k, half * 512:(half + 1) * 512],
                              start=(k == 0), stop=(k == 7), reads=[ok, ("W", k)], writes=[pk])
                    Sc.V("scalar_tensor_tensor", z[b2][:, half * 512:(half + 1) * 512],
                         xt[b2][:, half * 512:(half + 1) * 512], float(ALPHA), p[:], ALU.mult, ALU.add,
                         reads=[xk, pk], writes=[zk])
                stt = st[:, 2 * b2:2 * b2 + 2]
                layer_norm_tile(C, z[b2][:], zk, hh[b2][:], hk, g_bc[:], b_bc[:], stt, ("st", b2), junk[:], "junk")
                Sc.dma("sp", h_dst[r0:r0 + 128, :], hh[b2][:], reads=[hk], writes=["h_dst"])
                for g4 in range(2):
                    p, pk = ps[4 + (2 * ti + g4) % 4], ("ps", 4 + (2 * ti + g4) % 4)
                    for kk in range(4):
                        k = g4 * 4 + kk
                        Sc.tr(p[:, kk * 128:(kk + 1) * 128], hh[b2][:, k * 128:(k + 1) * 128], ident[:],
                              reads=[hk, "ident"], writes=[pk])
                    Sc.act(hT[b2][:, g4 * 4:g4 * 4 + 4, :], p[:].rearrange("p (k t) -> p k t", k=4), AF.Copy,
                           reads=[pk], writes=[hTk])
                Sc.dma("sp", hT_dst[:, r0:r0 + 128].rearrange("(k p) t -> p k t", p=128), hT[b2][:], reads=[hTk],
                       writes=["hT_dst"])
                if router is not None:
                    pl, plk = ps[(2 * ti) % 4], ("ps", (2 * ti) % 4)
                    for k in range(8):
                        Sc.mm(pl[:, 0:8], hT[b2][:, k, :], rT[:, k, :], start=(k == 0), stop=(k == 7),
                              reads=[hTk, "rT"], writes=[plk])
                    Sc.V("tensor_copy", lg[:], pl[:, 0:8], reads=[plk], writes=["lg"])
                    m1, m2, dd, g1, g2 = [st[:, 8 + q:9 + q] for q in range(5)]
                    Sc.V("tensor_reduce", m1, lg[:], AX.X, ALU.max, reads=["lg"], writes=["m1"])
                    Sc.V("tensor_scalar", tmp8[:], lg[:], m1, None, ALU.is_ge, reads=["lg", "m1"], writes=["tmp8"])
                    Sc.V("scalar_tensor_tensor", gt[:], tmp8[:], -1.0e30, lg[:], ALU.mult, ALU.add,
                         reads=["tmp8", "lg"], writes=["gt"])
                    Sc.V("tensor_reduce", m2, gt[:], AX.X, ALU.max, reads=["gt"], writes=["m2"])
                    Sc.V("tensor_tensor", dd, m2, m1, ALU.subtract, reads=["m1", "m2"], writes=["dd"])
                    Sc.act(g2, dd, AF.Exp, reads=["dd"], writes=["g2"])
                    Sc.V("tensor_scalar", g1, g2, 1.0, None, ALU.add, reads=["g2"], writes=["g1"])
                    Sc.V("reciprocal", g1, g1, reads=["g1"], writes=["g1"])
                    Sc.V("tensor_tensor", g2, g2, g1, ALU.mult, reads=["g1", "g2"], writes=["g2"])
                    Sc.V("tensor_scalar", tmp8[:], tmp8[:], g1, None, ALU.mult, reads=["tmp8", "g1"], writes=["tmp8"])
                    Sc.V("tensor_scalar", gt[:], gt[:], m2, g2, ALU.is_ge, ALU.mult, reads=["gt", "m2", "g2"],
                         writes=["gt"])
                    Sc.V("tensor_tensor", gt[:], gt[:], tmp8[:], ALU.add, reads=["gt", "tmp8"], writes=["gt"])
                    Sc.dma("sp", gate_dst[r0:r0 + 128, :], gt[:], reads=["gt"], writes=["gate_dst"])
        Sc.emit()


def phase_ffn(C, tag, hT_src, h_src, w1s, w3s, w2s, dff, gate_src, ln_g, ln_b, out_dst):
    nc, Sc, T = C.nc, C.S, C.T
    ps = C.ps
    nexp = len(w1s)
    ngrp = dff // 256
    with ExitStack() as es:
        def sb(name, shape, dt=F32):
            return es.enter_context(nc.sbuf_tensor(tag + name, shape, dt))

        hT = sb("hT", [128, 8, 1024], F32R)
        yaccs = [sb("yacc%d" % i, [128, 8, 1024]) for i in range(2)]
        pending_ln = []
        w1g = [sb("w1g%d" % i, [128, 8, 256], F32R) for i in range(2)]
        w3g = [sb("w3g%d" % i, [128, 8, 256], F32R) for i in range(2)]
        w2g = [sb("w2g%d" % i, [128, 2, 1024], F32R) for i in range(2)]
        hc = [sb("hc%d" % i, [128, 2, 1024], F32R) for i in range(2)]
        s1 = [sb("s1%d" % i, [128, 512]) for i in range(2)]
        g_bc = sb("g_bc", [128, 1024])
        b_bc = sb("b_bc", [128, 1024])
        gates = sb("gates", [128, 8, 8])
        ht = [sb("ht%d" % i, [128, 1024]) for i in range(2)]
        oo = [sb("oo%d" % i, [128, 1024]) for i in range(2)]
        junk = sb("junk", [128, 1024])
        st = sb("st", [128, 8])
        Sc.dma("sp", g_bc[:], bc_rows(ln_g, 128, 1024), writes=["ln_g"])
        Sc.dma("sp", b_bc[:], bc_rows(ln_b, 128, 1024), writes=["ln_b"])
        gi = 0
        for tb in range(S // 1024):
            t0 = tb * 1024
            yacc = yaccs[tb % 2]
            yb = tb % 2
            Sc.dma("pool", hT[:], hT_src.bitcast(F32R)[:, t0:t0 + 1024].rearrange("(k p) t -> p k t", p=128),
                   writes=["hT"])
            if gate_src is not None:
                Sc.dma("sp", gates[:], gate_src[t0:t0 + 1024, :].rearrange("(a p) e -> p a e", p=128), writes=["gates"])
            for e_ in range(nexp):
                for grp in range(ngrp):
                    b2 = gi % 2
                    gi += 1
                    c0 = grp * 256
                    Sc.dma("pool", w1g[b2][:], w1s[e_][:, c0:c0 + 256].rearrange("(k p) f -> p k f", p=128),
                           writes=[("w1g", b2)])
                    Sc.dma("pool", w3g[b2][:], w3s[e_][:, c0:c0 + 256].rearrange("(k p) f -> p k f", p=128),
                           writes=[("w3g", b2)])
                    Sc.dma("pool", w2g[b2][:], w2s[e_][c0:c0 + 256, :].rearrange("(c p) d -> p c d", p=128),
                           writes=[("w2g", b2)])
                    hcb = hc[b2]
                    hck = ("hc", b2)
                    u = 0
                    for c in range(2):
                        for th in range(2):
                            p1, p1k = ps[u % 2], ("ps", u % 2)
                            p3, p3k = ps[2 + u % 2], ("ps", 2 + u % 2)
                            u += 1
                            for k in range(8):
                                Sc.mm(p1[:], w1g[b2][:, k, c * 128:(c + 1) * 128], hT[:, k, th * 512:(th + 1) * 512],
                                      start=(k == 0), stop=(k == 7), reads=[("w1g", b2), "hT"], writes=[p1k])
                            for k in range(8):
                                Sc.mm(p3[:], w3g[b2][:, k, c * 128:(c + 1) * 128], hT[:, k, th * 512:(th + 1) * 512],
                                      start=(k == 0), stop=(k == 7), reads=[("w3g", b2), "hT"], writes=[p3k])
                            sx, sxk = s1[u % 2], ("s1", u % 2)
                            Sc.act(sx[:], p1[:], AF.Silu, reads=[p1k], writes=[sxk])
                            Sc.V("tensor_tensor", hcb[:, c, th * 512:(th + 1) * 512], sx[:], p3[:], ALU.mult,
                                 reads=[sxk, p3k], writes=[hck])
                    first = (e_ == 0 and grp == 0)
                    v = 0
                    for tt in range(8):
                        for dh in range(2):
                            py, pyk = ps[4 + v % 4], ("ps", 4 + v % 4)
                            v += 1
                            for c in range(2):
                                Sc.mm(py[:], hcb[:, c, tt * 128:(tt + 1) * 128], w2g[b2][:, c, dh * 512:(dh + 1) * 512],
                                      start=(c == 0), stop=(c == 1), reads=[hck, ("w2g", b2)], writes=[pyk])
                            ya = yacc[:, tt, dh * 512:(dh + 1) * 512]
                            yk = ("yacc", yb, tt, dh)
                            if gate_src is None:
                                if first:
                                    Sc.V("tensor_copy", ya, py[:], reads=[pyk], writes=[yk])
                                else:
                                    Sc.V("tensor_tensor", ya, ya, py[:], ALU.add, reads=[pyk, yk], writes=[yk])
                            else:
                                gcol = gates[:, tt, e_:e_ + 1]
                                if first:
                                    Sc.V("tensor_scalar", ya, py[:], gcol, None, ALU.mult, reads=[pyk, "gates"],
                                         writes=[yk])
                                else:
                                    Sc.V("scalar_tensor_tensor", ya, py[:], gcol, ya, ALU.mult, ALU.add,
                                         reads=[pyk, "gates", yk], writes=[yk])
                    if pending_ln:
                        pending_ln.pop(0)()
            for tt in range(8):
                def _ln(tt=tt, t0=t0, yacc=yacc, yb=yb):
                    r0 = t0 + tt * 128
                    b2 = tt % 2
                    hk, ok = ("ht", b2), ("oo", b2)
                    Sc.dma("sp", ht[b2][:], h_src[r0:r0 + 128, :], writes=[hk])
                    Sc.V("scalar_tensor_tensor", ht[b2][:], ht[b2][:], float(ALPHA), yacc[:, tt, :], ALU.mult, ALU.add,
                         reads=[hk, ("yacc", yb, tt, 0), ("yacc", yb, tt, 1)], writes=[hk])
                    layer_norm_tile(C, ht[b2][:], hk, oo[b2][:], ok, g_bc[:], b_bc[:], st[:, 2 * b2:2 * b2 + 2],
                                    ("st", b2), junk[:], "junk")
                    Sc.dma("sp", out_dst[r0:r0 + 128, :], oo[b2][:], reads=[ok], writes=["out_dst"])
                pending_ln.append(_ln)
        while pending_ln:
            pending_ln.pop(0)()
        Sc.emit()


def phase_E(C):
    nc, Sc, T = C.nc, C.S, C.T
    ps = C.ps
    with ExitStack() as es:
        def sb(name, shape, dt=F32):
            return es.enter_context(nc.sbuf_tensor("E_" + name, shape, dt))

        Win = sb("Win", [128, 8, 3072], F32R)
        ident = sb("ident", [128, 128])
        xin = [sb("xin%d" % i, [128, 4, 1024]) for i in range(2)]
        xT = sb("xT", [128, 8, 512], F32R)
        stg = Rot([(sb("stg%d" % i, [128, 512]), ("stg", i)) for i in range(6)])
        psr = Rot([(ps[i], ("ps", i)) for i in range(8)])
        for k in range(8):
            Sc.dma("pool", Win[:, k, :], T["o_w_in"][k * 128:(k + 1) * 128, :], writes=[("Win", k)])
        Sc.dma("sp", ident[:], T["c_ident"], writes=["ident"])
        n = [0]

        def evac(out, in_, reads, writes, scale=None):
            n[0] += 1
            if n[0] % 2 == 0:
                Sc.act(out, in_, AF.Copy, reads, writes, scale=float(scale if scale is not None else 1.0))
            else:
                Sc.V("tensor_scalar", out, in_, float(scale if scale is not None else 1.0), None, ALU.mult,
                     reads=reads, writes=writes)

        for blk in range(S // 512):
            t0 = blk * 512
            xi, xk = xin[blk % 2], ("xin", blk % 2)
            Sc.dma("sp", xi[:], T["s_x1"][t0:t0 + 512, :].rearrange("(a p) d -> p a d", p=128), writes=[xk])
            for k in range(8):
                p, pk = psr.next()
                for a in range(4):
                    Sc.tr(p[:, a * 128:(a + 1) * 128], xi[:, a, k * 128:(k + 1) * 128], ident[:],
                          reads=[xk, "ident"], writes=[pk])
                evac(xT[:, k, :], p[:], [pk], [("xT", k)])
            for j in range(16):
                p, pk = psr.next()
                c0 = j * 128
                for k in range(8):
                    Sc.mm(p[:], Win[:, k, c0:c0 + 128], xT[:, k, :], start=(k == 0), stop=(k == 7),
                          reads=[("Win", k), ("xT", k)], writes=[pk])
                st, sk = stg.next()
                if j < 8:
                    evac(st[:], p[:], [pk], [sk], scale=0.125)
                    Sc.dma("sp", T["s_qT1"][c0:c0 + 128, t0:t0 + 512], st[:], reads=[sk], writes=["s_qT1"])
                else:
                    evac(st[:], p[:], [pk], [sk])
                    Sc.dma("sp", T["s_kT1"][c0 - 1024:c0 - 896, t0:t0 + 512], st[:], reads=[sk], writes=["s_kT1"])
            for a in range(4):
                r0 = t0 + a * 128
                for half in range(2):
                    p, pk = psr.next()
                    c0 = 2048 + half * 512
                    for k in range(8):
                        Sc.mm(p[:], xT[:, k, a * 128:(a + 1) * 128], Win[:, k, c0:c0 + 512], start=(k == 0),
                              stop=(k == 7), reads=[("Win", k), ("xT", k)], writes=[pk])
                    st, sk = stg.next()
                    evac(st[:], p[:], [pk], [sk])
                    Sc.dma("sp", T["s_v1"][r0:r0 + 128, half * 512:(half + 1) * 512], st[:], reads=[sk],
                           writes=["s_v1"])
        Sc.emit()


def phase_F(C):
    nc, Sc, T = C.nc, C.S, C.T
    ps = C.ps
    with ExitStack() as es:
        def sb(name, shape, dt=F32):
            return es.enter_context(nc.sbuf_tensor("F_" + name, shape, dt))

        qz = [sb("qz%d" % i, [128, S], F32R) for i in range(2)]
        kT = sb("kT", [128, S], F32R)
        vaug = sb("vaug", [128, NT, 2, 128], F32R)
        selT = sb("selT", [128, S], F32R)
        ident = sb("ident", [128, 128])
        onesr = sb("onesr", [128, 128], F32R)
        xm = sb("xm", [128, 16, 128], F32R)
        gm = sb("gm", [128, 32, 16])
        adm = sb("adm", [128, 32, 16])
        km = sb("km", [128, 16])
        g0 = sb("g0", [128, 32, 16])
        g1 = sb("g1", [128, 32, 16])
        eq = sb("eq", [128, 32, 16])
        mxs = sb("mxs", [128, 32])
        OE = sb("OE", [128, 4, 512])
        EP = sb("EP", [128, 5, 512])
        pts = Rot([(sb("pt%d" % i, [128, 512]), ("pt", i)) for i in range(4)])
        pt2s = Rot([(sb("pt2%d" % i, [128, 512], F32R), ("pt2", i)) for i in range(5)])
        mts = Rot([(sb("mt%d" % i, [128, 512]), ("mt", i)) for i in range(3)])
        ost = Rot([(sb("ost%d" % i, [128, 512]), ("ost", i)) for i in range(2)])
        Ed, Eo = make_E(C, es, 0, 16, "F_")
        Sc.dma("pool", qz[0][64:128, :], T["c_zeros"][64:128, :], writes=["qz"])
        Sc.dma("pool", qz[1][0:64, :], T["c_zeros"][0:64, :], writes=["qz"])
        Sc.dma("pool", selT[:], T["c_zeros"], writes=["selT"])
        for r_ in range(2):
            Sc.dma("pool", vaug[:, :, r_, 64:128],
                   bass.AP(T["c_ones"].tensor, T["c_ones"].offset, [[128, 128], [0, NT], [1, 64]]), writes=["vaug"])
        Sc.dma("sp", ident[:], T["c_ident"], writes=["ident"])
        Sc.dma("pool", onesr[:], T["c_ones"], writes=["onesr"])
        Sc.dma("pool", xm[:], T["c_xm"], writes=["xm"])
        Sc.dma("sp", gm[:], T["c_gm"], writes=["gm"])
        Sc.dma("sp", adm[:], T["c_adm"], writes=["adm"])
        Sc.V("memset", OE[:], 0.0, writes=["OE"])
        Sc.V("memset", EP[:], 1.0, writes=["EP"])
        for pair in range(8):
            r0 = pair * 128
            Sc.dma("pool", qz[0][0:64, :], T["s_qT1"].bitcast(F32R)[r0:r0 + 64, :], writes=["qz"])
            Sc.dma("pool", qz[1][64:128, :], T["s_qT1"].bitcast(F32R)[r0 + 64:r0 + 128, :], writes=["qz"])
            Sc.dma("pool", kT[:], T["s_kT1"].bitcast(F32R)[r0:r0 + 128, :], writes=["kT"])
            for r_ in range(2):
                Sc.dma("pool", vaug[:, :, r_, 0:64],
                       T["s_v1"].bitcast(F32R)[:, r0 + r_ * 64:r0 + (r_ + 1) * 64].rearrange("(j p) c -> p j c", p=128),
                       writes=["vaug"])
            Sc.V("tensor_reduce", km[:], r32(kT[:]).rearrange("p (b s) -> p b s", s=256), AX.X, ALU.add,
                 reads=["kT"], writes=["km"])
            Sc.V("tensor_scalar", km[:], km[:], 1.0 / 256, None, ALU.mult, reads=["km"], writes=["km"])
            for r in range(2):
                h = pair * 2 + r
                pb = slice(r * 64, (r + 1) * 64)
                for ti in range(NT):
                    Sc.mm(ps[0][:, ti * 16:(ti + 1) * 16], r32(qz[r][pb, ti * 128:(ti + 1) * 128]), km[pb, :],
                          reads=["qz", "km"], writes=[("ps", 0)])
                g0f, g1f, eqf = g0[:], g1[:], eq[:]
                Sc.V("tensor_tensor", g0f, ps[0][:].rearrange("p (a b) -> p a b", b=16), gm[:], ALU.add,
                     reads=[("ps", 0), "gm"], writes=["g0"])
                src, srck = g0, "g0"
                for rnd in range(3):
                    Sc.V("tensor_reduce", mxs[:], src[:], AX.X, ALU.max, reads=[srck], writes=["mxs"])
                    if rnd < 2:
                        Sc.V("tensor_tensor", eqf, src[:], mxs[:].unsqueeze(2).to_broadcast([128, 32, 16]), ALU.is_ge,
                             reads=[srck, "mxs"], writes=["eq"])
                        Sc.V("scalar_tensor_tensor", g1f, eqf, -3.0e30, src[:], ALU.mult, ALU.add,
                             reads=["eq", srck], writes=["g1"])
                        src, srck = g1, "g1"
                Sc.V("tensor_tensor", eqf, g0f, mxs[:].unsqueeze(2).to_broadcast([128, 32, 16]), ALU.is_ge,
                     reads=["g0", "mxs"], writes=["eq"])
                Sc.V("tensor_tensor", eqf, eqf, adm[:], ALU.mult, reads=["eq", "adm"], writes=["eq"])
                for q4 in range(8):
                    p, pk = ps[1 + q4 % 2], ("ps", 1 + q4 % 2)
                    for a in range(4):
                        ti = q4 * 4 + a
                        Sc.tr(p[0:16, a * 128:(a + 1) * 128], eq[:, ti, :], ident[:], reads=["eq", "ident"],
                              writes=[pk])
                    Sc.act(selT[0:16, q4 * 512:(q4 + 1) * 512], p[0:16, :], AF.Copy, reads=[pk], writes=["selT"])
                for rel in range(4):
                    for a in range(4):
                        if rel // 2 == a // 2 and a - rel in (0, 1):
                            src_ = Ed if a == rel else Eo
                            Sc.V("tensor_copy", OE[:, rel, a * 128:(a + 1) * 128], src_[:, h, :],
                                 reads=["F_Ed", "F_Eo"], writes=["OE"])
                for reli in range(5):
                    rel = reli - 1
                    a = rel + 1
                    if 0 <= a < 4:
                        Sc.V("tensor_copy", EP[:, reli, a * 128:(a + 1) * 128], Eo[:, h, :], reads=["F_Eo"],
                             writes=["EP"])
                for I in range(S // 512):
                    qs_ = slice(I * 512, (I + 1) * 512)
                    po, pok = ps[6 + I % 2], ("ps", 6 + I % 2)
                    nk = 4 * I + 4
                    steps = []
                    for j in range(nk):
                        def fr(j=j, I=I, qs_=qs_, r=r):
                            rel = j - 4 * I
                            pS, pSk = ps[j % 3], ("ps", j % 3)
                            pM, pMk = ps[3 + j % 3], ("ps", 3 + j % 3)
                            Sc.mm(pS[:], kT[:, j * 128:(j + 1) * 128], qz[r][:, qs_], reads=["kT", "qz"], writes=[pSk])
                            Sc.mm(pM[:], xm[:, j // 2, :], selT[:, qs_], reads=["xm", "selT"], writes=[pMk])
                            pt, ptk = pts.next()
                            Sc.act(pt[:], pS[:], AF.Exp, reads=[pSk], writes=[ptk])
                            pt2, pt2k = pt2s.next()
                            if rel < -1:
                                Sc.V("tensor_tensor", pt2[:], pt[:], pM[:], ALU.mult, reads=[ptk, pMk], writes=[pt2k])
                            else:
                                mt, mtk = mts.next()
                                Sc.V("tensor_tensor", mt[:], EP[:, rel + 1, :], pM[:], ALU.mult, reads=["EP", pMk],
                                     writes=[mtk])
                                if rel >= 0:
                                    Sc.V("tensor_tensor", mt[:], mt[:], OE[:, rel, :], ALU.add, reads=[mtk, "OE"],
                                         writes=[mtk])
                                Sc.V("tensor_tensor", pt2[:], pt[:], mt[:], ALU.mult, reads=[ptk, mtk],
                                     writes=[pt2k])
                            return pt2, pt2k

                        def bk(cx, j=j, nk=nk, po=po, pok=pok, r=r):
                            pt2, pt2k = cx
                            Sc.mm(po[:], vaug[:, j, r, :], pt2[:], start=(j == 0), stop=(j == nk - 1),
                                  reads=["vaug", pt2k], writes=[pok])
                        steps.append((fr, bk))
                    pipeline(steps)
                    o_, ok = ost.next()
                    Sc.act(o_[:], po[:], AF.Copy, reads=[pok], writes=[ok])
                    Sc.dma("sp", T["s_oT1"][h * 64:(h + 1) * 64, qs_], o_[0:64, :], reads=[ok], writes=["s_oT1"])
                    Sc.dma("sp", T["s_su1"][h * 64:(h + 1) * 64, qs_], o_[64:128, :], reads=[ok], writes=["s_su1"])
        Sc.emit()


def phase_C0(C):
    nc, Sc, T = C.nc, C.S, C.T
    ps = C.ps
    with ExitStack() as es:
        def sb(name, shape, dt=F32):
            return es.enter_context(nc.sbuf_tensor("C0_" + name, shape, dt))

        src = {"k": sb("kcr", [64, 2, S]), "v": sb("vcr", [64, 2, S])}
        w1 = {"k": sb("w1k", [64, 32, 64]), "v": sb("w1v", [64, 32, 64])}
        w2 = {"k": sb("w2k", [64, 64]), "v": sb("w2v", [64, 64])}
        posT = {"k": sb("posk", [64, 32]), "v": sb("posv", [64, 32])}
        kpl = Rot([(sb("kpl%d" % i, [64, 256]), ("kpl", i)) for i in range(3)])
        xs = sb("xs", [64, 256])
        u2 = sb("u2", [64, 256])
        hid = sb("hid", [64, 256])
        stg = Rot([(sb("stg%d" % i, [128, 256]), ("stg", i)) for i in range(2)])
        pb = sb("pb", [32, 8, 128])
        b31 = sb("b31", [32, 8])
        pm = sb("pm", [32, 128])
        one_t = sb("one_t", [128, 8, 128])
        zero_t = sb("zero_t", [128, 8, 128])
        Sc.dma("sp", src["k"][:], T["s_kcT"].rearrange("(g d) t -> d g t", d=64), writes=["kcr"])
        Sc.dma("sp", src["v"][:], T["s_vcT"].rearrange("(g d) t -> d g t", d=64), writes=["vcr"])
        Sc.dma("sp", w1["k"][:], T["e_ck1"].rearrange("l d e -> d l e"), writes=["w1k"])
        Sc.dma("sp", w1["v"][:], T["e_cv1"].rearrange("l d e -> d l e"), writes=["w1v"])
        Sc.dma("sp", w2["k"][:], T["e_ck2"], writes=["w2k"])
        Sc.dma("sp", w2["v"][:], T["e_cv2"], writes=["w2v"])
        Sc.dma("sp", posT["k"][:], T["e_pos_kT"], writes=["posk"])
        Sc.dma("sp", posT["v"][:], T["e_pos_vT"], writes=["posv"])
        Sc.dma("sp", pb[:], T["g_pb"], writes=["pb"])
        Sc.dma("sp", b31[:], T["g_b31"][0:32, 8:16], writes=["b31"])
        Sc.dma("sp", pm[:], T["c_pm"], writes=["pm"])
        Sc.V("tensor_tensor", pb[:], pb[:], b31[:].unsqueeze(2).to_broadcast([32, 8, 128]), ALU.subtract,
             reads=["pb", "b31"], writes=["pb"])
        Sc.act(pb[:], pb[:], AF.Exp, reads=["pb"], writes=["pb"])
        Sc.V("tensor_tensor", pb[:], pb[:], pm[:].unsqueeze(1).to_broadcast([32, 8, 128]), ALU.mult,
             reads=["pb", "pm"], writes=["pb"])
        Sc.V("memset", one_t[:], 1.0, writes=["one_t"])
        Sc.V("memset", zero_t[:], 0.0, writes=["zero_t"])
        Sc.dma("sp", T["s_G"][0:128], one_t[:], reads=["one_t"], writes=["s_G"])
        Sc.dma("sp", T["s_G"][128:256], one_t[:], reads=["one_t"], writes=["s_G"])
        Sc.dma("sp", T["s_G"][256:288], pb[:], reads=["pb"], writes=["s_G"])
        Sc.dma("sp", T["s_G"][288:416], zero_t[:], reads=["zero_t"], writes=["s_G"])
        Sc.dma("sp", T["s_G"][416:528], zero_t[0:112], reads=["zero_t"], writes=["s_G"])
        Sc.V("memset", hid[:], 0.0, writes=["hid"])
        for which in ("k", "v"):
            for g in range(2):
                ph, phk = ps[g], ("ps", g)
                for l in range(32):
                    kp, kpk = kpl.next()
                    Sc.V("tensor_scalar", kp[:, 0:255], src[which][:, g, l:l + 16 * 254 + 1:16],
                         posT[which][:, l:l + 1], None, ALU.add, reads=[which + "cr", "pos" + which], writes=[kpk])
                    Sc.mm(ph[0:64, 0:255], w1[which][:, l, :], kp[:, 0:255], start=(l == 0), stop=(l == 31),
                          reads=["w1" + which, kpk], writes=[phk])
                Sc.act(xs[:, 0:255], ph[0:64, 0:255], AF.Copy, reads=[phk], writes=["xs"])
                Sc.V("tensor_tensor", u2[:, 0:255], xs[:, 0:255], xs[:, 0:255], ALU.mult, reads=["xs"], writes=["u2"])
                Sc.V("tensor_scalar", u2[:, 0:255], u2[:, 0:255], 0.044715, 1.0, ALU.mult, ALU.add, reads=["u2"],
                     writes=["u2"])
                Sc.V("tensor_tensor", u2[:, 0:255], u2[:, 0:255], xs[:, 0:255], ALU.mult, reads=["u2", "xs"],
                     writes=["u2"])
                Sc.act(u2[:, 0:255], u2[:, 0:255], AF.Sigmoid, reads=["u2"], writes=["u2"], scale=1.5957691216057308)
                Sc.V("tensor_tensor", hid[:, 0:255], xs[:, 0:255], u2[:, 0:255], ALU.mult, reads=["u2", "xs"],
                     writes=["hid"])
                if which == "k":
                    p2, p2k = ps[2 + g], ("ps", 2 + g)
                    Sc.mm(p2[0:64, 0:256], w2["k"][:], hid[:], reads=["w2k", "hid"], writes=[p2k])
                    st, sk = stg.next()
                    Sc.act(st[0:64, :], p2[0:64, 0:256], AF.Copy, reads=[p2k], writes=[sk])
                    Sc.dma("sp", T["s_kcc"][:, g, :], st[0:64, :], reads=[sk], writes=["s_kcc"])
                else:
                    for nt in range(2):
                        p2, p2k = ps[4 + nt], ("ps", 4 + nt)
                        Sc.mm(p2[:, 0:64], hid[:, nt * 128:(nt + 1) * 128], w2["v"][:], reads=["w2v", "hid"],
                              writes=[p2k])
                        st, sk = stg.next()
                        Sc.act(st[:, 0:64], p2[:, 0:64], AF.Copy, reads=[p2k], writes=[sk])
                        Sc.dma("sp", T["s_vcc"][nt * 128:(nt + 1) * 128, g, :], st[:, 0:64], reads=[sk],
                               writes=["s_vcc"])
        Sc.emit()


def phase_C(C):
    phase_C0(C)
    nc, Sc, T = C.nc, C.S, C.T
    ps = C.ps
    with ExitStack() as es:
        def sb(name, shape, dt=F32):
            return es.enter_context(nc.sbuf_tensor("C_" + name, shape, dt))

        ksT = sb("ksT", [128, S], F32R)
        kwT = sb("kwT", [128, S], F32R)
        vs = sb("vs", [128, NT, 2, 128], F32R)
        vw = sb("vw", [128, NT, 2, 128], F32R)
        kcT = sb("kcT", [128, 256], F32R)
        vc = sb("vc", [128, 2, 2, 64], F32R)
        qb = sb("qb", [128, 4, 8, 128], F32R)
        shiftM = sb("shiftM", [128, 64], F32R)
        poS = sb("poS", [128, 512], F32R)
        gtss = [sb("gts%d" % i, [24, 512]) for i in range(2)]
        xs = sb("xs", [64, 32, 128], BF16)
        ovl = sb("ovl", [128, 2, 64])
        w4 = sb("w4", [128, 128])
        gmask = sb("gmask", [24, 6, 4])
        ident = sb("ident", [128, 128])
        onesr = sb("onesr", [128, 128], F32R)
        fc = sb("fc", [128, 64])
        ac = sb("ac", [128, 64])
        Ft = [sb("Ft%d" % i, [128, 4, 128]) for i in range(2)]
        pts = Rot([(sb("pt%d" % i, [128, 512]), ("pt", i)) for i in range(3)])
        pt2s = Rot([(sb("pt2%d" % i, [128, 512], F32R), ("pt2", i)) for i in range(3)])
        pt2c = [sb("pt2c%d" % i, [128, 512], F32R) for i in range(2)]
        MTh = sb("MTh", [128, 4, 128])
        rcc = sb("rcc", [128, 512])
        pn = sb("pn", [128, 512])
        psT = sb("psT", [128, 2, 128])
        sc1 = sb("sc1", [128, 64])
        sc2 = sb("sc2", [128, 64])
        m8a = sb("m8a", [128, 8])
        m8b = sb("m8b", [128, 8])
        sel = sb("sel", [128, 64])
        selT = sb("selT", [64, 128], BF16)
        obs = [[sb("ob%d_%d" % (u_, i), [64, 512]) for i in range(3)] for u_ in range(2)]
        rcb = sb("rcb", [64, 512])
        Rbr = sb("Rbr", [24, 512], F32R)
        og = [sb("og%d" % i, [64, 512]) for i in range(2)]
        tmpc = sb("tmpc", [64, 512])
        Ed, Eo = make_E(C, es, 8, 8, "C_")

        Sc.dma("pool", ksT[:], T["s_ksT"].bitcast(F32R), writes=["ksT"])
        Sc.dma("pool", kwT[:], T["s_kwT"].bitcast(F32R), writes=["kwT"])
        ones_src = bass.AP(T["c_ones"].tensor, T["c_ones"].offset, [[128, 128], [0, NT], [1, 64]])
        for g_ in range(2):
            Sc.dma("pool", vs[:, :, g_, 0:64],
                   T["s_vs"].bitcast(F32R)[:, g_ * 64:(g_ + 1) * 64].rearrange("(j p) c -> p j c", p=128), writes=["vs"])
            Sc.dma("pool", vw[:, :, g_, 0:64],
                   T["s_vw"].bitcast(F32R)[:, g_ * 64:(g_ + 1) * 64].rearrange("(j p) c -> p j c", p=128), writes=["vw"])
            Sc.dma("pool", vs[:, :, g_, 64:128], ones_src, writes=["vs"])
            Sc.dma("pool", vw[:, :, g_, 64:128], ones_src, writes=["vw"])
            Sc.dma("pool", kcT[g_ * 64:(g_ + 1) * 64, :], T["s_kcc"].bitcast(F32R)[:, g_, :], writes=["kcT"])
        zsrc = T["c_zeros"][:, 0:2048].rearrange("p (a h t) -> p a h t", a=4, h=4)
        Sc.dma("pool", qb[64:128, :, 0:4, :], zsrc[64:128], writes=["qb"])
        Sc.dma("pool", qb[0:64, :, 4:8, :], zsrc[0:64], writes=["qb"])
        Sc.dma("pool", shiftM[:], T["c_shift"], writes=["shiftM"])
        Sc.dma("pool", vc[:], T["s_vcc"].bitcast(F32R).rearrange("(a p) g f -> p a g f", p=128), writes=["vc"])
        Sc.dma("sp", xs[:], T["c_xs"], writes=["xs"])
        Sc.dma("sp", ovl[:], T["c_ovl"], writes=["ovl"])
        Sc.dma("sp", w4[:], T["c_w4"], writes=["w4"])
        Sc.dma("sp", gmask[:], T["c_gmask"], writes=["gmask"])
        Sc.dma("sp", ident[:], T["c_ident"], writes=["ident"])
        Sc.dma("pool", onesr[:], T["c_ones"], writes=["onesr"])
        po, pok = ps[3], ("ps", 3)
        pu, puk = ps[4], ("ps", 4)
        scnt = [0]

        def nextS():
            scnt[0] += 1
            return ps[scnt[0] % 3], ("ps", scnt[0] % 3)

        def shift_sums():
            Sc.act(poS[:], po[:], AF.Copy, reads=[pok], writes=["poS"])
            Sc.mm(pu[0:64, :], shiftM[:], poS[:], reads=["shiftM", "poS"], writes=[puk])

        def finish_branch(br):
            if br == 0:
                Sc.V("tensor_tensor", ob[br][:], po[0:64, :], rcc[0:64, :], ALU.mult, reads=[pok, "rcc"],
                     writes=[(obk, br)])
                return
            Sc.act(rcb[:], pu[0:64, :], AF.Ln, reads=[puk], writes=["rcb"])
            Sc.act(rcb[:], rcb[:], AF.Exp, reads=["rcb"], writes=["rcb"], scale=-1.0)
            Sc.V("tensor_tensor", ob[br][:], po[0:64, :], rcb[:], ALU.mult, reads=[pok, "rcb"], writes=[(obk, br)])

        ucnt = [0]
        pending = [None]
        for blk in range(S // 512):
            t0 = blk * 512
            for a_ in range(4):
                qsrc = T["s_qbT"].bitcast(F32R)[:, t0 + a_ * 128:t0 + (a_ + 1) * 128].rearrange("(h d) t -> d h t", d=64)
                Sc.dma("pool", qb[0:64, a_, 0:4, :], qsrc[:, 0:4, :], writes=["qb"])
                Sc.dma("pool", qb[64:128, a_, 4:8, :], qsrc[:, 4:8, :], writes=["qb"])
            gts = gtss[blk % 2]
            gtk = ("gts", blk % 2)
            Sc.dma("sp", gts[:], T["s_gatesT"][:, t0:t0 + 512], writes=[gtk])
            for a in range(4):
                i = blk * 4 + a
                tsl = slice(a * 128, (a + 1) * 128)
                Sc.dma("sp", fc[:], T["c_fc"][:, i, :], writes=["fc"])
                Sc.dma("sp", ac[:], T["c_ac"][:, i, :], writes=["ac"])
                for g in range(2):
                    ucnt[0] += 1
                    ob = obs[ucnt[0] % 2]
                    obk = ("ob", ucnt[0] % 2)
                    hs = slice(4 * g, 4 * g + 4)
                    qrhs = qb[:, a, hs, :]
                    nts = [0] if i <= 14 else [0, 1]
                    for nt in nts:
                        r0 = nt * 128 - 8 * i + 272
                        Sc.dma("sp", Ft[nt][:], T["s_G"][r0:r0 + 128, hs, :], writes=[("Ft", nt)])
                    for q, nt in enumerate(nts):
                        pS, pSk = nextS()
                        Sc.mm(pS[:], kcT[:, nt * 128:(nt + 1) * 128], qrhs, reads=["kcT", "qb"], writes=[pSk])
                        pt, ptk = pts.next()
                        Sc.act(pt[:], pS[:], AF.Exp, reads=[pSk], writes=[ptk])
                        Sc.V("tensor_tensor", pt2c[nt][:], pt[:], Ft[nt][:], ALU.mult, reads=[ptk, ("Ft", nt)],
                             writes=[("pt2c", nt)])
                        Sc.mm(po[0:64, :], vc[:, nt, g, :], pt2c[nt][:], start=(q == 0), stop=(q == len(nts) - 1),
                              reads=["vc", ("pt2c", nt)], writes=[pok])
                        Sc.mm(pu[:], onesr[:], pt2c[nt][:], start=(q == 0), stop=(q == len(nts) - 1),
                              reads=["onesr", ("pt2c", nt)], writes=[puk])
                    if pending[0] is not None:
                        pending[0]()
                        pending[0] = None
                    Sc.V("tensor_scalar", rcc[:], pu[:], 1.0e-18, None, ALU.max, reads=[puk], writes=["rcc"])
                    Sc.act(rcc[:], rcc[:], AF.Ln, reads=["rcc"], writes=["rcc"])
                    Sc.act(rcc[:], rcc[:], AF.Exp, reads=["rcc"], writes=["rcc"], scale=-1.0)
                    finish_branch(0)
                    sel_ops = []
                    psc, psck = ps[5], ("ps", 5)
                    for q, nt in enumerate(nts):
                        def _s1(q=q, nt=nt):
                            Sc.V("tensor_tensor", pn[:], r32(pt2c[nt][:]), rcc[:], ALU.mult,
                                 reads=[("pt2c", nt), "rcc"], writes=["pn"])
                            Sc.V("tensor_reduce", psT[:, nt, :], pn[:].rearrange("p (h t) -> p t h", h=4), AX.X,
                                 ALU.add, reads=["pn"], writes=[("psT", nt)])
                            Sc.mm(psc[:, 0:64], psT[:, nt, :], ovl[:, nt, :], start=(q == 0),
                                  stop=(q == len(nts) - 1), reads=[("psT", nt), "ovl"], writes=[psck])
                        sel_ops.append(_s1)

                    def _s2():
                        Sc.V("tensor_tensor", sc1[:], psc[:, 0:64], fc[:], ALU.max, reads=[psck, "fc"], writes=["sc1"])
                        Sc.V("tensor_tensor", sc1[:], sc1[:], ac[:], ALU.min, reads=["sc1", "ac"], writes=["sc1"])
                        Sc.V("max", m8a[:], sc1[:], reads=["sc1"], writes=["m8a"])

                    def _s3():
                        Sc.V("match_replace", sc2[:], m8a[:], sc1[:], -3.0e30, reads=["sc1", "m8a"], writes=["sc2"])
                        Sc.V("max", m8b[:], sc2[:], reads=["sc2"], writes=["m8b"])
                        Sc.V("tensor_scalar", sel[:], sc1[:], m8b[:, 7:8], None, ALU.is_ge, reads=["sc1", "m8b"],
                             writes=["sel"])

                    def _s4():
                        pT, pTk = ps[5], ("ps", 5)
                        Sc.tr(pT[0:64, 0:128], sel[:], ident[:], reads=["sel", "ident"], writes=[pTk])
                        Sc.act(selT[:], pT[0:64, 0:128], AF.Copy, reads=[pTk], writes=["selT"])
                    sel_ops += [_s2, _s3, _s4]
                    j0 = max(0, i - 4)
                    steps = []
                    for j in range(j0, i + 1):
                        def fr(j=j, i=i, g=g, hs=hs, qrhs=qrhs):
                            d = i - j
                            pS, pSk = nextS()
                            Sc.mm(pS[:], kwT[:, j * 128:(j + 1) * 128], qrhs, reads=["kwT", "qb"], writes=[pSk])
                            pt2, pt2k = pt2s.next()
                            if d in (2, 3):
                                Sc.act(pt2[:], pS[:], AF.Exp, reads=[pSk], writes=[pt2k])
                            else:
                                pt, ptk = pts.next()
                                Sc.act(pt[:], pS[:], AF.Exp, reads=[pSk], writes=[ptk])
                                if d == 0:
                                    Sc.V("tensor_tensor", pt2[:].rearrange("p (h t) -> p h t", h=4),
                                         pt[:].rearrange("p (h t) -> p h t", h=4), Ed[:, hs, :], ALU.mult,
                                         reads=[ptk, "C_Ed"], writes=[pt2k])
                                elif d == 1:
                                    Sc.V("tensor_tensor", pt2[:].rearrange("p (h t) -> p h t", h=4),
                                         pt[:].rearrange("p (h t) -> p h t", h=4), Eo[:, hs, :], ALU.mult,
                                         reads=[ptk, "C_Eo"], writes=[pt2k])
                                else:
                                    Sc.V("tensor_tensor", pt2[:].rearrange("p (h t) -> p h t", h=4),
                                         pt[:].rearrange("p (h t) -> p h t", h=4),
                                         w4[:].unsqueeze(1).to_broadcast([128, 4, 128]), ALU.mult, reads=[ptk, "w4"],
                                         writes=[pt2k])
                            return pt2, pt2k

                        def bk(cx, j=j, i=i, g=g, j0=j0):
                            pt2, pt2k = cx
                            Sc.mm(po[:], vw[:, j, g, :], pt2[:], start=(j == j0), stop=(j == i),
                                  reads=["vw", pt2k], writes=[pok])
                        steps.append((fr, bk))
                    pend = []
                    for fr_, bk_ in steps:
                        cx_ = fr_()
                        if sel_ops:
                            sel_ops.pop(0)()
                        pend.append((bk_, cx_))
                        if len(pend) > 2:
                            b_, c_ = pend.pop(0)
                            b_(c_)
                    for b_, c_ in pend:
                        b_(c_)
                    while sel_ops:
                        sel_ops.pop(0)()
                    shift_sums()
                    finish_branch(2)
                    steps = []
                    for j in range(i + 1):
                        def fr(j=j, i=i, g=g, hs=hs, qrhs=qrhs):
                            pS, pSk = nextS()
                            pM, pMk = ps[6 + j % 2], ("ps", 6 + j % 2)
                            Sc.mm(pS[:], ksT[:, j * 128:(j + 1) * 128], qrhs, reads=["ksT", "qb"], writes=[pSk])
                            Sc.mm(pM[:, 0:128], xs[:, j, :], selT[:], reads=["xs", "selT"], writes=[pMk])
                            pt, ptk = pts.next()
                            Sc.act(pt[:], pS[:], AF.Exp, reads=[pSk], writes=[ptk])
                            pt2, pt2k = pt2s.next()
                            mb = pM[:, 0:128].unsqueeze(1).to_broadcast([128, 4, 128])
                            if j >= i - 1:
                                E = Ed if j == i else Eo
                                Sc.V("tensor_tensor", MTh[:], E[:, hs, :], mb, ALU.mult, reads=["C_Ed", "C_Eo", pMk],
                                     writes=["MTh"])
                                Sc.V("tensor_tensor", pt2[:].rearrange("p (h t) -> p h t", h=4),
                                     pt[:].rearrange("p (h t) -> p h t", h=4), MTh[:], ALU.mult,
                                     reads=[ptk, "MTh"], writes=[pt2k])
                            else:
                                Sc.V("tensor_tensor", pt2[:].rearrange("p (h t) -> p h t", h=4),
                                     pt[:].rearrange("p (h t) -> p h t", h=4), mb, ALU.mult, reads=[ptk, pMk],
                                     writes=[pt2k])
                            return pt2, pt2k

                        def bk(cx, j=j, i=i, g=g):
                            pt2, pt2k = cx
                            Sc.mm(po[:], vs[:, j, g, :], pt2[:], start=(j == 0), stop=(j == i),
                                  reads=["vs", pt2k], writes=[pok])
                        steps.append((fr, bk))
                    pipeline(steps)
                    shift_sums()
                    finish_branch(1)
                    def _combine(i=i, g=g, a=a, t0=t0, tsl=tsl, ob=ob, obk=obk, gts=gts, gtk=gtk):
                        ogt, ogk = og[(2 * i + g) % 2], ("og", (2 * i + g) % 2)
                        for br in range(3):
                            Sc.V("tensor_tensor", Rbr[:].rearrange("p (h t) -> p h t", h=4),
                                 gts[:, tsl].unsqueeze(1).to_broadcast([24, 4, 128]),
                                 gmask[:, g * 3 + br, :].unsqueeze(2).to_broadcast([24, 4, 128]), ALU.mult,
                                 reads=[gtk, "gmask"], writes=["Rbr"])
                            pg, pgk = ps[5], ("ps", 5)
                            Sc.mm(pg[0:64, :], onesr[0:24, 0:64], Rbr[:], reads=["onesr", "Rbr"], writes=[pgk])
                            if br == 0:
                                Sc.V("tensor_tensor", ogt[:], ob[0][:], pg[0:64, :], ALU.mult, reads=[(obk, 0), pgk],
                                     writes=[ogk])
                            else:
                                Sc.V("tensor_tensor", tmpc[:], ob[br][:], pg[0:64, :], ALU.mult, reads=[(obk, br), pgk],
                                     writes=["tmpc"])
                                Sc.V("tensor_tensor", ogt[:], ogt[:], tmpc[:], ALU.add, reads=[ogk, "tmpc"], writes=[ogk])
                        rbase = 512 + 4 * g * 64
                        Sc.dma("sp", T["s_oT"][rbase:rbase + 256, t0 + a * 128:t0 + (a + 1) * 128].rearrange(
                            "(h d) t -> d h t", d=64), ogt[:].rearrange("p (h t) -> p h t", h=4), reads=[ogk],
                            writes=["s_oT"])
                    pending[0] = _combine
        if pending[0] is not None:
            pending[0]()
        Sc.emit()


ALL_PHASES = ("A", "B", "C", "D1", "D2", "E", "F", "G", "H")


def kernel(**inputs):
    inputs = {k: np.asarray(v) for k, v in inputs.items()}
    nb = inputs["x"].shape[0]
    feeds = make_feeds(inputs, list(range(nb)), ALL_PHASES)
    nc = build(phases=ALL_PHASES)
    res = run_bass_kernel_spmd(nc, feeds, core_ids=list(range(nb)))
    out = np.stack([np.asarray(r["out"], dtype=np.float32) for r in res.results], axis=0)
    return out
```

```python
import math
from contextlib import ExitStack

import numpy as np
import ml_dtypes
import concourse.bass as bass
import concourse.mybir as mybir
from concourse.bass_utils import run_bass_kernel_spmd

F32 = mybir.dt.float32
F32R = mybir.dt.float32r
BF16 = mybir.dt.bfloat16
AF = mybir.ActivationFunctionType
ALU = mybir.AluOpType
AX = mybir.AxisListType

S = 4096
D = 1024
NT = S // 128
ALPHA = 4 ** 0.25
EVEN_IN = 1768
D_FF = 2816
D_FFE = 3584
NEXP = 8
NEG = -1.0e30


class Op:
    __slots__ = ("eng", "fn", "deps", "sig", "ticket", "isdma", "slot", "sval", "idx", "prev")

    def __init__(self, eng, fn, isdma):
        self.eng = eng
        self.fn = fn
        self.deps = []
        self.sig = False
        self.ticket = None
        self.isdma = isdma
        self.slot = None
        self.sval = None


class Sched:
    KSLOT = 6
    CENG = ("pe", "act", "dve", "pool")

    def __init__(self, nc, es):
        self.nc = nc
        self.sem = {e: es.enter_context(nc.semaphore("s_" + e)) for e in self.CENG}
        self.cnt = {e: 0 for e in self.CENG}
        self.dsem = {q: [es.enter_context(nc.semaphore("d_%s%d" % (q, i))) for i in range(self.KSLOT)]
                     for q in ("sp", "pool")}
        self.dval = {q: [0] * self.KSLOT for q in ("sp", "pool")}
        self.dnext = {q: 0 for q in ("sp", "pool")}
        self.waited = {e: {} for e in ("pe", "act", "dve", "pool", "sp")}
        self.first_phase = True
        self.begin()

    def begin(self):
        self.ops = []
        self.lastw = {}
        self.readers = {}

    def add(self, eng, fn, reads=(), writes=(), isdma=False):
        op = Op(eng, fn, isdma)
        op.idx = len(self.ops)
        deps = set()
        for k in reads:
            w = self.lastw.get(k)
            if w is not None:
                deps.add(w)
        for k in writes:
            w = self.lastw.get(k)
            if w is not None:
                deps.add(w)
            for r in self.readers.get(k, ()):
                deps.add(r)
        deps.discard(op.idx)
        op.deps = sorted(deps)
        for k in reads:
            self.readers.setdefault(k, []).append(op.idx)
        for k in writes:
            self.lastw[k] = op.idx
            self.readers[k] = []
        self.ops.append(op)
        return op

    def mm(self, out, lhsT, rhs, start=True, stop=True, reads=(), writes=()):
        return self.add("pe", lambda e: e.matmul(out, lhsT, rhs, start=start, stop=stop), reads, writes)

    def tr(self, out, in_, ident, reads=(), writes=()):
        return self.add("pe", lambda e: e.transpose(out, in_, ident), reads, writes)

    def act(self, out, in_, func, reads=(), writes=(), **kw):
        return self.add("act", lambda e: e.activation(out=out, in_=in_, func=func, **kw), reads, writes)

    def dve(self, fn, reads=(), writes=()):
        return self.add("dve", fn, reads, writes)

    def pool(self, fn, reads=(), writes=()):
        return self.add("pool", fn, reads, writes)

    def V(self, name, *args, reads=(), writes=(), **kw):
        return self.add("dve", lambda e: getattr(e, name)(*args, **kw), reads, writes)

    def G(self, name, *args, reads=(), writes=(), **kw):
        return self.add("pool", lambda e: getattr(e, name)(*args, **kw), reads, writes)

    def dma(self, q, out, in_, reads=(), writes=(), **kw):
        return self.add(q, lambda e: e.dma_start(out=out, in_=in_, **kw), reads, writes, isdma=True)

    def emit(self, final=False):
        nc = self.nc
        ops = self.ops
        for op in ops:
            for d in op.deps:
                dop = ops[d]
                if dop.isdma:
                    continue
                if dop.eng == "pe" and op.eng == "pe" and not op.isdma:
                    continue
                dop.sig = True
        lastc = {}
        for op in ops:
            if not op.isdma:
                lastc[op.eng] = op
        for op in lastc.values():
            op.sig = True
        for op in ops:
            if op.isdma:
                q = op.eng
                s = self.dnext[q]
                self.dnext[q] = (s + 1) % self.KSLOT
                op.slot = s
                op.prev = self.dval[q][s]
                self.dval[q][s] += 16
                op.sval = self.dval[q][s]
            elif op.sig:
                self.cnt[op.eng] += 1
                op.ticket = self.cnt[op.eng]
        start_waits = []
        if not self.first_phase:
            for e in self.CENG:
                if self.prev_cnt[e] > 0:
                    start_waits.append((self.sem[e], self.prev_cnt[e]))
            for q in ("sp", "pool"):
                for s in range(self.KSLOT):
                    if self.prev_dval[q][s] > 0:
                        start_waits.append((self.dsem[q][s], self.prev_dval[q][s]))
        streams = {e: [] for e in ("sp", "act", "dve", "pool", "pe")}
        for op in ops:
            streams[op.eng].append(op)

        def run_stream(ename, eng):
            wd = self.waited[ename]

            def wait(sem, val):
                key = id(sem)
                if wd.get(key, 0) >= val:
                    return
                eng.wait_ge(sem, val)
                wd[key] = val

            for sem, val in start_waits:
                wait(sem, val)
            for op in streams[ename]:
                need = {}
                for d in op.deps:
                    dop = ops[d]
                    if dop.isdma:
                        sem, val = self.dsem[dop.eng][dop.slot], dop.sval
                    else:
                        if dop.eng == "pe" and ename == "pe" and not op.isdma:
                            continue
                        sem, val = self.sem[dop.eng], dop.ticket
                    k = id(sem)
                    if k not in need or need[k][1] < val:
                        need[k] = (sem, val)
                if op.isdma and op.prev > 0:
                    sem = self.dsem[ename][op.slot]
                    k = id(sem)
                    if k not in need or need[k][1] < op.prev:
                        need[k] = (sem, op.prev)
                for sem, val in need.values():
                    wait(sem, val)
                ins = op.fn(eng)
                if op.isdma:
                    ins.then_inc(self.dsem[ename][op.slot], 16)
                elif op.sig:
                    ins.then_inc(self.sem[ename], 1)
            if final:
                for q in ("sp", "pool"):
                    for s in range(self.KSLOT):
                        if self.dval[q][s] > 0:
                            wait(self.dsem[q][s], self.dval[q][s])
                for e in self.CENG:
                    if self.cnt[e] > 0:
                        wait(self.sem[e], self.cnt[e])

        with nc.Block() as block:
            @block.sync
            def _(e):
                run_stream("sp", e)

            @block.scalar
            def _(e):
                run_stream("act", e)

            @block.vector
            def _(e):
                run_stream("dve", e)

            @block.gpsimd
            def _(e):
                run_stream("pool", e)

            @block.tensor
            def _(e):
                run_stream("pe", e)

        self.prev_cnt = dict(self.cnt)
        self.prev_dval = {q: list(v) for q, v in self.dval.items()}
        self.first_phase = False
        self.begin()


class Rot:
    def __init__(self, items):
        self.items = items
        self.i = 0

    def next(self):
        it = self.items[self.i]
        self.i = (self.i + 1) % len(self.items)
        return it


def bc_rows(ap1d, nparts, n, off=0):
    return bass.AP(ap1d.tensor, ap1d.offset + off, [[0, nparts], [1, n]])


class Ctx:
    pass


def r32(ap):
    return ap.bitcast(F32)


def rr(ap):
    return ap.bitcast(F32R)


def pipeline(steps, depth=2):
    pend = []
    for fr, bk in steps:
        cx = fr()
        pend.append((bk, cx))
        if len(pend) > depth:
            b, c = pend.pop(0)
            b(c)
    for b, c in pend:
        b(c)


def phase_A(C):
    nc, Sc, T = C.nc, C.S, C.T
    ps = C.ps
    with ExitStack() as es:
        def sb(name, shape, dt=F32):
            return es.enter_context(nc.sbuf_tensor(name, shape, dt))

        Win = sb("A_Win", [128, 8, EVEN_IN], F32R)
        wqi = sb("A_wqi", [128, 2, 1024], F32R)
        Wql = sb("A_Wql", [128, 2, 8, 128], F32R)
        uq = sb("A_uq", [128, 2, 512])
        uk = sb("A_uk", [128, 512])
        tmpT = sb("A_tmpT", [64, 384])
        ident = sb("A_ident", [128, 128])
        onesr = sb("A_onesr", [128, 128], F32R)
        selw = sb("A_selw", [16, 8, 128])
        fold = sb("A_fold", [128, 64])
        qn_col = sb("A_qncol", [128, 2])
        kvn_col = sb("A_kvncol", [128, 1])
        kvn_bc = sb("A_kvnbc", [128, 128])
        xin = [sb("A_xin%d" % i, [128, 4, 1024]) for i in range(2)]
        xT = sb("A_xT", [128, 8, 512], F32R)
        cq = sb("A_cq", [128, 2, 512])
        sq = sb("A_sq", [128, 3, 512], F32R)
        rstd = sb("A_rstd", [128, 2, 512])
        cqn = sb("A_cqn", [128, 2, 512], F32R)
        ckv = sb("A_ckv", [128, 512])
        wT = sb("A_wT", [16, 512])
        absw = sb("A_absw", [16, 512])
        wbc = sb("A_wbc", [128, 2, 512])
        prod = sb("A_prod", [128, 512])
        sstat = sb("A_sstat", [128, 8])
        stg = Rot([(sb("A_stg%d" % i, [128, 512]), ("stg", i)) for i in range(6)])
        psr = Rot([(ps[i], ("ps", i)) for i in range(6)])
        evi = [0]

        def evac(out, in_, reads, writes, scale=None):
            evi[0] += 1
            if evi[0] % 2 == 0:
                if scale is None:
                    Sc.act(out, in_, AF.Copy, reads, writes)
                else:
                    Sc.act(out, in_, AF.Copy, reads, writes, scale=float(scale))
            else:
                if scale is None:
                    Sc.dve(lambda e: e.tensor_copy(out, in_), reads, writes)
                else:
                    Sc.dve(lambda e: e.tensor_scalar(out, in_, float(scale), None, ALU.mult), reads, writes)

        Sc.dma("sp", ident[:], T["c_ident"], writes=["ident"])
        Sc.dma("pool", onesr[:], T["c_ones"], writes=["onesr"])
        Sc.dma("sp", selw[:], T["c_selw"], writes=["selw"])
        Sc.dma("sp", fold[:], T["c_fold"], writes=["fold"])
        for k in range(8):
            Sc.dma("pool", Win[:, k, :], T["e_w_in"][k * 128:(k + 1) * 128, :], writes=[("Win", k)])
        for rc in range(2):
            Sc.dma("pool", wqi[:, rc, :], T["e_w_qidx"][rc * 128:(rc + 1) * 128, :], writes=["wqi"])
            Sc.dma("sp", uq[:, rc, :], T["e_w_uq"][rc * 128:(rc + 1) * 128, :], writes=["uq"])
            Sc.dma("sp", qn_col[:, rc:rc + 1], T["e_q_norm"][rc * 128:(rc + 1) * 128].rearrange("(p o) -> p o", o=1),
                   writes=["qncol"])
        Sc.dma("sp", uk[:], T["e_w_uk"], writes=["uk"])
        Sc.dma("sp", kvn_col[:], T["e_kv_norm"].rearrange("(p o) -> p o", o=1), writes=["kvncol"])
        Sc.dma("sp", kvn_bc[:], bc_rows(T["e_kv_norm"], 128, 128), writes=["kvnbc"])
        for h in range(8):
            p, pk = psr.next()
            for rc in range(2):
                Sc.tr(p[0:64, rc * 128:(rc + 1) * 128], uq[:, rc, h * 64:(h + 1) * 64], ident[:],
                      reads=["uq", "ident"], writes=[pk])
            Sc.tr(p[0:64, 256:384], uk[:, h * 64:(h + 1) * 64], ident[:], reads=["uk", "ident"], writes=[pk])
            Sc.dve(lambda e, p=p: e.tensor_copy(tmpT[:], p[0:64, 0:384]), reads=[pk], writes=["tmpT"])
            p2, pk2 = psr.next()
            for rc in range(2):
                Sc.mm(p2[:, rc * 128:(rc + 1) * 128], tmpT[:, rc * 128:(rc + 1) * 128], tmpT[:, 256:384],
                      reads=["tmpT"], writes=[pk2])
            Sc.act(Wql[:, :, h, :], p2[:, 0:256].rearrange("p (a c) -> p a c", a=2), AF.Copy, reads=[pk2],
                   writes=["Wql"])

        col_chunks = [
            ("kc", 976, 128), ("vc", 1104, 128), ("ks", 1232, 128), ("kw", 1488, 128),
        ]
        for blk in range(S // 512):
            t0 = blk * 512
            xi = xin[blk % 2]
            xk = ("xin", blk % 2)
            Sc.dma("sp", xi[:], T["x"][t0:t0 + 512, :].rearrange("(a p) d -> p a d", p=128), writes=[xk])
            for k in range(8):
                p, pk = psr.next()
                for a in range(4):
                    Sc.tr(p[:, a * 128:(a + 1) * 128], xi[:, a, k * 128:(k + 1) * 128], ident[:],
                          reads=[xk, "ident"], writes=[pk])
                evac(xT[:, k, :], p[:], [pk], [("xT", k)])
            xTk = [("xT", k) for k in range(8)]

            def proj(c0, w, p, pk):
                for k in range(8):
                    Sc.mm(p[0:w, :], Win[:, k, c0:c0 + w], xT[:, k, :], start=(k == 0), stop=(k == 7),
                          reads=[("Win", k), ("xT", k)], writes=[pk])

            for rc in range(2):
                p, pk = psr.next()
                proj(rc * 128, 128, p, pk)
                Sc.act(cq[:, rc, :], p[:], AF.Copy, reads=[pk], writes=[("cq", rc)])
                Sc.dve(lambda e, rc=rc: e.tensor_tensor(sq[:, rc, :], cq[:, rc, :], cq[:, rc, :], ALU.mult),
                       reads=[("cq", rc)], writes=[("sq", rc)])
            p, pk = psr.next()
            for rc in range(2):
                Sc.mm(p[:], onesr[:], sq[:, rc, :], start=(rc == 0), stop=(rc == 1),
                      reads=["onesr", ("sq", rc)], writes=[pk])
            Sc.act(rstd[:, 0, :], p[:], AF.Sqrt, reads=[pk], writes=["rstd0"], scale=1.0 / 256, bias=C.eps6[:])
            Sc.dve(lambda e: e.reciprocal(rstd[:, 0, :], rstd[:, 0, :]), reads=["rstd0"], writes=["rstd0"])
            for rc in range(2):
                Sc.dve(lambda e, rc=rc: e.scalar_tensor_tensor(cqn[:, rc, :], cq[:, rc, :], qn_col[:, rc:rc + 1],
                                                               rstd[:, 0, :], ALU.mult, ALU.mult),
                       reads=[("cq", rc), "qncol", "rstd0"], writes=[("cqn", rc)])
            p, pk = psr.next()
            proj(256, 128, p, pk)
            Sc.act(ckv[:], p[:], AF.Copy, reads=[pk], writes=["ckv"])
            Sc.dve(lambda e: e.tensor_tensor(sq[:, 2, :], ckv[:], ckv[:], ALU.mult), reads=["ckv"], writes=[("sq", 2)])
            p, pk = psr.next()
            Sc.mm(p[:], onesr[:], sq[:, 2, :], reads=["onesr", ("sq", 2)], writes=[pk])
            Sc.act(rstd[:, 1, :], p[:], AF.Sqrt, reads=[pk], writes=["rstd1"], scale=1.0 / 128, bias=C.eps6[:])
            Sc.dve(lambda e: e.reciprocal(rstd[:, 1, :], rstd[:, 1, :]), reads=["rstd1"], writes=["rstd1"])
            st, sk = stg.next()
            Sc.dve(lambda e, st=st: e.scalar_tensor_tensor(st[:], ckv[:], kvn_col[:, 0:1], rstd[:, 1, :], ALU.mult,
                                                           ALU.mult),
                   reads=["ckv", "kvncol", "rstd1"], writes=[sk])
            Sc.dma("sp", T["s_ckvT"][:, t0:t0 + 512], st[:], reads=[sk], writes=["s_ckvT"])
            p, pk = psr.next()
            proj(384, 64, p, pk)
            st, sk = stg.next()
            evac(st[0:64, :], p[0:64, :], [pk], [sk])
            Sc.dma("sp", T["s_kidxT"][:, t0:t0 + 512], st[0:64, :], reads=[sk], writes=["s_kidxT"])
            p, pk = psr.next()
            proj(448, 16, p, pk)
            Sc.act(wT[:], p[0:16, :], AF.Copy, reads=[pk], writes=["wT"], scale=0.25)
            Sc.act(absw[:], p[0:16, :], AF.Abs, reads=[pk], writes=["absw"], scale=0.25)
            for h in range(8):
                p, pk = psr.next()
                for rc in range(2):
                    Sc.mm(p[:], Wql[:, rc, h, :], cqn[:, rc, :], start=(rc == 0), stop=(rc == 1),
                          reads=["Wql", ("cqn", rc)], writes=[pk])
                st, sk = stg.next()
                evac(st[:], p[:], [pk], [sk], scale=0.125)
                Sc.dma("sp", T["s_qlatT"][h * 128:(h + 1) * 128, t0:t0 + 512], st[:], reads=[sk], writes=["s_qlatT"])
            pqs, pqsk = ps[6], ("ps", 6)
            for j in range(8):
                p, pk = psr.next()
                for rc in range(2):
                    Sc.mm(p[:], wqi[:, rc, j * 128:(j + 1) * 128], cqn[:, rc, :], start=(rc == 0), stop=(rc == 1),
                          reads=["wqi", ("cqn", rc)], writes=[pk])
                pb, pbk = psr.next()
                Sc.mm(pb[:, 0:512], selw[:, j, :], absw[:], reads=["selw", "absw"], writes=[pbk])
                Sc.act(wbc[:, 0, :], pb[:, 0:512], AF.Copy, reads=[pbk], writes=["wbc0"])
                pb2, pbk2 = psr.next()
                Sc.mm(pb2[:, 0:512], selw[:, j, :], wT[:], reads=["selw", "wT"], writes=[pbk2])
                Sc.act(wbc[:, 1, :], pb2[:, 0:512], AF.Copy, reads=[pbk2], writes=["wbc1"])
                st, sk = stg.next()
                Sc.dve(lambda e, st=st, p=p: e.tensor_tensor(st[:], p[:], wbc[:, 0, :], ALU.mult),
                       reads=[pk, "wbc0"], writes=[sk])
                Sc.dma("sp", T["s_qaT"][j * 128:(j + 1) * 128, t0:t0 + 512], st[:], reads=[sk], writes=["s_qaT"])
                Sc.dve(lambda e, p=p: e.tensor_tensor(prod[:], p[:], wbc[:, 1, :], ALU.mult),
                       reads=[pk, "wbc1"], writes=["prod"])
                Sc.mm(pqs[0:64, :], fold[:], prod[:], start=(j == 0), stop=(j == 7), reads=["fold", "prod"],
                      writes=[pqsk])
            st, sk = stg.next()
            evac(st[0:64, :], pqs[0:64, :], [pqsk], [sk])
            Sc.dma("sp", T["s_qsT"][:, t0:t0 + 512], st[0:64, :], reads=[sk], writes=["s_qsT"])
            for j in range(4):
                p, pk = psr.next()
                proj(464 + j * 128, 128, p, pk)
                st, sk = stg.next()
                evac(st[:], p[:], [pk], [sk], scale=0.125)
                Sc.dma("sp", T["s_qbT"][j * 128:(j + 1) * 128, t0:t0 + 512], st[:], reads=[sk], writes=["s_qbT"])
            for name, c0, w in col_chunks:
                p, pk = psr.next()
                proj(c0, w, p, pk)
                st, sk = stg.next()
                evac(st[:], p[:], [pk], [sk])
                Sc.dma("sp", T["s_" + name + "T"][:, t0:t0 + 512], st[:], reads=[sk], writes=["s_" + name])
            p, pk = psr.next()
            proj(1744, 24, p, pk)
            st, sk = stg.next()
            Sc.act(st[0:24, :], p[0:24, :], AF.Sigmoid, reads=[pk], writes=[sk])
            Sc.dma("sp", T["s_gatesT"][:, t0:t0 + 512], st[0:24, :], reads=[sk], writes=["s_gatesT"])
            for a in range(4):
                r0 = t0 + a * 128
                p, pk = psr.next()
                for k in range(8):
                    Sc.mm(p[:, 0:128], xT[:, k, a * 128:(a + 1) * 128], Win[:, k, 256:384], start=(k == 0),
                          stop=(k == 7), reads=[("Win", k), ("xT", k)], writes=[pk])
                st, sk = stg.next()
                col = sstat[:, 2 * a:2 * a + 1]
                Sc.act(st[:, 128:256], p[:, 0:128], AF.Square, reads=[pk], writes=[sk, ("ss", a)], accum_out=col)
                Sc.act(col, col, AF.Sqrt, reads=[("ss", a)], writes=[("ss", a)], scale=1.0 / 128, bias=C.eps6[:])
                Sc.dve(lambda e, col=col: e.reciprocal(col, col), reads=[("ss", a)], writes=[("ss", a)])
                Sc.dve(lambda e, st=st, p=p, col=col: e.scalar_tensor_tensor(st[:, 0:128], p[:, 0:128], col,
                                                                             kvn_bc[:], ALU.mult, ALU.mult),
                       reads=[pk, ("ss", a), "kvnbc", sk], writes=[sk])
                Sc.dma("sp", T["s_ckv"][r0:r0 + 128, :], st[:, 0:128], reads=[sk], writes=["s_ckv"])
                p, pk = psr.next()
                for k in range(8):
                    Sc.mm(p[:, 0:16], xT[:, k, a * 128:(a + 1) * 128], Win[:, k, 448:464], start=(k == 0),
                          stop=(k == 7), reads=[("Win", k), ("xT", k)], writes=[pk])
                st, sk = stg.next()
                Sc.act(st[:, 0:16], p[:, 0:16], AF.Sign, reads=[pk], writes=[sk])
                Sc.dma("sp", T["s_sgn"][r0:r0 + 128, :], st[:, 0:16], reads=[sk], writes=["s_sgn"])
                p, pk = psr.next()
                for k in range(8):
                    Sc.mm(p[:, 0:384], xT[:, k, a * 128:(a + 1) * 128], Win[:, k, 1360:1744], start=(k == 0),
                          stop=(k == 7), reads=[("Win", k), ("xT", k)], writes=[pk])
                st, sk = stg.next()
                evac(st[:, 0:384], p[:, 0:384], [pk], [sk])
                Sc.dma("sp", T["s_vs"][r0:r0 + 128, :], st[:, 0:128], reads=[sk], writes=["s_vs"])
                Sc.dma("sp", T["s_vw"][r0:r0 + 128, :], st[:, 256:384], reads=[sk], writes=["s_vw"])
        Sc.emit()


IN_SPECS = {
    "x": ([S, D], F32),
    "e_w_in": ([D, EVEN_IN], F32R),
    "e_q_norm": ([256], F32),
    "e_kv_norm": ([128], F32),
    "e_w_uq": ([256, 512], F32),
    "e_w_uk": ([128, 512], F32),
    "e_w_uv": ([128, 512], F32R),
    "e_w_qidx": ([256, 1024], F32R),
    "c_ident": ([128, 128], F32),
    "c_ones": ([128, 128], F32R),
    "c_selw": ([16, 8, 128], F32),
    "c_fold": ([128, 64], F32),
    "c_negtri": ([128, 128], F32),
    "c_cdiag": ([128, 128], F32),
    "g_bdiag": ([128, 16, 128], F32),
    "g_boff": ([128, 16, 128], F32),
    "g_b31": ([128, 16], F32),
    "e_ck1": ([32, 64, 64], F32), "e_cv1": ([32, 64, 64], F32), "e_ck2": ([64, 64], F32), "e_cv2": ([64, 64], F32),
    "e_pos_kT": ([64, 32], F32), "e_pos_vT": ([64, 32], F32),
    "g_pb": ([32, 8, 128], F32), "c_pm": ([32, 128], F32), "c_xs": ([64, 32, 128], BF16),
    "c_ovl": ([128, 2, 64], F32), "c_shift": ([128, 64], F32R), "c_w4": ([128, 128], F32), "c_gmask": ([24, 6, 4], F32),
    "c_fc": ([128, 32, 64], F32), "c_ac": ([128, 32, 64], F32),
    "c_xm": ([128, 16, 128], F32R), "c_zeros": ([128, S], F32R), "c_gm": ([128, 32, 16], F32), "c_adm": ([128, 32, 16], F32),
    "e_w_out": ([D, D], F32R), "e_ln1_g": ([D], F32), "e_ln1_b": ([D], F32),
    "e_ffn_w1": ([D, D_FF], F32R), "e_ffn_w3": ([D, D_FF], F32R), "e_ffn_w2": ([D_FF, D], F32R),
    "e_ln2_g": ([D], F32), "e_ln2_b": ([D], F32),
    "o_w_in": ([D, 3072], F32R), "o_w_out": ([D, D], F32R), "o_ln1_g": ([D], F32), "o_ln1_b": ([D], F32),
    "o_routerT": ([8 * D], F32),
    "o_moe_w1": ([NEXP, D, D_FFE], F32R), "o_moe_w3": ([NEXP, D, D_FFE], F32R), "o_moe_w2": ([NEXP, D_FFE, D], F32R),
    "o_ln2_g": ([D], F32), "o_ln2_b": ([D], F32),
}

SCRATCH = {
    "s_ckvT": [128, S], "s_ckv": [S, 128], "s_kidxT": [64, S], "s_qlatT": [1024, S], "s_qaT": [1024, S],
    "s_qsT": [64, S], "s_qbT": [512, S], "s_kcT": [128, S], "s_vcT": [128, S], "s_ksT": [128, S],
    "s_kwT": [128, S], "s_vs": [S, 128], "s_vw": [S, 128], "s_gatesT": [24, S],
    "s_oT": [1024, S], "s_sgn": [S, 16],
    "s_h0": [S, D], "s_hT0": [D, S], "s_x1": [S, D], "s_qT1": [D, S], "s_kT1": [D, S], "s_v1": [S, D],
    "s_oT1": [D, S], "s_h1": [S, D], "s_hT1": [D, S], "s_gate": [S, 8],
    "s_su1": [D, S], "s_G": [528, 8, 128], "s_kcc": [64, 2, 256], "s_vcc": [256, 2, 64],
}


def host_gathers(rel_bias):
    g = {}
    ss = np.arange(128)[:, None]
    tt = np.arange(128)[None, :]
    bd = t5_bucket_np(tt - ss)
    bo = t5_bucket_np(128 + tt - ss)
    g["g_bdiag"] = np.ascontiguousarray(rel_bias[bd].transpose(0, 2, 1))
    g["g_boff"] = np.ascontiguousarray(rel_bias[bo].transpose(0, 2, 1))
    g["g_b31"] = np.ascontiguousarray(np.broadcast_to(rel_bias[31][None, :], (128, 16)))
    m = np.arange(32)[:, None]
    dist = np.arange(128)[None, :] - 16 * m + 225
    g["g_pb"] = np.ascontiguousarray(rel_bias[:, 8:16][t5_bucket_np(dist)].transpose(0, 2, 1))
    return g


def host_consts():
    c = {}
    ss = np.arange(128)[:, None]
    tt = np.arange(128)[None, :]
    c["c_negtri"] = np.where(tt <= ss, 0.0, NEG).astype(np.float32)
    c["c_cdiag"] = (tt >= ss).astype(np.float32)
    xm = np.zeros((128, 16, 128), np.float32)
    for b in range(16):
        xm[b, b, :] = 1.0
    c["c_xm"] = xm
    own = (np.arange(32) // 2)[:, None]
    blk = np.arange(16)[None, :]
    c["c_gm"] = np.ascontiguousarray(np.broadcast_to(np.where(blk < own, 0.0, NEG)[None], (128, 32, 16))).astype(np.float32)
    c["c_adm"] = np.ascontiguousarray(np.broadcast_to((blk < own).astype(np.float32)[None], (128, 32, 16)))
    m = np.arange(32)[:, None]
    c["c_pm"] = ((np.arange(128)[None, :] - 16 * m + 225) >= 0).astype(np.float32)
    xs = np.zeros((64, 32, 128), np.float32)
    for j in range(32):
        xs[2 * j, j, 0:64] = 1.0
        xs[2 * j + 1, j, 64:128] = 1.0
    c["c_xs"] = xs
    cs = np.arange(256) * 16
    ss_ = np.arange(64) * 64
    ov = ((cs[:, None] + 31 >= ss_[None, :]) & (cs[:, None] <= ss_[None, :] + 63)).astype(np.float32)
    ov[255] = 0.0
    c["c_ovl"] = np.ascontiguousarray(ov.reshape(2, 128, 64).transpose(1, 0, 2))
    sh_ = np.zeros((128, 64), np.float32)
    sh_[64:128, :] = np.eye(64, dtype=np.float32)
    c["c_shift"] = sh_
    c["c_w4"] = (np.arange(128)[:, None] > np.arange(128)[None, :]).astype(np.float32)
    gmk = np.zeros((24, 6, 4), np.float32)
    for g_ in range(2):
        for br in range(3):
            for h_ in range(4):
                gmk[(4 * g_ + h_) * 3 + br, g_ * 3 + br, h_] = 1.0
    c["c_gmask"] = gmk
    tq = np.arange(S).reshape(32, 128).T
    cur = tq // 64
    jb = np.arange(64)[None, None, :]
    forced = (jb == 0) | (jb == cur[:, :, None]) | (jb == cur[:, :, None] - 1)
    c["c_fc"] = np.where(forced, 1.0e30, NEG).astype(np.float32)
    c["c_ac"] = np.where(jb > cur[:, :, None], NEG, 1.0e30).astype(np.float32)
    c["c_zeros"] = np.zeros((128, S), np.float32)
    c["c_ident"] = np.eye(128, dtype=np.float32)
    c["c_ones"] = np.ones((128, 128), np.float32)
    selw = np.zeros((16, 8, 128), np.float32)
    for j in range(8):
        selw[2 * j, j, 0:64] = 1.0
        selw[2 * j + 1, j, 64:128] = 1.0
    c["c_selw"] = selw
    c["c_fold"] = np.concatenate([np.eye(64, dtype=np.float32)] * 2, axis=0)
    return c


DEBUG_T = {"d_ps": [128, 1024], "d_pt": [128, 512], "d_pt2": [128, 512], "d_MTh": [128, 8, 128], "d_Ed": [128, 8, 128], "d_acc": [128, S], "d_Mm": [128, S], "d_stt": [128, 8], "d_olat": [128, 8, 512], "d_MT": [128, NT, 128]}


def build(phases=("A",), debug_outs=(), dbg_tile=None, debug_ins=()):
    nc = bass.Bass("TRN2", target_bir_lowering=False)
    C = Ctx()
    C.nc = nc
    C.dbg_tile = dbg_tile
    T = {}
    if dbg_tile is not None:
        for name, shape in DEBUG_T.items():
            T[name] = nc.dram_tensor(name, shape, F32, kind="ExternalOutput").ap()
    used = needed_inputs(phases)
    for name, (shape, dt) in IN_SPECS.items():
        if name in used:
            T[name] = nc.dram_tensor(name, shape, dt, kind="ExternalInput").ap()
    for name, shape in SCRATCH.items():
        kind = "ExternalOutput" if name in debug_outs else ("ExternalInput" if name in debug_ins else "Internal")
        T[name] = nc.dram_tensor(name, shape, F32, kind=kind).ap()
    T["out"] = nc.dram_tensor("out", [S, D], F32, kind="ExternalOutput").ap()
    C.T = T
    with ExitStack() as es:
        C.S = Sched(nc, es)
        C.ps = [es.enter_context(nc.psum_tensor("ps%d" % i, [128, 512], F32)) for i in range(8)]
        C.eps6 = es.enter_context(nc.sbuf_tensor("eps6", [128, 1], F32))
        C.eps5 = es.enter_context(nc.sbuf_tensor("eps5", [128, 1], F32))
        C.S.dve(lambda e: e.memset(C.eps6[:], 1e-6), writes=["eps6"])
        C.S.dve(lambda e: e.memset(C.eps5[:], 1e-5), writes=["eps5"])
        C.S.emit()
        if "A" in phases:
            phase_A(C)
        if "B" in phases:
            phase_B(C)
        if "C" in phases:
            phase_C(C)
        if "D1" in phases:
            phase_outproj(C, "D1_", T["s_oT"], T["e_w_out"], T["x"], T["e_ln1_g"], T["e_ln1_b"], T["s_h0"], T["s_hT0"])
        if "D2" in phases:
            phase_ffn(C, "D2_", T["s_hT0"], T["s_h0"], [T["e_ffn_w1"]], [T["e_ffn_w3"]], [T["e_ffn_w2"]], D_FF, None,
                      T["e_ln2_g"], T["e_ln2_b"], T["s_x1"])
        if "E" in phases:
            phase_E(C)
        if "F" in phases:
            phase_F(C)
        if "G" in phases:
            phase_outproj(C, "G_", T["s_oT1"], T["o_w_out"], T["s_x1"], T["o_ln1_g"], T["o_ln1_b"], T["s_h1"],
                          T["s_hT1"], router=T["o_routerT"], gate_dst=T["s_gate"], su_src=T["s_su1"])
        if "H" in phases:
            phase_ffn(C, "H_", T["s_hT1"], T["s_h1"], [T["o_moe_w1"][e_] for e_ in range(NEXP)],
                      [T["o_moe_w3"][e_] for e_ in range(NEXP)], [T["o_moe_w2"][e_] for e_ in range(NEXP)], D_FFE,
                      T["s_gate"], T["o_ln2_g"], T["o_ln2_b"], T["out"])
        C.S.dve(lambda e: e.memset(C.eps6[:], 1e-6), writes=["eps6"])
        C.S.emit(final=True)
    return nc


PHASE_INPUTS = {
    "A": ["x", "e_w_in", "e_q_norm", "e_kv_norm", "e_w_uq", "e_w_uk", "e_w_qidx", "c_ident", "c_ones", "c_selw",
          "c_fold"],
    "B": ["c_ident", "c_ones", "c_negtri", "c_cdiag", "g_bdiag", "g_boff", "g_b31", "e_w_uv", "c_zeros"],
    "C": ["e_ck1", "e_cv1", "e_ck2", "e_cv2", "e_pos_kT", "e_pos_vT", "g_pb", "c_pm", "c_xs", "c_ovl", "c_w4",
          "c_gmask", "c_fc", "c_ac", "c_shift", "c_zeros", "c_ident", "c_ones", "c_cdiag", "g_bdiag", "g_boff", "g_b31"],
    "D1": ["x", "e_w_out", "e_ln1_g", "e_ln1_b", "c_ident"],
    "D2": ["e_ffn_w1", "e_ffn_w3", "e_ffn_w2", "e_ln2_g", "e_ln2_b"],
    "E": ["o_w_in", "c_ident"],
    "F": ["c_ident", "c_ones", "c_cdiag", "g_bdiag", "g_boff", "g_b31", "c_xm", "c_gm", "c_adm", "c_zeros"],
    "G": ["o_w_out", "o_ln1_g", "o_ln1_b", "o_routerT", "c_ident"],
    "H": ["o_moe_w1", "o_moe_w3", "o_moe_w2", "o_ln2_g", "o_ln2_b"],
}


def needed_inputs(phases):
    u = set()
    for p in phases:
        u.update(PHASE_INPUTS[p])
    return u


NIT = 16


def t5_bucket_np(dist):
    n = np.maximum(dist, 0)
    large = 16 + (np.log(np.maximum(n, 1).astype(np.float32) / np.float32(16)) / np.float32(math.log(8.0))
                  * np.float32(16)).astype(np.int32)
    large = np.minimum(large, 31)
    return np.where(n < 16, n, large)


def make_E(C, es, h0, nh, tagp):
    nc, Sc, T = C.nc, C.S, C.T
    Ed = es.enter_context(nc.sbuf_tensor(tagp + "Ed", [128, nh, 128], F32))
    Eo = es.enter_context(nc.sbuf_tensor(tagp + "Eo", [128, nh, 128], F32))
    b31 = es.enter_context(nc.sbuf_tensor(tagp + "b31", [128, nh], F32))
    cd = es.enter_context(nc.sbuf_tensor(tagp + "cd", [128, 128], F32))
    Sc.dma("sp", Ed[:], T["g_bdiag"][:, h0:h0 + nh, :], writes=[tagp + "Ed"])
    Sc.dma("sp", Eo[:], T["g_boff"][:, h0:h0 + nh, :], writes=[tagp + "Eo"])
    Sc.dma("sp", b31[:], T["g_b31"][:, h0:h0 + nh], writes=[tagp + "b31"])
    Sc.dma("sp", cd[:], T["c_cdiag"], writes=[tagp + "cd"])
    for E, k in ((Ed, tagp + "Ed"), (Eo, tagp + "Eo")):
        Sc.dve(lambda e, E=E: e.tensor_tensor(E[:], E[:], b31[:].unsqueeze(2).to_broadcast([128, nh, 128]),
                                              ALU.subtract), reads=[k, tagp + "b31"], writes=[k])
        Sc.act(E[:], E[:], AF.Exp, reads=[k], writes=[k])
    Sc.dve(lambda e: e.tensor_tensor(Ed[:], Ed[:], cd[:].unsqueeze(1).to_broadcast([128, nh, 128]), ALU.mult),
           reads=[tagp + "Ed", tagp + "cd"], writes=[tagp + "Ed"])
    return Ed, Eo


def phase_B(C):
    nc, Sc, T = C.nc, C.S, C.T
    ps = C.ps
    with ExitStack() as es:
        def sb(name, shape, dt=F32):
            return es.enter_context(nc.sbuf_tensor(name, shape, dt))

        kidx2 = sb("B_kidx2", [128, S], F32R)
        ckvT = sb("B_ckvT", [128, S], F32R)
        ckv = sb("B_ckv", [128, NT, 128], F32R)
        onesr = sb("B_onesr", [128, 128], F32R)
        ident = sb("B_ident", [128, 128])
        negtri = sb("B_negtri", [128, 128])
        wuv = sb("B_wuv", [128, 512], F32R)
        qa = sb("B_qa", [128, 8, 2, 512], F32R)
        ql = sb("B_ql", [128, 4, 8, 128], F32R)
        acc = sb("B_acc", [128, S])
        Mm = sb("B_Mm", [128, S])
        MT = sb("B_MT", [128, NT, 128], mybir.dt.bfloat16)
        MTh = sb("B_MTh", [128, 8, 128])
        tmps = Rot([(sb("B_tmp%d" % i, [128, 512], F32R), ("tmp", i)) for i in range(4)])
        pts = Rot([(sb("B_pt%d" % i, [128, 512]), ("pt", i)) for i in range(3)])
        pt2s = Rot([(sb("B_pt2%d" % i, [128, 512], F32R), ("pt2", i)) for i in range(3)])
        olat = sb("B_olat", [128, 8, 512], F32R)
        rec = sb("B_rec", [128, 2, 512])
        stt = sb("B_stt", [128, 8])
        ost = Rot([(sb("B_ost%d" % i, [64, 512]), ("ost", i)) for i in range(2)])
        Ed, Eo = make_E(C, es, 0, 8, "B_")
        sg = sb("B_sg", [128, 4, 16])
        dg = sb("B_dg", [128, 16, 128], F32R)

        Sc.dma("pool", kidx2[0:64, :], T["s_kidxT"].bitcast(F32R), writes=["kidx2"])
        Sc.dma("pool", kidx2[64:128, :], T["s_kidxT"].bitcast(F32R), writes=["kidx2"])
        Sc.dma("pool", ckvT[:], T["s_ckvT"].bitcast(F32R), writes=["ckvT"])
        Sc.dma("pool", ckv[:], T["s_ckv"].bitcast(F32R).rearrange("(j p) c -> p j c", p=128), writes=["ckv"])
        Sc.dma("pool", onesr[:], T["c_ones"], writes=["onesr"])
        Sc.dma("pool", qa[64:128, :, 0, :], T["c_zeros"][64:128, :].rearrange("p (j t) -> p j t", j=8), writes=["qa"])
        Sc.dma("pool", qa[0:64, :, 1, :], T["c_zeros"][0:64, :].rearrange("p (j t) -> p j t", j=8), writes=["qa"])
        Sc.dma("sp", ident[:], T["c_ident"], writes=["ident"])
        Sc.dma("sp", negtri[:], T["c_negtri"], writes=["negtri"])
        Sc.dma("pool", wuv[:], T["e_w_uv"], writes=["wuv"])
        mx, mn, w0, lo, mid, cnt, tg = [stt[:, i:i + 1] for i in range(7)]

        def load_idx_block(blk):
            t0 = blk * 512
            qav = T["s_qaT"].bitcast(F32R)[:, t0:t0 + 512].rearrange("(j r d) t -> r d j t", r=2, d=64)
            Sc.dma("pool", qa[0:64, :, 0, :], qav[0], writes=["qa"])
            Sc.dma("pool", qa[64:128, :, 1, :], qav[1], writes=["qa"])
            Sc.dma("sp", sg[:], T["s_sgn"][t0:t0 + 512, :].rearrange("(a p) h -> p a h", p=128), writes=["sg"])

        def load_att_block(blk):
            t0 = blk * 512
            for a_ in range(4):
                Sc.dma("pool", ql[:, a_, :, :],
                       T["s_qlatT"].bitcast(F32R)[:, t0 + a_ * 128:t0 + (a_ + 1) * 128].rearrange(
                           "(h c) t -> c h t", c=128), writes=["ql"])

        def indexer(i):
            a = i % 4
            nk = i + 1
            n = nk * 128
            tsl = slice(a * 128, (a + 1) * 128)
            for h in range(16):
                Sc.V("tensor_scalar", dg[:, h, :], ident[:], sg[:, a, h:h + 1], None, ALU.mult,
                     reads=["ident", "sg"], writes=[("dg", h)])
            for kb in range((nk + 3) // 4):
                w = min(512, n - kb * 512)
                ks = slice(kb * 512, kb * 512 + w)
                sacc, sk = ps[3], ("ps", 3)
                steps = []
                for h in range(16):
                    def fr(h=h, w=w, ks=ks):
                        p, pk = ps[h % 3], ("ps", h % 3)
                        Sc.mm(p[:, :w], qa[:, h // 2, h % 2, tsl], kidx2[:, ks], reads=["qa", "kidx2"], writes=[pk])
                        tm, tk = tmps.next()
                        if h % 2 == 0:
                            Sc.act(tm[:, :w], p[:, :w], AF.Relu, reads=[pk], writes=[tk])
                        else:
                            Sc.V("tensor_scalar", tm[:, :w], p[:, :w], 0.0, None, ALU.max, reads=[pk], writes=[tk])
                        return tm, tk

                    def bk(cx, h=h, w=w, sacc=sacc, sk=sk):
                        tm, tk = cx
                        Sc.mm(sacc[:, :w], dg[:, h, :], tm[:, :w], start=(h == 0), stop=(h == 15),
                              reads=[("dg", h), tk], writes=[sk])
                    steps.append((fr, bk))
                pipeline(steps)
                Sc.act(acc[:, ks], sacc[:, :w], AF.Copy, reads=[sk], writes=["acc"])
            if i >= 2:
                Sc.V("tensor_reduce", mx, acc[:, :n], AX.X, ALU.max, reads=["acc"], writes=["mx"])
                Sc.V("tensor_reduce", mn, acc[:, :n], AX.X, ALU.min, reads=["acc"], writes=["mn"])
                Sc.V("tensor_tensor", w0, mx, mn, ALU.subtract, reads=["mx", "mn"], writes=["w0"])
                Sc.V("tensor_copy", lo, mn, reads=["mn"], writes=["lo"])
            else:
                Sc.V("memset", lo, -1.0e29, writes=["lo"])
            Sc.V("tensor_tensor", acc[:, i * 128:(i + 1) * 128], acc[:, i * 128:(i + 1) * 128], negtri[:], ALU.add,
                 reads=["acc", "negtri"], writes=["acc"])

        def bisect_ops(i):
            n = (i + 1) * 128
            ops = []
            if i < 2:
                return ops
            for it in range(NIT):
                ck = 2.0 ** -(it + 1)
                ops.append(lambda ck=ck: Sc.V("scalar_tensor_tensor", mid, w0, ck, lo, ALU.mult, ALU.add,
                                              reads=["w0", "lo"], writes=["mid"]))
                ops.append(lambda n=n: Sc.V("tensor_scalar", Mm[:, :n], acc[:, :n], mid, None, ALU.is_ge, ALU.add,
                                            accum_out=cnt, reads=["acc", "mid"], writes=["Mm", "cnt"]))
                ops.append(lambda ck=ck: Sc.V("tensor_scalar", tg, cnt, 255.5, ck, ALU.is_ge, ALU.mult,
                                              reads=["cnt"], writes=["tg"]))
                ops.append(lambda: Sc.V("scalar_tensor_tensor", lo, tg, w0, lo, ALU.mult, ALU.add,
                                        reads=["tg", "w0", "lo"], writes=["lo"]))
            return ops

        def mask_op(i):
            n = (i + 1) * 128
            Sc.V("tensor_scalar", Mm[:, :n], acc[:, :n], lo, None, ALU.is_ge, reads=["acc", "lo"], writes=["Mm"])

        def transposes(i):
            nk = i + 1
            for j0 in range(0, nk, 4):
                g = min(4, nk - j0)
                p, pk = ps[4 + (j0 // 4) % 2], ("ps", 4 + (j0 // 4) % 2)
                for jj in range(g):
                    j = j0 + jj
                    Sc.tr(p[:, jj * 128:(jj + 1) * 128], Mm[:, j * 128:(j + 1) * 128], ident[:],
                          reads=["Mm", "ident"], writes=[pk])
                Sc.act(MT[:, j0:j0 + g, :], p[:, :g * 128].rearrange("p (g t) -> p g t", g=g), AF.Copy,
                       reads=[pk], writes=[("MT", j0 // 4)])

        def attention(i, extra):
            a = i % 4
            nk = i + 1
            tsl = slice(a * 128, (a + 1) * 128)
            nsteps = 2 * nk
            per = -(-len(extra) // nsteps) if extra else 0
            steps = []
            for j in range(nk):
                near = (j >= i - 1)
                for half in range(2):
                    def fr(j=j, half=half, near=near):
                        if near and half == 0:
                            E = Ed if j == i else Eo
                            ek = "B_Ed" if j == i else "B_Eo"
                            Sc.V("tensor_tensor", MTh[:], E[:], MT[:, j:j + 1, :].to_broadcast([128, 8, 128]),
                                 ALU.mult, reads=[ek, ("MT", j // 4)], writes=["MTh"])
                        sb_ = (2 * j + half) % 3
                        st_, stk = ps[sb_], ("ps", sb_)
                        Sc.mm(st_[:], ckvT[:, j * 128:(j + 1) * 128], ql[:, a, 4 * half:4 * half + 4, :],
                              reads=["ckvT", "ql"], writes=[stk])
                        pt, ptk = pts.next()
                        Sc.act(pt[:], st_[:], AF.Exp, reads=[stk], writes=[ptk])
                        pt2, pt2k = pt2s.next()
                        if near:
                            Sc.V("tensor_tensor", pt2[:].rearrange("p (h t) -> p h t", h=4),
                                 pt[:].rearrange("p (h t) -> p h t", h=4), MTh[:, 4 * half:4 * half + 4, :],
                                 ALU.mult, reads=[ptk, "MTh"], writes=[pt2k])
                        else:
                            Sc.V("tensor_tensor", pt2[:].rearrange("p (h t) -> p h t", h=4),
                                 pt[:].rearrange("p (h t) -> p h t", h=4),
                                 MT[:, j:j + 1, :].to_broadcast([128, 4, 128]), ALU.mult,
                                 reads=[ptk, ("MT", j // 4)], writes=[pt2k])
                        for _ in range(per):
                            if extra:
                                extra.pop(0)()
                        return pt2, pt2k

                    def bk(cx, j=j, half=half):
                        pt2, pt2k = cx
                        Sc.mm(ps[4 + half][:], ckv[:, j, :], pt2[:], start=(j == 0), stop=(j == nk - 1),
                              reads=["ckv", pt2k], writes=[("ps", 4 + half)])
                        Sc.mm(ps[6 + half][:], onesr[:], pt2[:], start=(j == 0), stop=(j == nk - 1),
                              reads=["onesr", pt2k], writes=[("ps", 6 + half)])
                    steps.append((fr, bk))
            pipeline(steps)
            while extra:
                extra.pop(0)()
            for half in range(2):
                Sc.act(rec[:, half, :], ps[6 + half][:], AF.Ln, reads=[("ps", 6 + half)], writes=[("rec", half)])
                Sc.act(rec[:, half, :], rec[:, half, :], AF.Exp, reads=[("rec", half)], writes=[("rec", half)],
                       scale=-1.0)
                Sc.V("tensor_tensor", olat[:, 4 * half:4 * half + 4, tsl],
                     ps[4 + half][:].rearrange("p (h t) -> p h t", h=4),
                     rec[:, half, :].rearrange("p (h t) -> p h t", h=4), ALU.mult,
                     reads=[("ps", 4 + half), ("rec", half)], writes=["olat"])

        def block_end(blk):
            t0 = blk * 512
            for h in range(8):
                p, pk = ps[h % 3], ("ps", h % 3)
                Sc.mm(p[0:64, :], wuv[:, h * 64:(h + 1) * 64], olat[:, h, :], reads=["wuv", "olat"], writes=[pk])
                o_, ok = ost.next()
                Sc.act(o_[:], p[0:64, :], AF.Copy, reads=[pk], writes=[ok])
                Sc.dma("sp", T["s_oT"][h * 64:(h + 1) * 64, t0:t0 + 512], o_[:], reads=[ok], writes=["s_oT"])

        load_idx_block(0)
        indexer(0)
        mask_op(0)
        indexer(1)
        transposes(0)
        for i in range(NT):
            if i % 4 == 0:
                load_att_block(i // 4)
            nxt = i + 1
            ops = bisect_ops(nxt) if nxt < NT else []
            attention(i, ops)
            if nxt < NT:
                mask_op(nxt)
                if nxt + 1 < NT:
                    if (nxt + 1) % 4 == 0:
                        load_idx_block((nxt + 1) // 4)
                    indexer(nxt + 1)
                transposes(nxt)
            if i % 4 == 3:
                block_end(i // 4)
        Sc.emit()


def make_feeds(inputs, batches, phases=None):
    consts = host_consts()
    consts.update(host_gathers(np.asarray(inputs["rel_bias"], np.float32)))
    shared = dict(consts)
    sh = {
        "e_w_in": inputs["e_w_in"][0], "e_q_norm": inputs["e_q_norm"][0], "e_kv_norm": inputs["e_kv_norm"][0],
        "e_w_uq": inputs["e_w_uq"][0].reshape(256, 512), "e_w_uk": inputs["e_w_uk"][0].reshape(128, 512),
        "e_w_uv": inputs["e_w_uv"][0].reshape(128, 512), "e_w_qidx": inputs["e_w_qidx"][0].reshape(256, 1024),
        "o_routerT": inputs["o_router"][0].T.reshape(-1),
        "e_ck1": inputs["e_ck1"][0], "e_cv1": inputs["e_cv1"][0], "e_ck2": inputs["e_ck2"][0],
        "e_cv2": inputs["e_cv2"][0], "e_pos_kT": inputs["e_pos_k"][0].T, "e_pos_vT": inputs["e_pos_v"][0].T,
    }
    for k in ("e_w_out", "e_ln1_g", "e_ln1_b", "e_ffn_w1", "e_ffn_w3", "e_ffn_w2", "e_ln2_g", "e_ln2_b", "o_w_in",
              "o_w_out", "o_ln1_g", "o_ln1_b", "o_moe_w1", "o_moe_w3", "o_moe_w2", "o_ln2_g", "o_ln2_b"):
        sh[k] = inputs[k][0]
    shared.update(sh)
    used = needed_inputs(phases) if phases is not None else set(IN_SPECS)
    shared = {k: np.ascontiguousarray(v, dtype=(ml_dtypes.bfloat16 if IN_SPECS[k][1] == BF16 else np.float32))
              for k, v in shared.items() if k in IN_SPECS and k in used}
    feeds = []
    for b in batches:
        f = dict(shared)
        if "x" in used:
            f["x"] = np.ascontiguousarray(inputs["x"][b], dtype=np.float32)
        feeds.append(f)
    return feeds


def layer_norm_tile(C, z, zk, out, outk, g_bc, b_bc, st, stk, junk, junkk):
    Sc = C.S
    mean, var = st[:, 0:1], st[:, 1:2]
    Sc.V("tensor_reduce", mean, z, AX.X, ALU.add, reads=[zk], writes=[stk])
    Sc.V("tensor_scalar", mean, mean, 1.0 / D, None, ALU.mult, reads=[stk], writes=[stk])
    Sc.V("tensor_scalar", z, z, mean, None, ALU.subtract, reads=[zk, stk], writes=[zk])
    Sc.act(junk, z, AF.Square, reads=[zk], writes=[junkk, stk], accum_out=var)
    Sc.act(var, var, AF.Sqrt, reads=[stk], writes=[stk], scale=1.0 / D, bias=C.eps5[:])
    Sc.V("reciprocal", var, var, reads=[stk], writes=[stk])
    Sc.V("scalar_tensor_tensor", out, z, var, g_bc, ALU.mult, ALU.mult, reads=[zk, stk, "ln_g"], writes=[outk])
    Sc.V("tensor_tensor", out, out, b_bc, ALU.add, reads=[outk, "ln_b"], writes=[outk])


def phase_outproj(C, tag, oT_src, wout, x_src, ln_g, ln_b, h_dst, hT_dst, router=None, gate_dst=None,
                  su_src=None):
    nc, Sc, T = C.nc, C.S, C.T
    ps = C.ps
    with ExitStack() as es:
        def sb(name, shape, dt=F32):
            return es.enter_context(nc.sbuf_tensor(tag + name, shape, dt))

        W = sb("W", [128, 8, 1024], F32R)
        ident = sb("ident", [128, 128])
        g_bc = sb("g_bc", [128, 1024])
        b_bc = sb("b_bc", [128, 1024])
        oT = [sb("oT%d" % i, [128, 8, 512], F32R) for i in range(2)]
        xt = [sb("xt%d" % i, [128, 1024]) for i in range(2)]
        z = [sb("z%d" % i, [128, 1024]) for i in range(2)]
        hh = [sb("h%d" % i, [128, 1024]) for i in range(2)]
        hT = [sb("hT%d" % i, [128, 8, 128]) for i in range(2)]
        junk = sb("junk", [128, 1024])
        st = sb("st", [128, 16])
        if su_src is not None:
            o32s = [sb("o32_%d" % i, [128, 8, 512]) for i in range(2)]
            sus = [sb("su_%d" % i, [128, 8, 512]) for i in range(2)]

            def load_osu(b_):
                tb_ = b_ * 512
                Sc.dma("sp", o32s[b_ % 2][:], oT_src[:, tb_:tb_ + 512].rearrange("(k p) t -> p k t", p=128),
                       writes=[("o32", b_ % 2)])
                Sc.dma("sp", sus[b_ % 2][:], su_src[:, tb_:tb_ + 512].rearrange("(k p) t -> p k t", p=128),
                       writes=[("su", b_ % 2)])
        if router is not None:
            rT = sb("rT", [128, 8, 8])
            lg = sb("lg", [128, 8])
            gt = sb("gt", [128, 8])
            tmp8 = sb("tmp8", [128, 8])
        for k in range(8):
            Sc.dma("pool", W[:, k, :], wout[k * 128:(k + 1) * 128, :], writes=[("W", k)])
        Sc.dma("sp", ident[:], T["c_ident"], writes=["ident"])
        Sc.dma("sp", g_bc[:], bc_rows(ln_g, 128, 1024), writes=["ln_g"])
        Sc.dma("sp", b_bc[:], bc_rows(ln_b, 128, 1024), writes=["ln_b"])
        if router is not None:
            rview = router.rearrange("(e k p) -> k p e", e=8, k=8)
            for k in range(8):
                Sc.dma("sp", rT[:, k, :], rview[k], writes=["rT"], allow_slow_non_contiguous=True)
        for blk in range(S // 512):
            t0 = blk * 512
            o_ = oT[blk % 2]
            ok = ("oT", blk % 2)
            if su_src is None:
                Sc.dma("pool", o_[:], oT_src.bitcast(F32R)[:, t0:t0 + 512].rearrange("(k p) t -> p k t", p=128),
                       writes=[ok])
            else:
                if blk == 0:
                    load_osu(0)
                if blk + 1 < S // 512:
                    load_osu(blk + 1)
                o32, su = o32s[blk % 2], sus[blk % 2]
                o32k, suk = ("o32", blk % 2), ("su", blk % 2)
                Sc.act(su[:], su[:], AF.Ln, reads=[suk], writes=[suk])
                Sc.act(su[:], su[:], AF.Exp, reads=[suk], writes=[suk], scale=-1.0)
                Sc.V("tensor_tensor", o_[:], o32[:], su[:], ALU.mult, reads=[o32k, suk], writes=[ok])
            for a in range(4):
                ti = blk * 4 + a
                r0 = ti * 128
                b2 = ti % 2
                xk, zk, hk, hTk = ("xt", b2), ("z", b2), ("h", b2), ("hT", b2)
                Sc.dma("sp", xt[b2][:], x_src[r0:r0 + 128, :], writes=[xk])
                for half in range(2):
                    p, pk = ps[(2 * ti + half) % 4], ("ps", (2 * ti + half) % 4)
                    for k in range(8):
                        Sc.mm(p[:], o_[:, k, a * 128:(a + 1) * 128], W[:, k, half * 512:(half + 1) * 512],
                              start=(k == 0), stop=(k == 7), reads=[ok, ("W", k)], writes=[pk])
                    Sc.V("scalar_tensor_tensor", z[b2][:, half * 512:(half + 1) * 512],
                         xt[b2][:, half * 512:(half + 1) * 512], float(ALPHA), p[:], ALU.mult, ALU.add,
                         reads=[xk, pk], writes=[zk])
                stt = st[:, 2 * b2:2 * b2 + 2]
                layer_norm_tile(C, z[b2][:], zk, hh[b2][:], hk, g_bc[:], b_bc[:], stt, ("st", b2), junk[:], "junk")
                Sc.dma("sp", h_dst[r0:r0 + 128, :], hh[b2][:], reads=[hk], writes=["h_dst"])
                for g4 in range(2):
                    p, pk = ps[4 + (2 * ti + g4) % 4], ("ps", 4 + (2 * ti + g4) % 4)
                    for kk in range(4):
                        k = g4 * 4 + kk
                        Sc.tr(p[:, kk * 128:(kk + 1) * 128], hh[b2][:, k * 128:(k + 1) * 128], ident[:],
                              reads=[hk, "ident"], writes=[pk])
                    Sc.act(hT[b2][:, g4 * 4:g4 * 4 + 4, :], p[:].rearrange("p (k t) -> p k t", k=4), AF.Copy,
                           reads=[pk], writes=[hTk])
                Sc.dma("sp", hT_dst[:, r0:r0 + 128].rearrange("(k p) t -> p k t", p=128), hT[b2][:], reads=[hTk],
                       writes=["hT_dst"])
                if router is not None:
                    pl, plk = ps[(2 * ti) % 4], ("ps", (2 * ti) % 4)
                    for k in range(8):
                        Sc.mm(pl[:, 0:8], hT[b2][:, k, :], rT[:, k, :], start=(k == 0), stop=(k == 7),
                              reads=[hTk, "rT"], writes=[plk])
                    Sc.V("tensor_copy", lg[:], pl[:, 0:8], reads=[plk], writes=["lg"])
                    m1, m2, dd, g1, g2 = [st[:, 8 + q:9 + q] for q in range(5)]
                    Sc.V("tensor_reduce", m1, lg[:], AX.X, ALU.max, reads=["lg"], writes=["m1"])
                    Sc.V("tensor_scalar", tmp8[:], lg[:], m1, None, ALU.is_ge, reads=["lg", "m1"], writes=["tmp8"])
                    Sc.V("scalar_tensor_tensor", gt[:], tmp8[:], -1.0e30, lg[:], ALU.mult, ALU.add,
                         reads=["tmp8", "lg"], writes=["gt"])
                    Sc.V("tensor_reduce", m2, gt[:], AX.X, ALU.max, reads=["gt"], writes=["m2"])
                    Sc.V("tensor_tensor", dd, m2, m1, ALU.subtract, reads=["m1", "m2"], writes=["dd"])
                    Sc.act(g2, dd, AF.Exp, reads=["dd"], writes=["g2"])
                    Sc.V("tensor_scalar", g1, g2, 1.0, None, ALU.add, reads=["g2"], writes=["g1"])
                    Sc.V("reciprocal", g1, g1, reads=["g1"], writes=["g1"])
                    Sc.V("tensor_tensor", g2, g2, g1, ALU.mult, reads=["g1", "g2"], writes=["g2"])
                    Sc.V("tensor_scalar", tmp8[:], tmp8[:], g1, None, ALU.mult, reads=["tmp8", "g1"], writes=["tmp8"])
                    Sc.V("tensor_scalar", gt[:], gt[:], m2, g2, ALU.is_ge, ALU.mult, reads=["gt", "m2", "g2"],
                         writes=["gt"])
                    Sc.V("tensor_tensor", gt[:], gt[:], tmp8[:], ALU.add, reads=["gt", "tmp8"], writes=["gt"])
                    Sc.dma("sp", gate_dst[r0:r0 + 128, :], gt[:], reads=["gt"], writes=["gate_dst"])
        Sc.emit()


def phase_ffn(C, tag, hT_src, h_src, w1s, w3s, w2s, dff, gate_src, ln_g, ln_b, out_dst):
    nc, Sc, T = C.nc, C.S, C.T
    ps = C.ps
    nexp = len(w1s)
    ngrp = dff // 256
    with ExitStack() as es:
        def sb(name, shape, dt=F32):
            return es.enter_context(nc.sbuf_tensor(tag + name, shape, dt))

        hT = sb("hT", [128, 8, 1024], F32R)
        yaccs = [sb("yacc%d" % i, [128, 8, 1024]) for i in range(2)]
        pending_ln = []
        w1g = [sb("w1g%d" % i, [128, 8, 256], F32R) for i in range(2)]
        w3g = [sb("w3g%d" % i, [128, 8, 256], F32R) for i in range(2)]
        w2g = [sb("w2g%d" % i, [128, 2, 1024], F32R) for i in range(2)]
        hc = [sb("hc%d" % i, [128, 2, 1024], F32R) for i in range(2)]
        s1 = [sb("s1%d" % i, [128, 512]) for i in range(2)]
        g_bc = sb("g_bc", [128, 1024])
        b_bc = sb("b_bc", [128, 1024])
        gates = sb("gates", [128, 8, 8])
        ht = [sb("ht%d" % i, [128, 1024]) for i in range(2)]
        oo = [sb("oo%d" % i, [128, 1024]) for i in range(2)]
        junk = sb("junk", [128, 1024])
        st = sb("st", [128, 8])
        Sc.dma("sp", g_bc[:], bc_rows(ln_g, 128, 1024), writes=["ln_g"])
        Sc.dma("sp", b_bc[:], bc_rows(ln_b, 128, 1024), writes=["ln_b"])
        gi = 0
        for tb in range(S // 1024):
            t0 = tb * 1024
            yacc = yaccs[tb % 2]
            yb = tb % 2
            Sc.dma("pool", hT[:], hT_src.bitcast(F32R)[:, t0:t0 + 1024].rearrange("(k p) t -> p k t", p=128),
                   writes=["hT"])
            if gate_src is not None:
                Sc.dma("sp", gates[:], gate_src[t0:t0 + 1024, :].rearrange("(a p) e -> p a e", p=128), writes=["gates"])
            for e_ in range(nexp):
                for grp in range(ngrp):
                    b2 = gi % 2
                    gi += 1
                    c0 = grp * 256
                    Sc.dma("pool", w1g[b2][:], w1s[e_][:, c0:c0 + 256].rearrange("(k p) f -> p k f", p=128),
                           writes=[("w1g", b2)])
                    Sc.dma("pool", w3g[b2][:], w3s[e_][:, c0:c0 + 256].rearrange("(k p) f -> p k f", p=128),
                           writes=[("w3g", b2)])
                    Sc.dma("pool", w2g[b2][:], w2s[e_][c0:c0 + 256, :].rearrange("(c p) d -> p c d", p=128),
                           writes=[("w2g", b2)])
                    hcb = hc[b2]
                    hck = ("hc", b2)
                    u = 0
                    for c in range(2):
                        for th in range(2):
                            p1, p1k = ps[u % 2], ("ps", u % 2)
                            p3, p3k = ps[2 + u % 2], ("ps", 2 + u % 2)
                            u += 1
                            for k in range(8):
                                Sc.mm(p1[:], w1g[b2][:, k, c * 128:(c + 1) * 128], hT[:, k, th * 512:(th + 1) * 512],
                                      start=(k == 0), stop=(k == 7), reads=[("w1g", b2), "hT"], writes=[p1k])
                            for k in range(8):
                                Sc.mm(p3[:], w3g[b2][:, k, c * 128:(c + 1) * 128], hT[:, k, th * 512:(th + 1) * 512],
                                      start=(k == 0), stop=(k == 7), reads=[("w3g", b2), "hT"], writes=[p3k])
                            sx, sxk = s1[u % 2], ("s1", u % 2)
                            Sc.act(sx[:], p1[:], AF.Silu, reads=[p1k], writes=[sxk])
                            Sc.V("tensor_tensor", hcb[:, c, th * 512:(th + 1) * 512], sx[:], p3[:], ALU.mult,
                                 reads=[sxk, p3k], writes=[hck])
                    first = (e_ == 0 and grp == 0)
                    v = 0
                    for tt in range(8):
                        for dh in range(2):
                            py, pyk = ps[4 + v % 4], ("ps", 4 + v % 4)
                            v += 1
                            for c in range(2):
                                Sc.mm(py[:], hcb[:, c, tt * 128:(tt + 1) * 128], w2g[b2][:, c, dh * 512:(dh + 1) * 512],
                                      start=(c == 0), stop=(c == 1), reads=[hck, ("w2g", b2)], writes=[pyk])
                            ya = yacc[:, tt, dh * 512:(dh + 1) * 512]
                            yk = ("yacc", yb, tt, dh)
                            if gate_src is None:
                                if first:
                                    Sc.V("tensor_copy", ya, py[:], reads=[pyk], writes=[yk])
                                else:
                                    Sc.V("tensor_tensor", ya, ya, py[:], ALU.add, reads=[pyk, yk], writes=[yk])
                            else:
                                gcol = gates[:, tt, e_:e_ + 1]
                                if first:
                                    Sc.V("tensor_scalar", ya, py[:], gcol, None, ALU.mult, reads=[pyk, "gates"],
                                         writes=[yk])
                                else:
                                    Sc.V("scalar_tensor_tensor", ya, py[:], gcol, ya, ALU.mult, ALU.add,
                                         reads=[pyk, "gates", yk], writes=[yk])
                    if pending_ln:
                        pending_ln.pop(0)()
            for tt in range(8):
                def _ln(tt=tt, t0=t0, yacc=yacc, yb=yb):
                    r0 = t0 + tt * 128
                    b2 = tt % 2
                    hk, ok = ("ht", b2), ("oo", b2)
                    Sc.dma("sp", ht[b2][:], h_src[r0:r0 + 128, :], writes=[hk])
                    Sc.V("scalar_tensor_tensor", ht[b2][:], ht[b2][:], float(ALPHA), yacc[:, tt, :], ALU.mult, ALU.add,
                         reads=[hk, ("yacc", yb, tt, 0), ("yacc", yb, tt, 1)], writes=[hk])
                    layer_norm_tile(C, ht[b2][:], hk, oo[b2][:], ok, g_bc[:], b_bc[:], st[:, 2 * b2:2 * b2 + 2],
                                    ("st", b2), junk[:], "junk")
                    Sc.dma("sp", out_dst[r0:r0 + 128, :], oo[b2][:], reads=[ok], writes=["out_dst"])
                pending_ln.append(_ln)
        while pending_ln:
            pending_ln.pop(0)()
        Sc.emit()


def phase_E(C):
    nc, Sc, T = C.nc, C.S, C.T
    ps = C.ps
    with ExitStack() as es:
        def sb(name, shape, dt=F32):
            return es.enter_context(nc.sbuf_tensor("E_" + name, shape, dt))

        Win = sb("Win", [128, 8, 3072], F32R)
        ident = sb("ident", [128, 128])
        xin = [sb("xin%d" % i, [128, 4, 1024]) for i in range(2)]
        xT = sb("xT", [128, 8, 512], F32R)
        stg = Rot([(sb("stg%d" % i, [128, 512]), ("stg", i)) for i in range(6)])
        psr = Rot([(ps[i], ("ps", i)) for i in range(8)])
        for k in range(8):
            Sc.dma("pool", Win[:, k, :], T["o_w_in"][k * 128:(k + 1) * 128, :], writes=[("Win", k)])
        Sc.dma("sp", ident[:], T["c_ident"], writes=["ident"])
        n = [0]

        def evac(out, in_, reads, writes, scale=None):
            n[0] += 1
            if n[0] % 2 == 0:
                Sc.act(out, in_, AF.Copy, reads, writes, scale=float(scale if scale is not None else 1.0))
            else:
                Sc.V("tensor_scalar", out, in_, float(scale if scale is not None else 1.0), None, ALU.mult,
                     reads=reads, writes=writes)

        for blk in range(S // 512):
            t0 = blk * 512
            xi, xk = xin[blk % 2], ("xin", blk % 2)
            Sc.dma("sp", xi[:], T["s_x1"][t0:t0 + 512, :].rearrange("(a p) d -> p a d", p=128), writes=[xk])
            for k in range(8):
                p, pk = psr.next()
                for a in range(4):
                    Sc.tr(p[:, a * 128:(a + 1) * 128], xi[:, a, k * 128:(k + 1) * 128], ident[:],
                          reads=[xk, "ident"], writes=[pk])
                evac(xT[:, k, :], p[:], [pk], [("xT", k)])
            for j in range(16):
                p, pk = psr.next()
                c0 = j * 128
                for k in range(8):
                    Sc.mm(p[:], Win[:, k, c0:c0 + 128], xT[:, k, :], start=(k == 0), stop=(k == 7),
                          reads=[("Win", k), ("xT", k)], writes=[pk])
                st, sk = stg.next()
                if j < 8:
                    evac(st[:], p[:], [pk], [sk], scale=0.125)
                    Sc.dma("sp", T["s_qT1"][c0:c0 + 128, t0:t0 + 512], st[:], reads=[sk], writes=["s_qT1"])
                else:
                    evac(st[:], p[:], [pk], [sk])
                    Sc.dma("sp", T["s_kT1"][c0 - 1024:c0 - 896, t0:t0 + 512], st[:], reads=[sk], writes=["s_kT1"])
            for a in range(4):
                r0 = t0 + a * 128
                for half in range(2):
                    p, pk = psr.next()
                    c0 = 2048 + half * 512
                    for k in range(8):
                        Sc.mm(p[:], xT[:, k, a * 128:(a + 1) * 128], Win[:, k, c0:c0 + 512], start=(k == 0),
                              stop=(k == 7), reads=[("Win", k), ("xT", k)], writes=[pk])
                    st, sk = stg.next()
                    evac(st[:], p[:], [pk], [sk])
                    Sc.dma("sp", T["s_v1"][r0:r0 + 128, half * 512:(half + 1) * 512], st[:], reads=[sk],
                           writes=["s_v1"])
        Sc.emit()


def phase_F(C):
    nc, Sc, T = C.nc, C.S, C.T
    ps = C.ps
    with ExitStack() as es:
        def sb(name, shape, dt=F32):
            return es.enter_context(nc.sbuf_tensor("F_" + name, shape, dt))

        qz = [sb("qz%d" % i, [128, S], F32R) for i in range(2)]
        kT = sb("kT", [128, S], F32R)
        vaug = sb("vaug", [128, NT, 2, 128], F32R)
        selT = sb("selT", [128, S], F32R)
        ident = sb("ident", [128, 128])
        onesr = sb("onesr", [128, 128], F32R)
        xm = sb("xm", [128, 16, 128], F32R)
        gm = sb("gm", [128, 32, 16])
        adm = sb("adm", [128, 32, 16])
        km = sb("km", [128, 16])
        g0 = sb("g0", [128, 32, 16])
        g1 = sb("g1", [128, 32, 16])
        eq = sb("eq", [128, 32, 16])
        mxs = sb("mxs", [128, 32])
        OE = sb("OE", [128, 4, 512])
        EP = sb("EP", [128, 5, 512])
        pts = Rot([(sb("pt%d" % i, [128, 512]), ("pt", i)) for i in range(4)])
        pt2s = Rot([(sb("pt2%d" % i, [128, 512], F32R), ("pt2", i)) for i in range(5)])
        mts = Rot([(sb("mt%d" % i, [128, 512]), ("mt", i)) for i in range(3)])
        ost = Rot([(sb("ost%d" % i, [128, 512]), ("ost", i)) for i in range(2)])
        Ed, Eo = make_E(C, es, 0, 16, "F_")
        Sc.dma("pool", qz[0][64:128, :], T["c_zeros"][64:128, :], writes=["qz"])
        Sc.dma("pool", qz[1][0:64, :], T["c_zeros"][0:64, :], writes=["qz"])
        Sc.dma("pool", selT[:], T["c_zeros"], writes=["selT"])
        for r_ in range(2):
            Sc.dma("pool", vaug[:, :, r_, 64:128],
                   bass.AP(T["c_ones"].tensor, T["c_ones"].offset, [[128, 128], [0, NT], [1, 64]]), writes=["vaug"])
        Sc.dma("sp", ident[:], T["c_ident"], writes=["ident"])
        Sc.dma("pool", onesr[:], T["c_ones"], writes=["onesr"])
        Sc.dma("pool", xm[:], T["c_xm"], writes=["xm"])
        Sc.dma("sp", gm[:], T["c_gm"], writes=["gm"])
        Sc.dma("sp", adm[:], T["c_adm"], writes=["adm"])
        Sc.V("memset", OE[:], 0.0, writes=["OE"])
        Sc.V("memset", EP[:], 1.0, writes=["EP"])
        for pair in range(8):
            r0 = pair * 128
            Sc.dma("pool", qz[0][0:64, :], T["s_qT1"].bitcast(F32R)[r0:r0 + 64, :], writes=["qz"])
            Sc.dma("pool", qz[1][64:128, :], T["s_qT1"].bitcast(F32R)[r0 + 64:r0 + 128, :], writes=["qz"])
            Sc.dma("pool", kT[:], T["s_kT1"].bitcast(F32R)[r0:r0 + 128, :], writes=["kT"])
            for r_ in range(2):
                Sc.dma("pool", vaug[:, :, r_, 0:64],
                       T["s_v1"].bitcast(F32R)[:, r0 + r_ * 64:r0 + (r_ + 1) * 64].rearrange("(j p) c -> p j c", p=128),
                       writes=["vaug"])
            Sc.V("tensor_reduce", km[:], r32(kT[:]).rearrange("p (b s) -> p b s", s=256), AX.X, ALU.add,
                 reads=["kT"], writes=["km"])
            Sc.V("tensor_scalar", km[:], km[:], 1.0 / 256, None, ALU.mult, reads=["km"], writes=["km"])
            for r in range(2):
                h = pair * 2 + r
                pb = slice(r * 64, (r + 1) * 64)
                for ti in range(NT):
                    Sc.mm(ps[0][:, ti * 16:(ti + 1) * 16], r32(qz[r][pb, ti * 128:(ti + 1) * 128]), km[pb, :],
                          reads=["qz", "km"], writes=[("ps", 0)])
                g0f, g1f, eqf = g0[:], g1[:], eq[:]
                Sc.V("tensor_tensor", g0f, ps[0][:].rearrange("p (a b) -> p a b", b=16), gm[:], ALU.add,
                     reads=[("ps", 0), "gm"], writes=["g0"])
                src, srck = g0, "g0"
                for rnd in range(3):
                    Sc.V("tensor_reduce", mxs[:], src[:], AX.X, ALU.max, reads=[srck], writes=["mxs"])
                    if rnd < 2:
                        Sc.V("tensor_tensor", eqf, src[:], mxs[:].unsqueeze(2).to_broadcast([128, 32, 16]), ALU.is_ge,
                             reads=[srck, "mxs"], writes=["eq"])
                        Sc.V("scalar_tensor_tensor", g1f, eqf, -3.0e30, src[:], ALU.mult, ALU.add,
                             reads=["eq", srck], writes=["g1"])
                        src, srck = g1, "g1"
                Sc.V("tensor_tensor", eqf, g0f, mxs[:].unsqueeze(2).to_broadcast([128, 32, 16]), ALU.is_ge,
                     reads=["g0", "mxs"], writes=["eq"])
                Sc.V("tensor_tensor", eqf, eqf, adm[:], ALU.mult, reads=["eq", "adm"], writes=["eq"])
                for q4 in range(8):
                    p, pk = ps[1 + q4 % 2], ("ps", 1 + q4 % 2)
                    for a in range(4):
                        ti = q4 * 4 + a
                        Sc.tr(p[0:16, a * 128:(a + 1) * 128], eq[:, ti, :], ident[:], reads=["eq", "ident"],
                              writes=[pk])
                    Sc.act(selT[0:16, q4 * 512:(q4 + 1) * 512], p[0:16, :], AF.Copy, reads=[pk], writes=["selT"])
                for rel in range(4):
                    for a in range(4):
                        if rel // 2 == a // 2 and a - rel in (0, 1):
                            src_ = Ed if a == rel else Eo
                            Sc.V("tensor_copy", OE[:, rel, a * 128:(a + 1) * 128], src_[:, h, :],
                                 reads=["F_Ed", "F_Eo"], writes=["OE"])
                for reli in range(5):
                    rel = reli - 1
                    a = rel + 1
                    if 0 <= a < 4:
                        Sc.V("tensor_copy", EP[:, reli, a * 128:(a + 1) * 128], Eo[:, h, :], reads=["F_Eo"],
                             writes=["EP"])
                for I in range(S // 512):
                    qs_ = slice(I * 512, (I + 1) * 512)
                    po, pok = ps[6 + I % 2], ("ps", 6 + I % 2)
                    nk = 4 * I + 4
                    steps = []
                    for j in range(nk):
                        def fr(j=j, I=I, qs_=qs_, r=r):
                            rel = j - 4 * I
                            pS, pSk = ps[j % 3], ("ps", j % 3)
                            pM, pMk = ps[3 + j % 3], ("ps", 3 + j % 3)
                            Sc.mm(pS[:], kT[:, j * 128:(j + 1) * 128], qz[r][:, qs_], reads=["kT", "qz"], writes=[pSk])
                            Sc.mm(pM[:], xm[:, j // 2, :], selT[:, qs_], reads=["xm", "selT"], writes=[pMk])
                            pt, ptk = pts.next()
                            Sc.act(pt[:], pS[:], AF.Exp, reads=[pSk], writes=[ptk])
                            pt2, pt2k = pt2s.next()
                            if rel < -1:
                                Sc.V("tensor_tensor", pt2[:], pt[:], pM[:], ALU.mult, reads=[ptk, pMk], writes=[pt2k])
                            else:
                                mt, mtk = mts.next()
                                Sc.V("tensor_tensor", mt[:], EP[:, rel + 1, :], pM[:], ALU.mult, reads=["EP", pMk],
                                     writes=[mtk])
                                if rel >= 0:
                                    Sc.V("tensor_tensor", mt[:], mt[:], OE[:, rel, :], ALU.add, reads=[mtk, "OE"],
                                         writes=[mtk])
                                Sc.V("tensor_tensor", pt2[:], pt[:], mt[:], ALU.mult, reads=[ptk, mtk],
                                     writes=[pt2k])
                            return pt2, pt2k

                        def bk(cx, j=j, nk=nk, po=po, pok=pok, r=r):
                            pt2, pt2k = cx
                            Sc.mm(po[:], vaug[:, j, r, :], pt2[:], start=(j == 0), stop=(j == nk - 1),
                                  reads=["vaug", pt2k], writes=[pok])
                        steps.append((fr, bk))
                    pipeline(steps)
                    o_, ok = ost.next()
                    Sc.act(o_[:], po[:], AF.Copy, reads=[pok], writes=[ok])
                    Sc.dma("sp", T["s_oT1"][h * 64:(h + 1) * 64, qs_], o_[0:64, :], reads=[ok], writes=["s_oT1"])
                    Sc.dma("sp", T["s_su1"][h * 64:(h + 1) * 64, qs_], o_[64:128, :], reads=[ok], writes=["s_su1"])
        Sc.emit()


def phase_C0(C):
    nc, Sc, T = C.nc, C.S, C.T
    ps = C.ps
    with ExitStack() as es:
        def sb(name, shape, dt=F32):
            return es.enter_context(nc.sbuf_tensor("C0_" + name, shape, dt))

        src = {"k": sb("kcr", [64, 2, S]), "v": sb("vcr", [64, 2, S])}
        w1 = {"k": sb("w1k", [64, 32, 64]), "v": sb("w1v", [64, 32, 64])}
        w2 = {"k": sb("w2k", [64, 64]), "v": sb("w2v", [64, 64])}
        posT = {"k": sb("posk", [64, 32]), "v": sb("posv", [64, 32])}
        kpl = Rot([(sb("kpl%d" % i, [64, 256]), ("kpl", i)) for i in range(3)])
        xs = sb("xs", [64, 256])
        u2 = sb("u2", [64, 256])
        hid = sb("hid", [64, 256])
        stg = Rot([(sb("stg%d" % i, [128, 256]), ("stg", i)) for i in range(2)])
        pb = sb("pb", [32, 8, 128])
        b31 = sb("b31", [32, 8])
        pm = sb("pm", [32, 128])
        one_t = sb("one_t", [128, 8, 128])
        zero_t = sb("zero_t", [128, 8, 128])
        Sc.dma("sp", src["k"][:], T["s_kcT"].rearrange("(g d) t -> d g t", d=64), writes=["kcr"])
        Sc.dma("sp", src["v"][:], T["s_vcT"].rearrange("(g d) t -> d g t", d=64), writes=["vcr"])
        Sc.dma("sp", w1["k"][:], T["e_ck1"].rearrange("l d e -> d l e"), writes=["w1k"])
        Sc.dma("sp", w1["v"][:], T["e_cv1"].rearrange("l d e -> d l e"), writes=["w1v"])
        Sc.dma("sp", w2["k"][:], T["e_ck2"], writes=["w2k"])
        Sc.dma("sp", w2["v"][:], T["e_cv2"], writes=["w2v"])
        Sc.dma("sp", posT["k"][:], T["e_pos_kT"], writes=["posk"])
        Sc.dma("sp", posT["v"][:], T["e_pos_vT"], writes=["posv"])
        Sc.dma("sp", pb[:], T["g_pb"], writes=["pb"])
        Sc.dma("sp", b31[:], T["g_b31"][0:32, 8:16], writes=["b31"])
        Sc.dma("sp", pm[:], T["c_pm"], writes=["pm"])
        Sc.V("tensor_tensor", pb[:], pb[:], b31[:].unsqueeze(2).to_broadcast([32, 8, 128]), ALU.subtract,
             reads=["pb", "b31"], writes=["pb"])
        Sc.act(pb[:], pb[:], AF.Exp, reads=["pb"], writes=["pb"])
        Sc.V("tensor_tensor", pb[:], pb[:], pm[:].unsqueeze(1).to_broadcast([32, 8, 128]), ALU.mult,
             reads=["pb", "pm"], writes=["pb"])
        Sc.V("memset", one_t[:], 1.0, writes=["one_t"])
        Sc.V("memset", zero_t[:], 0.0, writes=["zero_t"])
        Sc.dma("sp", T["s_G"][0:128], one_t[:], reads=["one_t"], writes=["s_G"])
        Sc.dma("sp", T["s_G"][128:256], one_t[:], reads=["one_t"], writes=["s_G"])
        Sc.dma("sp", T["s_G"][256:288], pb[:], reads=["pb"], writes=["s_G"])
        Sc.dma("sp", T["s_G"][288:416], zero_t[:], reads=["zero_t"], writes=["s_G"])
        Sc.dma("sp", T["s_G"][416:528], zero_t[0:112], reads=["zero_t"], writes=["s_G"])
        Sc.V("memset", hid[:], 0.0, writes=["hid"])
        for which in ("k", "v"):
            for g in range(2):
                ph, phk = ps[g], ("ps", g)
                for l in range(32):
                    kp, kpk = kpl.next()
                    Sc.V("tensor_scalar", kp[:, 0:255], src[which][:, g, l:l + 16 * 254 + 1:16],
                         posT[which][:, l:l + 1], None, ALU.add, reads=[which + "cr", "pos" + which], writes=[kpk])
                    Sc.mm(ph[0:64, 0:255], w1[which][:, l, :], kp[:, 0:255], start=(l == 0), stop=(l == 31),
                          reads=["w1" + which, kpk], writes=[phk])
                Sc.act(xs[:, 0:255], ph[0:64, 0:255], AF.Copy, reads=[phk], writes=["xs"])
                Sc.V("tensor_tensor", u2[:, 0:255], xs[:, 0:255], xs[:, 0:255], ALU.mult, reads=["xs"], writes=["u2"])
                Sc.V("tensor_scalar", u2[:, 0:255], u2[:, 0:255], 0.044715, 1.0, ALU.mult, ALU.add, reads=["u2"],
                     writes=["u2"])
                Sc.V("tensor_tensor", u2[:, 0:255], u2[:, 0:255], xs[:, 0:255], ALU.mult, reads=["u2", "xs"],
                     writes=["u2"])
                Sc.act(u2[:, 0:255], u2[:, 0:255], AF.Sigmoid, reads=["u2"], writes=["u2"], scale=1.5957691216057308)
                Sc.V("tensor_tensor", hid[:, 0:255], xs[:, 0:255], u2[:, 0:255], ALU.mult, reads=["u2", "xs"],
                     writes=["hid"])
                if which == "k":
                    p2, p2k = ps[2 + g], ("ps", 2 + g)
                    Sc.mm(p2[0:64, 0:256], w2["k"][:], hid[:], reads=["w2k", "hid"], writes=[p2k])
                    st, sk = stg.next()
                    Sc.act(st[0:64, :], p2[0:64, 0:256], AF.Copy, reads=[p2k], writes=[sk])
                    Sc.dma("sp", T["s_kcc"][:, g, :], st[0:64, :], reads=[sk], writes=["s_kcc"])
                else:
                    for nt in range(2):
                        p2, p2k = ps[4 + nt], ("ps", 4 + nt)
                        Sc.mm(p2[:, 0:64], hid[:, nt * 128:(nt + 1) * 128], w2["v"][:], reads=["w2v", "hid"],
                              writes=[p2k])
                        st, sk = stg.next()
                        Sc.act(st[:, 0:64], p2[:, 0:64], AF.Copy, reads=[p2k], writes=[sk])
                        Sc.dma("sp", T["s_vcc"][nt * 128:(nt + 1) * 128, g, :], st[:, 0:64], reads=[sk],
                               writes=["s_vcc"])
        Sc.emit()


def phase_C(C):
    phase_C0(C)
    nc, Sc, T = C.nc, C.S, C.T
    ps = C.ps
    with ExitStack() as es:
        def sb(name, shape, dt=F32):
            return es.enter_context(nc.sbuf_tensor("C_" + name, shape, dt))

        ksT = sb("ksT", [128, S], F32R)
        kwT = sb("kwT", [128, S], F32R)
        vs = sb("vs", [128, NT, 2, 128], F32R)
        vw = sb("vw", [128, NT, 2, 128], F32R)
        kcT = sb("kcT", [128, 256], F32R)
        vc = sb("vc", [128, 2, 2, 64], F32R)
        qb = sb("qb", [128, 4, 8, 128], F32R)
        shiftM = sb("shiftM", [128, 64], F32R)
        poS = sb("poS", [128, 512], F32R)
        gtss = [sb("gts%d" % i, [24, 512]) for i in range(2)]
        xs = sb("xs", [64, 32, 128], BF16)
        ovl = sb("ovl", [128, 2, 64])
        w4 = sb("w4", [128, 128])
        gmask = sb("gmask", [24, 6, 4])
        ident = sb("ident", [128, 128])
        onesr = sb("onesr", [128, 128], F32R)
        fc = sb("fc", [128, 64])
        ac = sb("ac", [128, 64])
        Ft = [sb("Ft%d" % i, [128, 4, 128]) for i in range(2)]
        pts = Rot([(sb("pt%d" % i, [128, 512]), ("pt", i)) for i in range(3)])
        pt2s = Rot([(sb("pt2%d" % i, [128, 512], F32R), ("pt2", i)) for i in range(3)])
        pt2c = [sb("pt2c%d" % i, [128, 512], F32R) for i in range(2)]
        MTh = sb("MTh", [128, 4, 128])
        rcc = sb("rcc", [128, 512])
        pn = sb("pn", [128, 512])
        psT = sb("psT", [128, 2, 128])
        sc1 = sb("sc1", [128, 64])
        sc2 = sb("sc2", [128, 64])
        m8a = sb("m8a", [128, 8])
        m8b = sb("m8b", [128, 8])
        sel = sb("sel", [128, 64])
        selT = sb("selT", [64, 128], BF16)
        obs = [[sb("ob%d_%d" % (u_, i), [64, 512]) for i in range(3)] for u_ in range(2)]
        rcb = sb("rcb", [64, 512])
        Rbr = sb("Rbr", [24, 512], F32R)
        og = [sb("og%d" % i, [64, 512]) for i in range(2)]
        tmpc = sb("tmpc", [64, 512])
        Ed, Eo = make_E(C, es, 8, 8, "C_")

        Sc.dma("pool", ksT[:], T["s_ksT"].bitcast(F32R), writes=["ksT"])
        Sc.dma("pool", kwT[:], T["s_kwT"].bitcast(F32R), writes=["kwT"])
        ones_src = bass.AP(T["c_ones"].tensor, T["c_ones"].offset, [[128, 128], [0, NT], [1, 64]])
        for g_ in range(2):
            Sc.dma("pool", vs[:, :, g_, 0:64],
                   T["s_vs"].bitcast(F32R)[:, g_ * 64:(g_ + 1) * 64].rearrange("(j p) c -> p j c", p=128), writes=["vs"])
            Sc.dma("pool", vw[:, :, g_, 0:64],
                   T["s_vw"].bitcast(F32R)[:, g_ * 64:(g_ + 1) * 64].rearrange("(j p) c -> p j c", p=128), writes=["vw"])
            Sc.dma("pool", vs[:, :, g_, 64:128], ones_src, writes=["vs"])
            Sc.dma("pool", vw[:, :, g_, 64:128], ones_src, writes=["vw"])
            Sc.dma("pool", kcT[g_ * 64:(g_ + 1) * 64, :], T["s_kcc"].bitcast(F32R)[:, g_, :], writes=["kcT"])
        zsrc = T["c_zeros"][:, 0:2048].rearrange("p (a h t) -> p a h t", a=4, h=4)
        Sc.dma("pool", qb[64:128, :, 0:4, :], zsrc[64:128], writes=["qb"])
        Sc.dma("pool", qb[0:64, :, 4:8, :], zsrc[0:64], writes=["qb"])
        Sc.dma("pool", shiftM[:], T["c_shift"], writes=["shiftM"])
        Sc.dma("pool", vc[:], T["s_vcc"].bitcast(F32R).rearrange("(a p) g f -> p a g f", p=128), writes=["vc"])
        Sc.dma("sp", xs[:], T["c_xs"], writes=["xs"])
        Sc.dma("sp", ovl[:], T["c_ovl"], writes=["ovl"])
        Sc.dma("sp", w4[:], T["c_w4"], writes=["w4"])
        Sc.dma("sp", gmask[:], T["c_gmask"], writes=["gmask"])
        Sc.dma("sp", ident[:], T["c_ident"], writes=["ident"])
        Sc.dma("pool", onesr[:], T["c_ones"], writes=["onesr"])
        po, pok = ps[3], ("ps", 3)
        pu, puk = ps[4], ("ps", 4)
        scnt = [0]

        def nextS():
            scnt[0] += 1
            return ps[scnt[0] % 3], ("ps", scnt[0] % 3)

        def shift_sums():
            Sc.act(poS[:], po[:], AF.Copy, reads=[pok], writes=["poS"])
            Sc.mm(pu[0:64, :], shiftM[:], poS[:], reads=["shiftM", "poS"], writes=[puk])

        def finish_branch(br):
            if br == 0:
                Sc.V("tensor_tensor", ob[br][:], po[0:64, :], rcc[0:64, :], ALU.mult, reads=[pok, "rcc"],
                     writes=[(obk, br)])
                return
            Sc.act(rcb[:], pu[0:64, :], AF.Ln, reads=[puk], writes=["rcb"])
            Sc.act(rcb[:], rcb[:], AF.Exp, reads=["rcb"], writes=["rcb"], scale=-1.0)
            Sc.V("tensor_tensor", ob[br][:], po[0:64, :], rcb[:], ALU.mult, reads=[pok, "rcb"], writes=[(obk, br)])

        ucnt = [0]
        pending = [None]
        for blk in range(S // 512):
            t0 = blk * 512
            for a_ in range(4):
                qsrc = T["s_qbT"].bitcast(F32R)[:, t0 + a_ * 128:t0 + (a_ + 1) * 128].rearrange("(h d) t -> d h t", d=64)
                Sc.dma("pool", qb[0:64, a_, 0:4, :], qsrc[:, 0:4, :], writes=["qb"])
                Sc.dma("pool", qb[64:128, a_, 4:8, :], qsrc[:, 4:8, :], writes=["qb"])
            gts = gtss[blk % 2]
            gtk = ("gts", blk % 2)
            Sc.dma("sp", gts[:], T["s_gatesT"][:, t0:t0 + 512], writes=[gtk])
            for a in range(4):
                i = blk * 4 + a
                tsl = slice(a * 128, (a + 1) * 128)
                Sc.dma("sp", fc[:], T["c_fc"][:, i, :], writes=["fc"])
                Sc.dma("sp", ac[:], T["c_ac"][:, i, :], writes=["ac"])
                for g in range(2):
                    ucnt[0] += 1
                    ob = obs[ucnt[0] % 2]
                    obk = ("ob", ucnt[0] % 2)
                    hs = slice(4 * g, 4 * g + 4)
                    qrhs = qb[:, a, hs, :]
                    nts = [0] if i <= 14 else [0, 1]
                    for nt in nts:
                        r0 = nt * 128 - 8 * i + 272
                        Sc.dma("sp", Ft[nt][:], T["s_G"][r0:r0 + 128, hs, :], writes=[("Ft", nt)])
                    for q, nt in enumerate(nts):
                        pS, pSk = nextS()
                        Sc.mm(pS[:], kcT[:, nt * 128:(nt + 1) * 128], qrhs, reads=["kcT", "qb"], writes=[pSk])
                        pt, ptk = pts.next()
                        Sc.act(pt[:], pS[:], AF.Exp, reads=[pSk], writes=[ptk])
                        Sc.V("tensor_tensor", pt2c[nt][:], pt[:], Ft[nt][:], ALU.mult, reads=[ptk, ("Ft", nt)],
                             writes=[("pt2c", nt)])
                        Sc.mm(po[0:64, :], vc[:, nt, g, :], pt2c[nt][:], start=(q == 0), stop=(q == len(nts) - 1),
                              reads=["vc", ("pt2c", nt)], writes=[pok])
                        Sc.mm(pu[:], onesr[:], pt2c[nt][:], start=(q == 0), stop=(q == len(nts) - 1),
                              reads=["onesr", ("pt2c", nt)], writes=[puk])
                    if pending[0] is not None:
                        pending[0]()
                        pending[0] = None
                    Sc.V("tensor_scalar", rcc[:], pu[:], 1.0e-18, None, ALU.max, reads=[puk], writes=["rcc"])
                    Sc.act(rcc[:], rcc[:], AF.Ln, reads=["rcc"], writes=["rcc"])
                    Sc.act(rcc[:], rcc[:], AF.Exp, reads=["rcc"], writes=["rcc"], scale=-1.0)
                    finish_branch(0)
                    sel_ops = []
                    psc, psck = ps[5], ("ps", 5)
                    for q, nt in enumerate(nts):
                        def _s1(q=q, nt=nt):
                            Sc.V("tensor_tensor", pn[:], r32(pt2c[nt][:]), rcc[:], ALU.mult,
                                 reads=[("pt2c", nt), "rcc"], writes=["pn"])
                            Sc.V("tensor_reduce", psT[:, nt, :], pn[:].rearrange("p (h t) -> p t h", h=4), AX.X,
                                 ALU.add, reads=["pn"], writes=[("psT", nt)])
                            Sc.mm(psc[:, 0:64], psT[:, nt, :], ovl[:, nt, :], start=(q == 0),
                                  stop=(q == len(nts) - 1), reads=[("psT", nt), "ovl"], writes=[psck])
                        sel_ops.append(_s1)

                    def _s2():
                        Sc.V("tensor_tensor", sc1[:], psc[:, 0:64], fc[:], ALU.max, reads=[psck, "fc"], writes=["sc1"])
                        Sc.V("tensor_tensor", sc1[:], sc1[:], ac[:], ALU.min, reads=["sc1", "ac"], writes=["sc1"])
                        Sc.V("max", m8a[:], sc1[:], reads=["sc1"], writes=["m8a"])

                    def _s3():
                        Sc.V("match_replace", sc2[:], m8a[:], sc1[:], -3.0e30, reads=["sc1", "m8a"], writes=["sc2"])
                        Sc.V("max", m8b[:], sc2[:], reads=["sc2"], writes=["m8b"])
                        Sc.V("tensor_scalar", sel[:], sc1[:], m8b[:, 7:8], None, ALU.is_ge, reads=["sc1", "m8b"],
                             writes=["sel"])

                    def _s4():
                        pT, pTk = ps[5], ("ps", 5)
                        Sc.tr(pT[0:64, 0:128], sel[:], ident[:], reads=["sel", "ident"], writes=[pTk])
                        Sc.act(selT[:], pT[0:64, 0:128], AF.Copy, reads=[pTk], writes=["selT"])
                    sel_ops += [_s2, _s3, _s4]
                    j0 = max(0, i - 4)
                    steps = []
                    for j in range(j0, i + 1):
                        def fr(j=j, i=i, g=g, hs=hs, qrhs=qrhs):
                            d = i - j
                            pS, pSk = nextS()
                            Sc.mm(pS[:], kwT[:, j * 128:(j + 1) * 128], qrhs, reads=["kwT", "qb"], writes=[pSk])
                            pt2, pt2k = pt2s.next()
                            if d in (2, 3):
                                Sc.act(pt2[:], pS[:], AF.Exp, reads=[pSk], writes=[pt2k])
                            else:
                                pt, ptk = pts.next()
                                Sc.act(pt[:], pS[:], AF.Exp, reads=[pSk], writes=[ptk])
                                if d == 0:
                                    Sc.V("tensor_tensor", pt2[:].rearrange("p (h t) -> p h t", h=4),
                                         pt[:].rearrange("p (h t) -> p h t", h=4), Ed[:, hs, :], ALU.mult,
                                         reads=[ptk, "C_Ed"], writes=[pt2k])
                                elif d == 1:
                                    Sc.V("tensor_tensor", pt2[:].rearrange("p (h t) -> p h t", h=4),
                                         pt[:].rearrange("p (h t) -> p h t", h=4), Eo[:, hs, :], ALU.mult,
                                         reads=[ptk, "C_Eo"], writes=[pt2k])
                                else:
                                    Sc.V("tensor_tensor", pt2[:].rearrange("p (h t) -> p h t", h=4),
                                         pt[:].rearrange("p (h t) -> p h t", h=4),
                                         w4[:].unsqueeze(1).to_broadcast([128, 4, 128]), ALU.mult, reads=[ptk, "w4"],
                                         writes=[pt2k])
                            return pt2, pt2k

                        def bk(cx, j=j, i=i, g=g, j0=j0):
                            pt2, pt2k = cx
                            Sc.mm(po[:], vw[:, j, g, :], pt2[:], start=(j == j0), stop=(j == i),
                                  reads=["vw", pt2k], writes=[pok])
                        steps.append((fr, bk))
                    pend = []
                    for fr_, bk_ in steps:
                        cx_ = fr_()
                        if sel_ops:
                            sel_ops.pop(0)()
                        pend.append((bk_, cx_))
                        if len(pend) > 2:
                            b_, c_ = pend.pop(0)
                            b_(c_)
                    for b_, c_ in pend:
                        b_(c_)
                    while sel_ops:
                        sel_ops.pop(0)()
                    shift_sums()
                    finish_branch(2)
                    steps = []
                    for j in range(i + 1):
                        def fr(j=j, i=i, g=g, hs=hs, qrhs=qrhs):
                            pS, pSk = nextS()
                            pM, pMk = ps[6 + j % 2], ("ps", 6 + j % 2)
                            Sc.mm(pS[:], ksT[:, j * 128:(j + 1) * 128], qrhs, reads=["ksT", "qb"], writes=[pSk])
                            Sc.mm(pM[:, 0:128], xs[:, j, :], selT[:], reads=["xs", "selT"], writes=[pMk])
                            pt, ptk = pts.next()
                            Sc.act(pt[:], pS[:], AF.Exp, reads=[pSk], writes=[ptk])
                            pt2, pt2k = pt2s.next()
                            mb = pM[:, 0:128].unsqueeze(1).to_broadcast([128, 4, 128])
                            if j >= i - 1:
                                E = Ed if j == i else Eo
                                Sc.V("tensor_tensor", MTh[:], E[:, hs, :], mb, ALU.mult, reads=["C_Ed", "C_Eo", pMk],
                                     writes=["MTh"])
                                Sc.V("tensor_tensor", pt2[:].rearrange("p (h t) -> p h t", h=4),
                                     pt[:].rearrange("p (h t) -> p h t", h=4), MTh[:], ALU.mult,
                                     reads=[ptk, "MTh"], writes=[pt2k])
                            else:
                                Sc.V("tensor_tensor", pt2[:].rearrange("p (h t) -> p h t", h=4),
                                     pt[:].rearrange("p (h t) -> p h t", h=4), mb, ALU.mult, reads=[ptk, pMk],
                                     writes=[pt2k])
                            return pt2, pt2k

                        def bk(cx, j=j, i=i, g=g):
                            pt2, pt2k = cx
                            Sc.mm(po[:], vs[:, j, g, :], pt2[:], start=(j == 0), stop=(j == i),
                                  reads=["vs", pt2k], writes=[pok])
                        steps.append((fr, bk))
                    pipeline(steps)
                    shift_sums()
                    finish_branch(1)
                    def _combine(i=i, g=g, a=a, t0=t0, tsl=tsl, ob=ob, obk=obk, gts=gts, gtk=gtk):
                        ogt, ogk = og[(2 * i + g) % 2], ("og", (2 * i + g) % 2)
                        for br in range(3):
                            Sc.V("tensor_tensor", Rbr[:].rearrange("p (h t) -> p h t", h=4),
                                 gts[:, tsl].unsqueeze(1).to_broadcast([24, 4, 128]),
                                 gmask[:, g * 3 + br, :].unsqueeze(2).to_broadcast([24, 4, 128]), ALU.mult,
                                 reads=[gtk, "gmask"], writes=["Rbr"])
                            pg, pgk = ps[5], ("ps", 5)
                            Sc.mm(pg[0:64, :], onesr[0:24, 0:64], Rbr[:], reads=["onesr", "Rbr"], writes=[pgk])
                            if br == 0:
                                Sc.V("tensor_tensor", ogt[:], ob[0][:], pg[0:64, :], ALU.mult, reads=[(obk, 0), pgk],
                                     writes=[ogk])
                            else:
                                Sc.V("tensor_tensor", tmpc[:], ob[br][:], pg[0:64, :], ALU.mult, reads=[(obk, br), pgk],
                                     writes=["tmpc"])
                                Sc.V("tensor_tensor", ogt[:], ogt[:], tmpc[:], ALU.add, reads=[ogk, "tmpc"], writes=[ogk])
                        rbase = 512 + 4 * g * 64
                        Sc.dma("sp", T["s_oT"][rbase:rbase + 256, t0 + a * 128:t0 + (a + 1) * 128].rearrange(
                            "(h d) t -> d h t", d=64), ogt[:].rearrange("p (h t) -> p h t", h=4), reads=[ogk],
                            writes=["s_oT"])
                    pending[0] = _combine
        if pending[0] is not None:
            pending[0]()
        Sc.emit()


ALL_PHASES = ("A", "B", "C", "D1", "D2", "E", "F", "G", "H")


def kernel(**inputs):
    inputs = {k: np.asarray(v) for k, v in inputs.items()}
    nb = inputs["x"].shape[0]
    feeds = make_feeds(inputs, list(range(nb)), ALL_PHASES)
    nc = build(phases=ALL_PHASES)
    res = run_bass_kernel_spmd(nc, feeds, core_ids=list(range(nb)))
    out = np.stack([np.asarray(r["out"], dtype=np.float32) for r in res.results], axis=0)
    return out
```
